# Optimizing a Trainium2 kernel written in Bass

```python
import math
import jax, jax.numpy as jnp
from jax import lax
import numpy as np

D_MODEL = 1024
BATCH = 16
SEQ = 4096
DEPTH = 1

PLE_DIM = 256
D_FF = 2816
D_MIX = D_MODEL
RET_WIDTH = D_MIX // 2
RET_HEADS = 8
RET_HEAD_DIM = RET_WIDTH // RET_HEADS
CONV_WIDTH = D_MIX - RET_WIDTH
CONV_KERNEL = 31
CHUNK = 128
ROPE_BASE = 10000.0
EPS = 1e-6
D_IN_PROJ = 4 * RET_WIDTH + 2 * CONV_WIDTH

kernel_name = "hybrid_retention_conformerconv_macaron_layer"


def _rmsnorm(x, g):
    xf = x.astype(jnp.float32)
    y = xf * lax.rsqrt(jnp.mean(xf * xf, axis=-1, keepdims=True) + EPS)
    return (y * g.astype(jnp.float32)).astype(x.dtype)


def _layernorm(x, g, b):
    xf = x.astype(jnp.float32)
    mu = jnp.mean(xf, axis=-1, keepdims=True)
    xc = xf - mu
    y = xc * lax.rsqrt(jnp.mean(xc * xc, axis=-1, keepdims=True) + EPS)
    return (y * g.astype(jnp.float32) + b.astype(jnp.float32)).astype(x.dtype)


def _swiglu(x, w_gu, w_down):
    gu = x @ w_gu
    g, u = jnp.split(gu, 2, axis=-1)
    return (jax.nn.silu(g) * u) @ w_down


def _rotary(x, cos, sin):
    x1, x2 = jnp.split(x, 2, axis=-1)
    c = cos[None, :, None, :].astype(x.dtype)
    s = sin[None, :, None, :].astype(x.dtype)
    return jnp.concatenate([x1 * c - x2 * s, x1 * s + x2 * c], axis=-1)


def _retention_chunkwise(q, k, v):
    B_, S_, H, d = q.shape
    N = S_ // CHUNK
    dt = q.dtype
    log_gamma = jnp.log1p(-jnp.exp2(-5.0 - jnp.arange(H, dtype=jnp.float32)))
    pos = jnp.arange(CHUNK, dtype=jnp.float32)
    diff = pos[:, None] - pos[None, :]
    intra_decay = jnp.where(diff[None] >= 0,
                            jnp.exp(log_gamma[:, None, None] * jnp.maximum(diff, 0.0)[None]),
                            0.0).astype(dt)
    k_decay = jnp.exp(log_gamma[None, :] * (CHUNK - 1.0 - pos)[:, None]).astype(dt)
    q_decay = jnp.exp(log_gamma[None, :] * (pos + 1.0)[:, None]).astype(dt)
    chunk_decay = jnp.exp(log_gamma * CHUNK).astype(dt)

    qc = q.reshape(B_, N, CHUNK, H, d)
    kc = k.reshape(B_, N, CHUNK, H, d)
    vc = v.reshape(B_, N, CHUNK, H, d)

    scores = jnp.einsum('bnihd,bnjhd->bnhij', qc, kc) * intra_decay[None, None]
    intra = jnp.einsum('bnhij,bnjhd->bnihd', scores, vc)

    kv = jnp.einsum('bnjhd,bnjhe->bnhde', kc * k_decay[None, None, :, :, None], vc)

    def step(R, kv_n):
        return R * chunk_decay[None, :, None, None] + kv_n, R

    R0 = jnp.zeros((B_, H, d, d), dtype=kv.dtype)
    _, R_prev = lax.scan(step, R0, jnp.moveaxis(kv, 1, 0))
    R_prev = jnp.moveaxis(R_prev, 0, 1)
    cross = jnp.einsum('bnihd,bnhde->bnihe', qc * q_decay[None, None, :, :, None], R_prev)
    return (intra + cross).reshape(B_, S_, H, d)


def _causal_depthwise_conv(x, w, b):
    C = x.shape[-1]
    y = lax.conv_general_dilated(
        x, w[:, None, :].astype(x.dtype), window_strides=(1,),
        padding=[(CONV_KERNEL - 1, 0)],
        dimension_numbers=('NWC', 'WIO', 'NWC'),
        feature_group_count=C)
    return y + b.astype(x.dtype)


def setup_inputs(seed: int = 0) -> dict:
    key = jax.random.key(seed)
    ks = iter(jax.random.split(key, 40))

    def w(shape, fan_in):
        return jax.random.normal(next(ks), shape, jnp.float32) * (fan_in ** -0.5)

    def gain(shape):
        return 1.0 + 0.1 * jax.random.normal(next(ks), shape, jnp.float32)

    def bias(shape):
        return 0.02 * jax.random.normal(next(ks), shape, jnp.float32)

    L = DEPTH
    return {
        "x": jax.random.normal(next(ks), (BATCH, SEQ, D_MODEL), jnp.float32),
        "p": jax.random.normal(next(ks), (DEPTH, BATCH, SEQ, PLE_DIM), jnp.float32),
        "ffn1_pre_g": gain((L, D_MODEL)),
        "ffn1_w_gu": w((L, D_MODEL, 2 * D_FF), D_MODEL),
        "ffn1_w_down": w((L, D_FF, D_MODEL), D_FF),
        "ffn1_post_g": gain((L, D_MODEL)),
        "mix_pre_g": gain((L, D_MODEL)),
        "w_in": w((L, D_MODEL, D_IN_PROJ), D_MODEL),
        "ret_gn_g": gain((L, RET_WIDTH)),
        "ret_gn_b": bias((L, RET_WIDTH)),
        "conv_w": w((L, CONV_KERNEL, CONV_WIDTH), CONV_KERNEL),
        "conv_b": bias((L, CONV_WIDTH)),
        "conv_ln_g": gain((L, CONV_WIDTH)),
        "conv_ln_b": bias((L, CONV_WIDTH)),
        "w_out": w((L, D_MIX, D_MODEL), D_MIX),
        "mix_post_g": gain((L, D_MODEL)),
        "ffn2_pre_g": gain((L, D_MODEL)),
        "ffn2_w_gu": w((L, D_MODEL, 2 * D_FF), D_MODEL),
        "ffn2_w_down": w((L, D_FF, D_MODEL), D_FF),
        "ffn2_post_g": gain((L, D_MODEL)),
        "ple_w": w((L, PLE_DIM, D_MODEL), PLE_DIM),
        "ple_gate_norm_g": gain((L, D_MODEL)),
        "ple_gate_w": w((L, D_MODEL, D_MODEL), D_MODEL),
        "ple_post_g": gain((L, D_MODEL)),
    }


def reference(x, p, ffn1_pre_g, ffn1_w_gu, ffn1_w_down, ffn1_post_g,
              mix_pre_g, w_in, ret_gn_g, ret_gn_b, conv_w, conv_b, conv_ln_g, conv_ln_b,
              w_out, mix_post_g,
              ffn2_pre_g, ffn2_w_gu, ffn2_w_down, ffn2_post_g,
              ple_w, ple_gate_norm_g, ple_gate_w, ple_post_g):
    B_, S_, _ = x.shape
    inv_freq = ROPE_BASE ** (-jnp.arange(0, RET_HEAD_DIM, 2, dtype=jnp.float32) / RET_HEAD_DIM)
    ang = jnp.arange(S_, dtype=jnp.float32)[:, None] * inv_freq[None, :]
    cos, sin = jnp.cos(ang), jnp.sin(ang)
    q_scale = RET_HEAD_DIM ** -0.5

    h = x
    for i in range(DEPTH):
        f = _swiglu(_rmsnorm(h, ffn1_pre_g[i]), ffn1_w_gu[i], ffn1_w_down[i])
        h = h + 0.5 * _rmsnorm(f, ffn1_post_g[i])

        u = _rmsnorm(h, mix_pre_g[i])
        proj = u @ w_in[i]
        q, k, v, g_ret, glu_a, glu_b = jnp.split(
            proj, [RET_WIDTH, 2 * RET_WIDTH, 3 * RET_WIDTH, 4 * RET_WIDTH,
                   4 * RET_WIDTH + CONV_WIDTH], axis=-1)

        q = _rotary(q.reshape(B_, S_, RET_HEADS, RET_HEAD_DIM), cos, sin) * q_scale
        k = _rotary(k.reshape(B_, S_, RET_HEADS, RET_HEAD_DIM), cos, sin)
        v = v.reshape(B_, S_, RET_HEADS, RET_HEAD_DIM)
        o = _retention_chunkwise(q, k, v)
        o = _layernorm(o, ret_gn_g[i].reshape(RET_HEADS, RET_HEAD_DIM),
                       ret_gn_b[i].reshape(RET_HEADS, RET_HEAD_DIM))
        ret_out = jax.nn.silu(g_ret) * o.reshape(B_, S_, RET_WIDTH)

        c = glu_a * jax.nn.sigmoid(glu_b)
        c = _causal_depthwise_conv(c, conv_w[i], conv_b[i])
        conv_out = jax.nn.silu(_layernorm(c, conv_ln_g[i], conv_ln_b[i]))

        mixed = jnp.concatenate([ret_out, conv_out], axis=-1) @ w_out[i]
        h = h + _rmsnorm(mixed, mix_post_g[i])

        f = _swiglu(_rmsnorm(h, ffn2_pre_g[i]), ffn2_w_gu[i], ffn2_w_down[i])
        h = h + 0.5 * _rmsnorm(f, ffn2_post_g[i])

        e = p[i] @ ple_w[i]
        gate = jax.nn.sigmoid(_rmsnorm(h, ple_gate_norm_g[i]) @ ple_gate_w[i])
        h = h + _rmsnorm(gate * e, ple_post_g[i])
    return h
```

```python
import contextlib
import numpy as np
import ml_dtypes
import concourse.bass as bass
import concourse.mybir as mybir
from concourse.bass_utils import run_bass_kernel_spmd

F32 = mybir.dt.float32
BF16 = mybir.dt.bfloat16
I32 = mybir.dt.int32
AF = mybir.ActivationFunctionType
ALU = mybir.AluOpType

D = 1024
DFF = 2816
NFC = 22
SEQ = 4096
BATCH = 16
NCORES = 8
TOK_CORE = BATCH * SEQ // NCORES
T = 512
NTILES = TOK_CORE // T
TILES_PER_SEQ = SEQ // T
EPS = 1e-6
NSLOT = 4
PIECE = 4096
NPIECE = 45
KCONV = 31

P_GU1 = 0
P_DN1 = 11
P_WIN = 17
P_WOUT = 23
P_GU2 = 25
P_DN2 = 36
P_PLEW = 42
P_GATE = 43
WIN_ORDER = [4, 5, 0, 1, 2, 3]

C_GPRE = 0
C_GPOST = 32
C_CONVW = C_GPOST + 4096
C_CONVB = C_CONVW + 124
C_CLNG = C_CONVB + 4
C_CLNB = C_CLNG + 4
C_RGG = C_CLNB + 4
C_RGB = C_RGG + 4
NCPAR = C_RGB + 4

F_QDEC = 0
F_MASK = 512
F_BDM = 1536
F_KDEC = 1664
F_CDEC = 1672
F_MHALF = 1676
NCF = 1680


class Buf:
    __slots__ = ("name", "w", "r", "over", "excl")

    def __init__(self, name, excl=False):
        self.name = name
        self.w = {}
        self.r = {}
        self.over = []
        self.excl = excl


def overlap(a_list, b_list):
    for a in a_list:
        for b in b_list:
            a.over.append(b)
            b.over.append(a)


class Prog:
    ENG = ("pe", "act", "dve", "pool", "sp")

    def __init__(self, nc):
        self.nc = nc
        self.engs = {"pe": nc.tensor, "act": nc.scalar, "dve": nc.vector, "pool": nc.gpsimd, "sp": nc.sync}
        self.ops = []
        self.nseq = {e: 0 for e in self.ENG}
        self.seen = {e: {} for e in self.ENG}
        self.nchan = 0
        self.chan_count = []

    def chan(self):
        self.chan_count.append(0)
        self.nchan += 1
        return self.nchan - 1

    def _deps(self, eng, reads, writes):
        deps = {}
        seen = self.seen[eng]

        def need(ev, hazard):
            kind, key, val = ev
            if kind == "c" and key == eng:
                if eng == "pe" or hazard == "WAR":
                    return
            k = (kind, key)
            if seen.get(k, 0) >= val:
                return
            if deps.get(k, 0) < val:
                deps[k] = val

        for b in reads:
            for ev in b.w.values():
                need(ev, "RAW")
            if b.excl:
                for (kk, key), ev in b.r.items():
                    if not (kk == "c" and key == eng):
                        need(ev, "RAR")
        for b in writes:
            for bb in [b] + b.over:
                for ev in bb.w.values():
                    need(ev, "WAW")
                for ev in bb.r.values():
                    need(ev, "WAR")
        for k, v in deps.items():
            seen[k] = v
        return deps

    def op(self, eng, name, kw, reads=(), writes=()):
        deps = self._deps(eng, reads, writes)
        self.nseq[eng] += 1
        seq = self.nseq[eng]
        ev = ("c", eng, seq)
        for b in reads:
            b.r[("c", eng)] = ev
        for b in writes:
            b.w = {("c", eng): ev}
            b.r = {}
        self.ops.append((eng, name, kw, deps, seq, None))

    def dma(self, q, out, in_, reads, writes, chan):
        deps = self._deps(q, reads, writes)
        self.nseq[q] += 1
        seq = self.nseq[q]
        self.chan_count[chan] += 16
        ev = ("d", chan, self.chan_count[chan])
        for b in reads:
            b.r[("d", chan)] = ev
        for b in writes:
            b.w = {("d", chan): ev}
            b.r = {}
        self.ops.append((q, "dma_start", dict(out=out, in_=in_), deps, seq, chan))

    def wait_all(self, eng, bufs):
        deps = self._deps(eng, bufs, bufs)
        self.nseq[eng] += 1
        self.ops.append((eng, None, None, deps, self.nseq[eng], None))

    def emit(self, stack):
        nc = self.nc
        esem = {e: stack.enter_context(nc.semaphore("s_" + e)) for e in self.ENG}
        csem = [stack.enter_context(nc.semaphore("c_%d" % i)) for i in range(self.nchan)]
        needed = {e: set() for e in self.ENG}
        for (_, _, _, deps, _, _) in self.ops:
            for (kind, key), val in deps.items():
                if kind == "c":
                    needed[key].add(val)
        cnt = {}
        for e in self.ENG:
            cnt[e] = {s: i + 1 for i, s in enumerate(sorted(needed[e]))}
        nwait = 0
        for (eng, name, kw, deps, seq, chan) in self.ops:
            E = self.engs[eng]
            for (kind, key), val in deps.items():
                if kind == "c":
                    E.wait_ge(esem[key], cnt[key][val])
                else:
                    E.wait_ge(csem[key], val)
                nwait += 1
            if name is None:
                continue
            ins = getattr(E, name)(**kw)
            if chan is not None:
                ins.then_inc(csem[chan], 16)
            elif seq in cnt[eng]:
                ins.then_inc(esem[eng], 1)
        return nwait


def _const_tables():
    lg = np.log1p(-np.exp2(-5.0 - np.arange(8, dtype=np.float32))).astype(np.float32)
    pos = np.arange(128, dtype=np.float32)
    diff = pos[None, :] - pos[:, None]
    mask = np.where(diff[:, None, :] >= 0,
                    np.exp(lg[None, :, None] * np.maximum(diff, 0.0)[:, None, :]), 0.0).astype(np.float32)
    mask = mask[:, [0, 2, 4, 6, 1, 3, 5, 7], :]
    kdec = np.exp(lg[None, :] * (127.0 - pos)[:, None]).astype(np.float32)
    qdec_h = np.exp(lg[:, None] * (pos + 1.0)[None, :]).astype(np.float32)
    cdec_h = np.exp(lg * 128.0).astype(np.float32)
    qdec = np.zeros((128, 4, 128), np.float32)
    cdec = np.zeros((128, 4), np.float32)
    for p in range(4):
        for hh in range(2):
            qdec[hh * 64:(hh + 1) * 64, p, :] = qdec_h[2 * p + hh][None, :]
            cdec[hh * 64:(hh + 1) * 64, p] = cdec_h[2 * p + hh]
    bdm = np.zeros((128, 128), np.float32)
    bdm[:64, :64] = 1.0
    bdm[64:, 64:] = 1.0
    cf = np.zeros((128, NCF), np.float32)
    cf[:, F_QDEC:F_QDEC + 512] = qdec.reshape(128, 512)
    cf[:, F_MASK:F_MASK + 1024] = mask.reshape(128, 1024)
    cf[:, F_BDM:F_BDM + 128] = bdm
    cf[:, F_KDEC:F_KDEC + 8] = kdec
    cf[:, F_CDEC:F_CDEC + 4] = cdec
    cf[:, F_MHALF] = -0.5
    inv_freq = (10000.0 ** (-np.arange(0, 64, 2, dtype=np.float32) / 64.0)).astype(np.float32)
    ang = (np.arange(SEQ, dtype=np.float32)[:, None] * inv_freq[None, :]).astype(np.float32)
    cos = np.cos(ang).astype(np.float32)
    sin = np.sin(ang).astype(np.float32)
    cos64 = np.concatenate([cos, cos], axis=1).T
    ssin64 = np.concatenate([-sin, sin], axis=1).T
    cosT = np.ascontiguousarray(np.concatenate([cos64, cos64], axis=0))
    ssinT = np.ascontiguousarray(np.concatenate([ssin64, ssin64], axis=0))
    ident = np.eye(128, dtype=np.float32)
    pm = np.zeros((128, 128), np.float32)
    for f in range(128):
        pm[f ^ 32, f] = 1.0
    bd64 = bdm / 64.0
    o512 = np.full((128, 128), 1.0 / 512.0, np.float32)
    cb = np.concatenate([ident, pm, bd64, o512], axis=1).astype(ml_dtypes.bfloat16)
    return cf, cosT, ssinT, cb


def _layout_weights(inp):
    wp = np.zeros((NPIECE, 128, PIECE), np.float32)

    def rows(w):
        return w.reshape(-1, 128, w.shape[1]).transpose(1, 0, 2)

    for base_gu, base_dn, wgu, wdn in ((P_GU1, P_DN1, inp["ffn1_w_gu"][0], inp["ffn1_w_down"][0]),
                                        (P_GU2, P_DN2, inp["ffn2_w_gu"][0], inp["ffn2_w_down"][0])):
        r = rows(wgu)
        for jj in range(11):
            pc = wp[base_gu + jj].reshape(128, 8, 512)
            pc[:, :, 0:256] = r[:, :, jj * 256:(jj + 1) * 256]
            pc[:, :, 256:512] = r[:, :, DFF + jj * 256:DFF + (jj + 1) * 256]
        r = rows(wdn)
        for dh in range(2):
            for fb in range(3):
                n = min(8, NFC - fb * 8)
                pc = wp[base_dn + dh * 3 + fb].reshape(128, 8, 512)
                pc[:, 0:n, :] = r[:, fb * 8:fb * 8 + n, dh * 512:(dh + 1) * 512]
    r = rows(inp["w_in"][0])
    for i, cbk in enumerate(WIN_ORDER):
        wp[P_WIN + i].reshape(128, 8, 512)[:] = r[:, :, cbk * 512:(cbk + 1) * 512]
    r = rows(inp["w_out"][0])
    for dh in range(2):
        wp[P_WOUT + dh].reshape(128, 8, 512)[:] = r[:, :, dh * 512:(dh + 1) * 512]
    r = rows(inp["ple_w"][0])
    wp[P_PLEW][:, 0:2048] = r.reshape(128, 2048)
    r = rows(inp["ple_gate_w"][0])
    for dh in range(2):
        wp[P_GATE + dh].reshape(128, 8, 512)[:] = r[:, :, dh * 512:(dh + 1) * 512]
    return wp


def _layout_params(inp):
    cp = np.zeros((128, NCPAR), np.float32)

    def col(v, n):
        return v.reshape(n, 128).T

    for i, k in enumerate(("ffn1_pre_g", "mix_pre_g", "ffn2_pre_g", "ple_gate_norm_g")):
        cp[:, C_GPRE + 8 * i:C_GPRE + 8 * (i + 1)] = col(inp[k][0], 8)
    for i, k in enumerate(("ffn1_post_g", "mix_post_g", "ffn2_post_g", "ple_post_g")):
        cp[:, C_GPOST + 1024 * i:C_GPOST + 1024 * (i + 1)] = np.broadcast_to(inp[k][0][None, :], (128, 1024))
    cw = inp["conv_w"][0]
    cp[:, C_CONVW:C_CONVW + 124] = cw.T.reshape(4, 128, KCONV).transpose(1, 0, 2).reshape(128, 124)
    cp[:, C_CONVB:C_CONVB + 4] = col(inp["conv_b"][0], 4)
    cp[:, C_CLNG:C_CLNG + 4] = col(inp["conv_ln_g"][0], 4)
    cp[:, C_CLNB:C_CLNB + 4] = col(inp["conv_ln_b"][0], 4)
    cp[:, C_RGG:C_RGG + 4] = col(inp["ret_gn_g"][0], 4)
    cp[:, C_RGB:C_RGB + 4] = col(inp["ret_gn_b"][0], 4)
    return cp


def build_program(ntiles=NTILES, debug=None):
    nc = bass.Bass("TRN2", target_bir_lowering=False)
    x_d = nc.dram_tensor("x", [TOK_CORE, D], F32, kind="ExternalInput").ap()
    p_d = nc.dram_tensor("p", [TOK_CORE, 256], F32, kind="ExternalInput").ap()
    wp_d = nc.dram_tensor("wp", [NPIECE, 128, PIECE], F32, kind="ExternalInput").ap()
    cpar_d = nc.dram_tensor("cpar", [128, NCPAR], F32, kind="ExternalInput").ap()
    cf_d = nc.dram_tensor("cf", [128, NCF], F32, kind="ExternalInput").ap()
    cos_d = nc.dram_tensor("cosT", [128, SEQ], F32, kind="ExternalInput").ap()
    ssin_d = nc.dram_tensor("ssinT", [128, SEQ], F32, kind="ExternalInput").ap()
    cb_d = nc.dram_tensor("cb", [128, 512], BF16, kind="ExternalInput").ap()
    out_d = nc.dram_tensor("out", [TOK_CORE, D], F32, kind="ExternalOutput").ap()
    scr_d = nc.dram_tensor("wscr", [NPIECE, 128, PIECE], BF16, kind="Internal").ap()

    stack = contextlib.ExitStack()
    with stack:
        def sb(name, shape, dt):
            return stack.enter_context(nc.sbuf_tensor(name, shape, dt))

        P = Prog(nc)

        h_t = sb("h", [128, 4, D], F32)
        arenaA = sb("arenaA", [128, 4096], F32)
        fbuf = arenaA[:].rearrange("p (m d) -> p m d", m=4)
        xn = arenaA[:, 0:2048].bitcast(BF16).rearrange("p (m d) -> p m d", m=4)
        xT = arenaA[:, 2048:4096].bitcast(BF16).rearrange("p (c t) -> p c t", c=8)
        arenaB = sb("arenaB", [128, NFC * 512], BF16)
        hidT = arenaB[:].rearrange("p (j t) -> p j t", j=NFC)
        cext = arenaB[:, 0:2168].rearrange("p (c t) -> p c t", c=4)
        cacc = arenaB[:, 4336:8432].bitcast(F32).rearrange("p (c t) -> p c t", c=4)
        ctmp = arenaB[:, 8432:9456].bitcast(F32)
        wring = sb("wring", [128, NSLOT, PIECE], BF16)
        ptile = sb("ptile", [128, 4, 256], F32)
        p_bf = sb("p_bf", [128, 4, 256], BF16)
        pT = sb("pT", [128, 2, 512], BF16)
        cpar = sb("cpar_s", [128, NCPAR], F32)
        cf = sb("cf_s", [128, NCF], F32)
        cb = sb("cb_s", [128, 512], BF16)
        cosS = sb("cosS", [128, 512], F32)
        ssinS = sb("ssinS", [128, 512], F32)
        sgt = sb("sgt", [128, 2, 512], F32)
        junk = sb("junk", [128, 1024], BF16)
        stats = sb("stats", [128, 64], F32)
        qraw = sb("qraw", [128, 512], BF16)
        t1 = sb("t1", [128, 512], F32)
        t2 = sb("t2", [128, 512], F32)
        qT = sb("qT", [128, 4, 512], BF16)
        qdT = sb("qdT", [128, 4, 512], BF16)
        kT = sb("kT", [128, 4, 512], BF16)
        kd = sb("kd", [128, 2, 512], BF16)
        vpl = sb("vpl", [128, 4, 512], BF16)
        vpad = sb("vpad", [128, 2, 8, 128], BF16)
        sgr = sb("sgr", [128, 4, 512], BF16)
        sgrb = sb("sgrb", [128, 4, 512], BF16)
        sTm = sb("sTm", [128, 2, 8, 128], BF16)
        R_t = sb("R", [128, 512], F32)
        Rbf = sb("Rbf", [128, 512], BF16)
        kvt = sb("kvt", [128, 512], F32)
        o_f = sb("o_f", [128, 512], F32)
        o_bf = sb("o_bf", [128, 512], BF16)
        cen2 = sb("cen", [128, 2, 512], F32)
        csq2 = sb("csq", [128, 2, 512], BF16)
        vare = sb("vare", [128, 512], F32)
        rstdL = sb("rstdL", [128, 512], F32)
        zt = sb("zt", [128, 512], F32)
        mixT = sb("mixT", [128, 8, 512], BF16)
        cb16 = sb("cb16", [128, 4, 512], BF16)
        halo = sb("halo", [128, 4, 30], BF16)
        dg = sb("dg", [128, 2, 8, 128], BF16)
        fple = arenaB[:, 0:8192].bitcast(F32).rearrange("p (m d) -> p m d", m=4)
        banks = [stack.enter_context(nc.psum_tensor("bank%d" % i, [128, 512], F32)) for i in range(8)]

        ident = cb[:, 0:128]
        pm = cb[:, 128:256]
        bd64 = cb[:, 256:384]
        o512 = cb[:, 384:512]
        mhalf = cf[:, F_MHALF:F_MHALF + 1]

        B_h = [Buf("h%d" % m) for m in range(4)]
        B_xn = [Buf("xn%d" % m) for m in range(4)]
        B_xT = Buf("xT")
        B_f = [Buf("f%d" % m) for m in range(4)]
        overlap(B_f, B_xn + [B_xT])
        B_hid = [Buf("hid%d" % j) for j in range(NFC)]
        B_cext = [Buf("cext%d" % c) for c in range(4)]
        B_cacc = [Buf("cacc%d" % c) for c in range(4)]
        B_ctmp = Buf("ctmp")
        overlap(B_hid, B_cext + B_cacc + [B_ctmp])
        B_fp = [Buf("fp%d" % m) for m in range(4)]
        overlap(B_fp, B_hid + B_cext + B_cacc + [B_ctmp])
        B_halo = [Buf("halo%d" % c) for c in range(4)]
        B_dg = [Buf("dg0"), Buf("dg1")]
        B_ss_f, B_ss_p = Buf("ss8f"), Buf("ss8p")
        B_slot = [Buf("slot%d" % s) for s in range(NSLOT)]
        B_scr = [Buf("scr%d" % k) for k in range(NPIECE)]
        B_bank = [Buf("bank%d" % i, excl=True) for i in range(8)]
        B_const = Buf("const")
        B_tab = Buf("tab")
        B_p = Buf("ptile")
        B_pbf = Buf("pbf")
        B_pT = Buf("pT")
        B_sg = [Buf("sg0"), Buf("sg1")]
        B_st = [Buf("st%d" % i) for i in range(16)]
        B_qraw, B_t1, B_t2 = Buf("qraw"), Buf("t1"), Buf("t2")
        B_qT = [Buf("qT%d" % c) for c in range(4)]
        B_qdT = [Buf("qdT%d" % c) for c in range(4)]
        B_kT = [Buf("kT%d" % c) for c in range(4)]
        B_kd = [Buf("kd0"), Buf("kd1")]
        B_vpl = [Buf("vpl%d" % m) for m in range(4)]
        B_vpad = [Buf("vpad0"), Buf("vpad1")]
        B_sgr = [Buf("sgr%d" % c) for c in range(4)]
        B_sgrb = [Buf("sgrb%d" % c) for c in range(4)]
        B_sTm = [Buf("sTm0"), Buf("sTm1")]
        B_R, B_Rbf, B_kvt = Buf("R"), Buf("Rbf"), Buf("kvt")
        B_of, B_obf, B_vare, B_rstdL, B_zt = (Buf(n) for n in ("of", "obf", "vare", "rstdL", "zt"))
        B_cen2 = [Buf("cen0"), Buf("cen1")]
        B_csq2 = [Buf("csq0"), Buf("csq1")]
        B_mix = [Buf("mix%d" % c) for c in range(8)]
        B_cb16 = [Buf("cb16_%d" % c) for c in range(4)]
        B_out = [Buf("out%d" % m) for m in range(4)]

        ch_slot = [P.chan() for _ in range(NSLOT)]
        ch_store = [P.chan() for _ in range(NSLOT)]
        ch_x = [P.chan() for _ in range(4)]
        ch_o = [P.chan() for _ in range(4)]
        ch_p = P.chan()
        ch_tab = P.chan()
        ch_const = P.chan()

        state = {"bank": 0, "loaded": 0, "st": 0}

        def next_bank():
            i = state["bank"]
            state["bank"] = (i + 1) % 7
            return banks[i], B_bank[i]

        def next_st():
            i = state["st"]
            state["st"] = (i + 1) % 16
            return stats[:, 4 * i:4 * i + 4], B_st[i]

        total_pieces = ntiles * NPIECE

        def piece_len(k):
            if k == P_PLEW:
                return 2048
            if k in (P_DN1 + 2, P_DN1 + 5, P_DN2 + 2, P_DN2 + 5):
                return 6 * 512
            return PIECE

        def load_piece(g):
            t, k = divmod(g, NPIECE)
            s = g % NSLOT
            n = piece_len(k)
            if t == 0:
                P.dma("pool", wring[:, s, 0:n], wp_d[k, :, 0:n], [], [B_slot[s]], ch_slot[s])
                P.dma("sp", scr_d[k, :, 0:n], wring[:, s, 0:n], [B_slot[s]], [B_scr[k]], ch_store[s])
            else:
                P.dma("sp", wring[:, s, 0:n], scr_d[k, :, 0:n], [B_scr[k]], [B_slot[s]], ch_slot[s])

        def consume(t, k, hold=0):
            g = t * NPIECE + k
            while state["loaded"] < min(total_pieces, g + NSLOT - hold):
                load_piece(state["loaded"])
                state["loaded"] += 1
            s = g % NSLOT
            return wring[:, s, :], B_slot[s]

        P.dma("sp", cpar[:], cpar_d, [], [B_const], ch_const)
        P.dma("sp", cf[:], cf_d, [], [B_const], ch_const)
        P.dma("sp", cb[:], cb_d, [], [B_const], ch_const)
        P.op("pool", "memset", dict(ap=vpad[:, 0], constant=0.0), [], [B_vpad[0]])
        P.op("pool", "memset", dict(ap=vpad[:, 1], constant=0.0), [], [B_vpad[1]])

        def load_tile(t):
            r0 = t * T
            for m in range(4):
                P.dma("sp", h_t[:, m, :], x_d[r0 + m * 128:r0 + (m + 1) * 128, :], [], [B_h[m]], ch_x[m])
            P.dma("sp", ptile[:], p_d[r0:r0 + T, :].rearrange("(m q) c -> q m c", q=128), [], [B_p], ch_p)
            pos0 = (t % TILES_PER_SEQ) * T
            P.dma("sp", cosS[:], cos_d[:, pos0:pos0 + T], [], [B_tab], ch_tab)
            P.dma("sp", ssinS[:], ssin_d[:, pos0:pos0 + T], [], [B_tab], ch_tab)

        def rsqrt_dve(x_ap, B_x, y_ap, B_y, t_ap, B_t, iters=2):
            xi = x_ap.bitcast(I32)
            yi = y_ap.bitcast(I32)
            P.op("dve", "tensor_scalar", dict(out=yi, in0=xi, scalar1=1, scalar2=None, op0=ALU.arith_shift_right),
                 [B_x], [B_y])
            P.op("dve", "tensor_scalar", dict(out=yi, in0=yi, scalar1=-1, scalar2=0x5f3759df, op0=ALU.mult, op1=ALU.add),
                 [B_y], [B_y])
            for _ in range(iters):
                P.op("dve", "scalar_tensor_tensor", dict(out=t_ap, in0=y_ap, scalar=-0.5, in1=y_ap, op0=ALU.mult,
                                                         op1=ALU.mult), [B_y], [B_t])
                P.op("dve", "tensor_tensor", dict(out=t_ap, in0=t_ap, in1=x_ap, op=ALU.mult), [B_t, B_x], [B_t])
                P.op("dve", "scalar_tensor_tensor", dict(out=y_ap, in0=t_ap, scalar=1.5, in1=y_ap, op0=ALU.add,
                                                         op1=ALU.mult), [B_t, B_y], [B_y])

        def rstd_from(ms_ap, B_ms, n, factor=1.0):
            rs, B_rs = next_st()
            tt_, B_tt = next_st()
            rsqrt_dve(ms_ap, B_ms, rs[:, 0:n], B_rs, tt_[:, 0:n], B_tt, iters=2)
            if factor != 1.0:
                rs2, B_rs2 = next_st()
                P.op("dve", "tensor_scalar", dict(out=rs2[:, 0:n], in0=rs[:, 0:n], scalar1=float(factor),
                                                  scalar2=None, op0=ALU.mult), [B_rs], [B_rs2])
                return rs2, B_rs2
            return rs, B_rs

        def norm_to_xT(gidx):
            ms, B_ms = next_st()
            for m in range(4):
                P.op("act", "activation", dict(out=junk[:], in_=h_t[:, m, :], func=AF.Square, scale=1.0 / 32.0,
                                               accum_out=ms[:, m:m + 1]), [B_h[m]], [B_ms])
            me, B_me = next_st()
            P.op("dve", "tensor_scalar", dict(out=me[:, 0:4], in0=ms[:, 0:4], scalar1=EPS, scalar2=None,
                                              op0=ALU.add), [B_ms], [B_me])
            rs, B_rs = rstd_from(me[:, 0:4], B_me, 4)
            for m in range(4):
                if m % 2 == 0:
                    P.op("act", "activation", dict(out=xn[:, m, :], in_=h_t[:, m, :], func=AF.Copy, scale=rs[:, m:m + 1]),
                         [B_h[m], B_rs], [B_xn[m]])
                else:
                    P.op("dve", "tensor_scalar", dict(out=xn[:, m, :], in0=h_t[:, m, :], scalar1=rs[:, m:m + 1],
                                                      scalar2=None, op0=ALU.mult), [B_h[m], B_rs], [B_xn[m]])
            for dc in range(8):
                bk, B_bk = next_bank()
                bv = bk[:].bitcast(BF16)
                for m in range(4):
                    P.op("pe", "transpose", dict(out=bv[:, m * 128:(m + 1) * 128],
                                                 in_=xn[:, m, dc * 128:(dc + 1) * 128], identity=ident),
                         [B_xn[m], B_const], [B_bk])
                gcol = cpar[:, C_GPRE + 8 * gidx + dc:C_GPRE + 8 * gidx + dc + 1]
                if dc % 2 == 0:
                    P.op("act", "activation", dict(out=xT[:, dc, :], in_=bv[:, 0:512], func=AF.Copy, scale=gcol),
                         [B_bk, B_const], [B_xT])
                else:
                    P.op("dve", "tensor_scalar", dict(out=xT[:, dc, :], in0=bv[:, 0:512], scalar1=gcol,
                                                      scalar2=None, op0=ALU.mult), [B_bk, B_const], [B_xT])

        def aform(t, k, nchunk, col0, width=128):
            w, B_w = consume(t, k)
            wv = w.rearrange("p (c n) -> p c n", c=8)
            for ch in range(nchunk):
                bk, B_bk = next_bank()
                for dc in range(8):
                    P.op("pe", "matmul", dict(out=bk[:], lhsT=wv[:, dc, col0 + ch * width:col0 + (ch + 1) * width],
                                              rhs=xT[:, dc, :], start=(dc == 0), stop=(dc == 7)),
                         [B_w, B_xT], [B_bk])
                yield ch, bk, B_bk

        def post_update(gidx, factor, ss, B_ss, fb=None, B_fb=None, pre_scaled=False, out_fb=False):
            fb = fbuf if fb is None else fb
            B_fb = B_f if B_fb is None else B_fb
            me, B_me = next_st()
            ssv = ss.rearrange("p (m two) -> p m two", two=2)
            P.op("dve", "scalar_tensor_tensor", dict(out=me[:, 0:4], in0=ssv[:, :, 0], scalar=EPS, in1=ssv[:, :, 1],
                                                     op0=ALU.add, op1=ALU.add), [B_ss], [B_me])
            ck("post_a")
            rs, B_rs = rstd_from(me[:, 0:4], B_me, 4, factor)
            ck("post_b")
            gtab = cpar[:, C_GPOST + 1024 * gidx:C_GPOST + 1024 * (gidx + 1)]
            for m in range(4):
                if pre_scaled and out_fb:
                    P.op("dve", "scalar_tensor_tensor", dict(out=fb[:, m, :], in0=fb[:, m, :], scalar=rs[:, m:m + 1],
                                                             in1=h_t[:, m, :], op0=ALU.mult, op1=ALU.add),
                         [B_fb[m], B_rs, B_h[m]], [B_fb[m]])
                elif pre_scaled:
                    P.op("dve", "scalar_tensor_tensor", dict(out=h_t[:, m, :], in0=fb[:, m, :], scalar=rs[:, m:m + 1],
                                                             in1=h_t[:, m, :], op0=ALU.mult, op1=ALU.add),
                         [B_fb[m], B_rs, B_h[m]], [B_h[m]])
                else:
                    P.op("dve", "scalar_tensor_tensor", dict(out=fb[:, m, :], in0=fb[:, m, :], scalar=rs[:, m:m + 1],
                                                             in1=gtab, op0=ALU.mult, op1=ALU.mult),
                         [B_fb[m], B_rs, B_const], [B_fb[m]])
                    P.op("dve", "tensor_tensor", dict(out=h_t[:, m, :], in0=h_t[:, m, :], in1=fb[:, m, :], op=ALU.add),
                         [B_h[m], B_fb[m]], [B_h[m]])

        def bform_post(t, chunks, pieces, gidx, factor):
            nchunks = len(chunks)
            ss8 = stats[:, 56:64]
            B_ss = B_ss_f
            for dh in range(2):
                acc = [next_bank() for _ in range(4)]
                fc = 0
                for (k, n) in pieces[dh]:
                    w, B_w = consume(t, k)
                    for fcl in range(n):
                        ap, B_c = chunks[fc]
                        for m in range(4):
                            P.op("pe", "matmul", dict(out=acc[m][0][:], lhsT=ap[:, m * 128:(m + 1) * 128],
                                                      rhs=w[:, fcl * 512:(fcl + 1) * 512],
                                                      start=(fc == 0), stop=(fc == nchunks - 1)),
                                 [B_c, B_w], [acc[m][1]])
                        fc += 1
                gt_h = cpar[:, C_GPOST + 1024 * gidx + dh * 512:C_GPOST + 1024 * gidx + (dh + 1) * 512]
                for m in range(4):
                    bk, B_bk = acc[m]
                    fsl = fbuf[:, m, dh * 512:(dh + 1) * 512]
                    def _sq():
                        P.op("act", "activation", dict(out=junk[:, 0:512], in_=bk[:], func=AF.Square, scale=1.0 / 32.0,
                                                       accum_out=ss8[:, 2 * m + dh:2 * m + dh + 1]), [B_bk], [B_ss])

                    def _mul():
                        P.op("dve", "tensor_tensor", dict(out=fsl, in0=bk[:], in1=gt_h, op=ALU.mult),
                             [B_bk, B_const], [B_f[m]])
                    if m % 2 == 0:
                        _sq()
                        _mul()
                    else:
                        _mul()
                        _sq()
            post_update(gidx, factor, ss8, B_ss, pre_scaled=True)

        class _Stop(Exception):
            pass

        def ck(name):
            if debug == name:
                raise _Stop()

        def ffn(t, gidx, p_gu, p_dn):
            norm_to_xT(gidx)
            ck("norm%d" % gidx)
            for jj in range(11):
                w, B_w = consume(t, p_gu + jj)
                wv = w.rearrange("p (c n) -> p c n", c=8)
                for jl in range(2):
                    j = 2 * jj + jl
                    gb, B_gb = next_bank()
                    ub, B_ub = next_bank()
                    for (bk, B_bk, c0) in ((gb, B_gb, jl * 128), (ub, B_ub, 256 + jl * 128)):
                        for dc in range(8):
                            P.op("pe", "matmul", dict(out=bk[:], lhsT=wv[:, dc, c0:c0 + 128], rhs=xT[:, dc, :],
                                                      start=(dc == 0), stop=(dc == 7)), [B_w, B_xT], [B_bk])
                    P.op("act", "activation", dict(out=sgt[:, j % 2, :], in_=gb[:], func=AF.Silu),
                         [B_gb], [B_sg[j % 2]])
                    P.op("dve", "tensor_tensor", dict(out=hidT[:, j, :], in0=sgt[:, j % 2, :], in1=ub[:], op=ALU.mult),
                         [B_sg[j % 2], B_ub], [B_hid[j]])
            ck("gu%d" % gidx)
            chunks = [(hidT[:, j, :], B_hid[j]) for j in range(NFC)]
            pieces = [[(p_dn + dh * 3 + fb, min(8, NFC - 8 * fb)) for fb in range(3)] for dh in range(2)]
            bform_post(t, chunks, pieces, gidx, 0.5)

        def ln_feature_major(src_list, ones_ap, n_src, gb_cols, out_fn):
            raise NotImplementedError

        def mixer(t):
            first = (t % TILES_PER_SEQ == 0)
            norm_to_xT(1)
            if first:
                for c in range(4):
                    P.op("pool", "memset", dict(ap=cext[:, c, 0:30], constant=0.0), [], [B_cext[c]])
            else:
                for c in range(4):
                    P.op("pool", "tensor_copy", dict(out=cext[:, c, 0:30], in_=halo[:, c, :]),
                         [B_halo[c]], [B_cext[c]])
            for c, bk, B_bk in aform(t, P_WIN + 0, 4, 0):
                P.op("act", "activation", dict(out=cacc[:, c, :], in_=bk[:], func=AF.Copy, scale=0.5),
                     [B_bk], [B_cacc[c]])
            for c, bk, B_bk in aform(t, P_WIN + 1, 4, 0):
                P.op("act", "activation", dict(out=ctmp, in_=bk[:], func=AF.Tanh, scale=0.5), [B_bk], [B_ctmp])
                P.op("dve", "scalar_tensor_tensor", dict(out=cext[:, c, 30:542], in0=ctmp, scalar=1.0,
                                                         in1=cacc[:, c, :], op0=ALU.add, op1=ALU.mult),
                     [B_ctmp, B_cacc[c], B_cext[c]], [B_cext[c]])
            for c in range(4):
                P.op("pool", "tensor_copy", dict(out=halo[:, c, :], in_=cext[:, c, 512:542]), [B_cext[c]], [B_halo[c]])
            bgq = []

            def pump(n):
                for _ in range(min(n, len(bgq))):
                    bgq.pop(0)()

            def gen_dg(c, k0, b2):
                nk = min(8, KCONV - k0)
                wk = cpar[:, C_CONVW + c * KCONV + k0:C_CONVW + c * KCONV + k0 + nk]
                P.op("pool", "tensor_tensor", dict(out=dg[:, b2, 0:nk, :],
                                                   in0=ident.unsqueeze(1).to_broadcast([128, nk, 128]),
                                                   in1=wk.unsqueeze(2).to_broadcast([128, nk, 128]), op=ALU.mult),
                     [B_const], [B_dg[b2]])

            gen_dg(0, 0, 0)
            gen_dg(0, 8, 1)

            def conv_pe():
                di = 0
                for c in range(4):
                    bk, B_bk = next_bank()
                    for k0 in range(0, KCONV, 8):
                        nk = min(8, KCONV - k0)
                        b2 = di % 2
                        di += 1
                        if di > 2:
                            gen_dg(c, k0, b2)
                        for kk in range(nk):
                            k = k0 + kk
                            P.op("pe", "matmul", dict(out=bk[:], lhsT=dg[:, b2, kk, :], rhs=cext[:, c, k:k + 512],
                                                      start=(k == 0), stop=(k == KCONV - 1)),
                                 [B_dg[b2], B_cext[c]], [B_bk])
                    P.op("act", "activation", dict(out=cacc[:, c, :], in_=bk[:], func=AF.Identity,
                                                   bias=cpar[:, C_CONVB + c:C_CONVB + c + 1]),
                         [B_bk, B_const], [B_cacc[c]])

            ck("conv")
            for (pk, dstT, B_dst, scale, is_q) in ((P_WIN + 2, qT, B_qT, 0.125, True), (P_WIN + 3, kT, B_kT, 1.0, False)):
                if not is_q:
                    conv_pe()
                for c, bk, B_bk in aform(t, pk, 4, 0):
                    P.op("act", "activation", dict(out=qraw[:], in_=bk[:], func=AF.Copy, scale=scale), [B_bk], [B_qraw])
                    P.op("act", "activation", dict(out=t1[:], in_=bk[:], func=AF.Copy, scale=scale), [B_bk], [B_t1])
                    P.op("pool", "tensor_tensor", dict(out=t1[:], in0=t1[:], in1=cosS[:], op=ALU.mult),
                         [B_t1, B_tab], [B_t1])
                    sw, B_sw = next_bank()
                    P.op("pe", "matmul", dict(out=sw[:], lhsT=pm, rhs=qraw[:], start=True, stop=True),
                         [B_qraw, B_const], [B_sw])
                    P.op("dve", "tensor_tensor", dict(out=t2[:], in0=sw[:], in1=ssinS[:], op=ALU.mult),
                         [B_sw, B_tab], [B_t2])
                    P.op("pool", "tensor_tensor", dict(out=dstT[:, c, :], in0=t1[:], in1=t2[:], op=ALU.add),
                         [B_t1, B_t2], [B_dst[c]])
                    if is_q:
                        qdec_c = cf[:, F_QDEC + c * 128:F_QDEC + (c + 1) * 128]
                        P.op("pool", "tensor_tensor", dict(
                            out=qdT[:, c, :].rearrange("p (m i) -> p m i", m=4),
                            in0=qT[:, c, :].rearrange("p (m i) -> p m i", m=4),
                            in1=qdec_c.unsqueeze(1).to_broadcast([128, 4, 128]), op=ALU.mult),
                             [B_qT[c], B_const], [B_qdT[c]])
            ck("qk")
            w, B_w = consume(t, P_WIN + 4)
            wv = w.rearrange("p (c n) -> p c n", c=8)
            for m in range(4):
                bk, B_bk = next_bank()
                for dc in range(8):
                    P.op("pe", "matmul", dict(out=bk[:], lhsT=xT[:, dc, m * 128:(m + 1) * 128], rhs=wv[:, dc, :],
                                              start=(dc == 0), stop=(dc == 7)), [B_w, B_xT], [B_bk])
                P.op("act", "activation", dict(out=vpl[:, m, :], in_=bk[:], func=AF.Copy), [B_bk], [B_vpl[m]])
            for c, bk, B_bk in aform(t, P_WIN + 5, 4, 0):
                P.op("act", "activation", dict(out=sgr[:, c, :], in_=bk[:], func=AF.Silu), [B_bk], [B_sgr[c]])
                P.op("dve", "tensor_scalar", dict(out=sgrb[:, c, :], in0=sgr[:, c, :], scalar1=cpar[:, C_RGB + c:C_RGB + c + 1],
                                                  scalar2=None, op0=ALU.mult), [B_sgr[c], B_const], [B_sgrb[c]])
                P.op("dve", "tensor_scalar", dict(out=sgr[:, c, :], in0=sgr[:, c, :], scalar1=cpar[:, C_RGG + c:C_RGG + c + 1],
                                                  scalar2=None, op0=ALU.mult), [B_sgr[c], B_const], [B_sgr[c]])
            ck("vg")
            cl = {}

            def conv_ln1():
                for c in range(4):
                    P.op("act", "activation", dict(out=cb16[:, c, :], in_=cacc[:, c, :], func=AF.Copy),
                         [B_cacc[c]], [B_cb16[c]])
                mb, B_mb = banks[7], B_bank[7]
                cl["mb"] = (mb, B_mb)
                for c in range(4):
                    P.op("pe", "matmul", dict(out=mb[:], lhsT=o512, rhs=cb16[:, c, :], start=(c == 0), stop=(c == 3)),
                         [B_cb16[c], B_const], [B_mb])

            def conv_ln2():
                mb, B_mb = cl["mb"]
                for c in range(4):
                    P.op("dve", "tensor_tensor", dict(out=cacc[:, c, :], in0=cacc[:, c, :], in1=mb[:], op=ALU.subtract),
                         [B_cacc[c], B_mb], [B_cacc[c]])
                    P.op("act", "activation", dict(out=cb16[:, c, :], in_=cacc[:, c, :], func=AF.Square),
                         [B_cacc[c]], [B_cb16[c]])
                vb, B_vb = banks[7], B_bank[7]
                cl["vb"] = (vb, B_vb)
                for c in range(4):
                    P.op("pe", "matmul", dict(out=vb[:], lhsT=o512, rhs=cb16[:, c, :], start=(c == 0), stop=(c == 3)),
                         [B_cb16[c], B_const], [B_vb])

            def conv_ln3():
                vb, B_vb = cl["vb"]
                P.op("act", "activation", dict(out=ctmp, in_=vb[:], func=AF.Sqrt, bias=EPS), [B_vb], [B_ctmp])
                P.op("dve", "reciprocal", dict(out=ctmp, in_=ctmp), [B_ctmp], [B_ctmp])
                for c in range(4):
                    P.op("pool" if c % 2 else "dve", "tensor_tensor",
                         dict(out=cacc[:, c, :], in0=cacc[:, c, :], in1=ctmp, op=ALU.mult),
                         [B_cacc[c], B_ctmp], [B_cacc[c]])
                    P.op("act", "activation", dict(out=mixT[:, 4 + c, :], in_=cacc[:, c, :], func=AF.Silu,
                                                   scale=cpar[:, C_CLNG + c:C_CLNG + c + 1],
                                                   bias=cpar[:, C_CLNB + c:C_CLNB + c + 1]),
                         [B_cacc[c], B_const], [B_mix[4 + c]])

            ck("convln")
            if first:
                P.op("pool", "memset", dict(ap=R_t[:], constant=0.0), [], [B_R])
                P.op("pool", "memset", dict(ap=Rbf[:], constant=0.0), [], [B_Rbf])
            def A1(m):
                cs = slice(m * 128, (m + 1) * 128)
                i2 = m % 2
                kb, B_kb = next_bank()
                kbv = kb[:].bitcast(BF16)
                for pp in range(4):
                    P.op("pe", "transpose", dict(out=kbv[:, pp * 128:(pp + 1) * 128], in_=kT[:, pp, cs], identity=ident),
                         [B_kT[pp], B_const], [B_kb])
                P.op("dve", "tensor_tensor", dict(
                    out=kd[:, i2, :].rearrange("p (h d) -> p h d", h=8),
                    in0=kbv[:, 0:512].rearrange("p (h d) -> p h d", h=8),
                    in1=cf[:, F_KDEC:F_KDEC + 8].unsqueeze(2).to_broadcast([128, 8, 64]), op=ALU.mult),
                     [B_kb, B_const], [B_kd[i2]])
                vsrc = vpl[:, m, :].rearrange("p (a b e) -> p a b e", a=4, b=2)
                vdst = vpad[:, i2].rearrange("p (a b) e -> p a b e", a=4)
                P.op("pool", "tensor_copy", dict(out=vdst[:, :, 0, 0:64], in_=vsrc[:, :, 0, :]), [B_vpl[m]], [B_vpad[i2]])
                P.op("pool", "tensor_copy", dict(out=vdst[:, :, 1, 64:128], in_=vsrc[:, :, 1, :]), [B_vpl[m]], [B_vpad[i2]])
                sa, B_sa = next_bank()
                sbk, B_sbk = next_bank()
                for pp in range(4):
                    for hh, (bk, B_bk) in enumerate(((sa, B_sa), (sbk, B_sbk))):
                        rs_ = slice(hh * 64, (hh + 1) * 64)
                        P.op("pe", "matmul", dict(out=bk[:, pp * 128:(pp + 1) * 128], lhsT=kT[rs_, pp, cs],
                                                  rhs=qT[rs_, pp, cs], start=True, stop=True),
                             [B_kT[pp], B_qT[pp]], [B_bk])
                for hh, (bk, B_bk) in enumerate(((sa, B_sa), (sbk, B_sbk))):
                    P.op("dve", "tensor_tensor", dict(
                        out=sTm[:, i2, hh * 4:(hh + 1) * 4, :].rearrange("p a i -> p (a i)"),
                        in0=bk[:], in1=cf[:, F_MASK + hh * 512:F_MASK + (hh + 1) * 512], op=ALU.mult),
                         [B_bk, B_const], [B_sTm[i2]])

            obanks = {}

            def A2(m):
                cs = slice(m * 128, (m + 1) * 128)
                i2 = m % 2
                ob, B_ob = next_bank()
                obanks[m] = (ob, B_ob)
                for pp in range(4):
                    osl = ob[:, pp * 128:(pp + 1) * 128]
                    P.op("pe", "matmul", dict(out=osl, lhsT=vpad[:, i2, 2 * pp, :], rhs=sTm[:, i2, pp, :],
                                              start=True, stop=False), [B_vpad[i2], B_sTm[i2]], [B_ob])
                    P.op("pe", "matmul", dict(out=osl, lhsT=vpad[:, i2, 2 * pp + 1, :], rhs=sTm[:, i2, 4 + pp, :],
                                              start=False, stop=False), [B_vpad[i2], B_sTm[i2]], [B_ob])
                    P.op("pe", "matmul", dict(out=osl, lhsT=Rbf[:, pp * 128:(pp + 1) * 128], rhs=qdT[:, pp, cs],
                                              start=False, stop=True), [B_Rbf, B_qdT[pp]], [B_ob])
                kvb, B_kvb = next_bank()
                for pp in range(4):
                    P.op("pe", "matmul", dict(out=kvb[:, pp * 128:(pp + 1) * 128], lhsT=kd[:, i2, pp * 128:(pp + 1) * 128],
                                              rhs=vpl[:, m, pp * 128:(pp + 1) * 128], start=True, stop=True),
                         [B_kd[i2], B_vpl[m]], [B_kvb])
                P.op("dve", "tensor_tensor", dict(
                    out=kvt[:].rearrange("p (a e) -> p a e", a=4), in0=kvb[:].rearrange("p (a e) -> p a e", a=4),
                    in1=cf[:, F_BDM:F_BDM + 128].unsqueeze(1).to_broadcast([128, 4, 128]), op=ALU.mult),
                     [B_kvb, B_const], [B_kvt])
                P.op("pool", "tensor_tensor", dict(
                    out=R_t[:].rearrange("p (a e) -> p a e", a=4), in0=R_t[:].rearrange("p (a e) -> p a e", a=4),
                    in1=cf[:, F_CDEC:F_CDEC + 4].unsqueeze(2).to_broadcast([128, 4, 128]), op=ALU.mult),
                     [B_R, B_const], [B_R])
                P.op("pool", "tensor_tensor", dict(out=R_t[:], in0=R_t[:], in1=kvt[:], op=ALU.add), [B_R, B_kvt], [B_R])
                P.op("act", "activation", dict(out=Rbf[:], in_=R_t[:], func=AF.Copy), [B_R], [B_Rbf])

            def B1(m):
                cen, B_cen, csq, B_csq = cen2[:, m % 2, :], B_cen2[m % 2], csq2[:, m % 2, :], B_csq2[m % 2]
                ob, B_ob = obanks[m]
                P.op("act", "activation", dict(out=o_f[:], in_=ob[:], func=AF.Copy), [B_ob], [B_of])
                P.op("act", "activation", dict(out=o_bf[:], in_=ob[:], func=AF.Copy), [B_ob], [B_obf])
                mb, B_mb = next_bank()
                P.op("pe", "matmul", dict(out=mb[:], lhsT=bd64, rhs=o_bf[:], start=True, stop=True),
                     [B_obf, B_const], [B_mb])
                P.op("dve", "tensor_tensor", dict(out=cen[:], in0=o_f[:], in1=mb[:], op=ALU.subtract),
                     [B_of, B_mb], [B_cen])
                P.op("act", "activation", dict(out=csq[:], in_=cen[:], func=AF.Square), [B_cen], [B_csq])

            def B2(m):
                cen, B_cen, csq, B_csq = cen2[:, m % 2, :], B_cen2[m % 2], csq2[:, m % 2, :], B_csq2[m % 2]
                cs = slice(m * 128, (m + 1) * 128)
                vb, B_vb = next_bank()
                P.op("pe", "matmul", dict(out=vb[:], lhsT=bd64, rhs=csq[:], start=True, stop=True),
                     [B_csq, B_const], [B_vb])
                P.op("act", "activation", dict(out=vare[:], in_=vb[:], func=AF.Sqrt, bias=EPS), [B_vb], [B_vare])
                P.op("dve", "reciprocal", dict(out=rstdL[:], in_=vare[:]), [B_vare], [B_rstdL])
                P.op("dve", "tensor_tensor", dict(out=cen[:], in0=cen[:], in1=rstdL[:], op=ALU.mult),
                     [B_cen, B_rstdL], [B_cen])
                P.op("pool", "tensor_tensor", dict(out=zt[:].rearrange("p (a i) -> p a i", a=4),
                                                   in0=cen[:].rearrange("p (a i) -> p a i", a=4),
                                                   in1=sgr[:, :, cs], op=ALU.mult), [B_cen] + B_sgr, [B_zt])
                P.op("pool", "tensor_tensor", dict(out=mixT[:, 0:4, cs], in0=zt[:].rearrange("p (a i) -> p a i", a=4),
                                                   in1=sgrb[:, :, cs], op=ALU.add), [B_zt] + B_sgrb, B_mix[0:4])

            conv_ln1()
            A1(0)
            A2(0)
            B1(0)
            for m in range(1, 4):
                A1(m)
                A2(m)
                B1(m)
                B2(m - 1)
                if m == 1:
                    conv_ln2()
            B2(3)
            conv_ln3()
            pump(len(bgq))
            ck("ret")
            chunks = [(mixT[:, c, :], B_mix[c]) for c in range(8)]
            bform_post(t, chunks, [[(P_WOUT + 0, 8)], [(P_WOUT + 1, 8)]], 1, 1.0)

        def ple(t):
            norm_to_xT(3)
            P.op("pool", "tensor_copy", dict(out=p_bf[:], in_=ptile[:]), [B_p], [B_pbf])
            bk, B_bk = next_bank()
            bv = bk[:].bitcast(BF16)
            for dc in range(2):
                for m in range(4):
                    P.op("pe", "transpose", dict(out=bv[:, dc * 512 + m * 128:dc * 512 + (m + 1) * 128],
                                                 in_=p_bf[:, m, dc * 128:(dc + 1) * 128], identity=ident),
                         [B_pbf, B_const], [B_bk])
            P.op("act", "activation", dict(out=pT[:].rearrange("p c t -> p (c t)"), in_=bv[:, 0:1024], func=AF.Copy),
                 [B_bk], [B_pT])
            we, B_we = consume(t, P_PLEW)
            wev = we[:, 0:2048].rearrange("p (c n) -> p c n", c=2)
            ss8 = stats[:, 48:56]
            B_ss = B_ss_p
            for dh in range(2):
                wg, B_wg = consume(t, P_GATE + dh, hold=1 + dh)
                wgv = wg.rearrange("p (c n) -> p c n", c=8)
                for m in range(4):
                    ms_ = slice(m * 128, (m + 1) * 128)
                    gbk, B_gbk = next_bank()
                    ebk, B_ebk = next_bank()
                    for dc in range(8):
                        P.op("pe", "matmul", dict(out=gbk[:], lhsT=xT[:, dc, ms_], rhs=wgv[:, dc, :],
                                                  start=(dc == 0), stop=(dc == 7)), [B_xT, B_wg], [B_gbk])
                    for dc in range(2):
                        P.op("pe", "matmul", dict(out=ebk[:], lhsT=pT[:, dc, ms_], rhs=wev[:, dc, dh * 512:(dh + 1) * 512],
                                                  start=(dc == 0), stop=(dc == 1)), [B_pT, B_we], [B_ebk])
                    P.op("act", "activation", dict(out=sgt[:, m % 2, :], in_=gbk[:], func=AF.Tanh, scale=0.5),
                         [B_gbk], [B_sg[m % 2]])
                    fsl = fple[:, m, dh * 512:(dh + 1) * 512]
                    P.op("dve", "scalar_tensor_tensor", dict(out=fsl, in0=sgt[:, m % 2, :], scalar=1.0, in1=ebk[:],
                                                             op0=ALU.add, op1=ALU.mult), [B_sg[m % 2], B_ebk], [B_fp[m]])
                    P.op("act", "activation", dict(out=junk[:, 0:512], in_=fsl, func=AF.Square, scale=1.0 / 64.0,
                                                   accum_out=ss8[:, 2 * m + dh:2 * m + dh + 1]), [B_fp[m]], [B_ss])
                    P.op("dve", "tensor_tensor", dict(
                        out=fsl, in0=fsl, in1=cpar[:, C_GPOST + 3 * 1024 + dh * 512:C_GPOST + 3 * 1024 + (dh + 1) * 512],
                        op=ALU.mult), [B_fp[m], B_const], [B_fp[m]])
            post_update(3, 0.5, ss8, B_ss, fple, B_fp, pre_scaled=True, out_fb=True)
            state["final_in_fple"] = True

        def store_tile(t):
            r0 = t * T
            for m in range(4):
                if state.get("final_in_fple"):
                    P.dma("sp", out_d[r0 + m * 128:r0 + (m + 1) * 128, :], fple[:, m, :], [B_fp[m]], [B_out[m]], ch_o[m])
                else:
                    P.dma("sp", out_d[r0 + m * 128:r0 + (m + 1) * 128, :], h_t[:, m, :], [B_h[m]], [B_out[m]], ch_o[m])
            state["final_in_fple"] = False

        for t in range(ntiles):
            load_tile(t)
            try:
                ck("load")
                ffn(t, 0, P_GU1, P_DN1)
                ck("ffn1")
                mixer(t)
                ck("mixer")
                ffn(t, 2, P_GU2, P_DN2)
                ck("ffn2")
                ple(t)
            except _Stop:
                pass
            store_tile(t)
        P.wait_all("sp", B_out + B_scr)
        nwait = P.emit(stack)
        build_program.info = dict(nops=len(P.ops), nwait=nwait, per_eng=dict(P.nseq))
    return nc


_CACHE = {}


def _host_inputs(inp):
    cf, cosT, ssinT, cb = _const_tables()
    wp = _layout_weights(inp)
    cp = _layout_params(inp)
    x = np.ascontiguousarray(np.asarray(inp["x"], np.float32)).reshape(NCORES, TOK_CORE, D)
    p = np.ascontiguousarray(np.asarray(inp["p"], np.float32)[0]).reshape(NCORES, TOK_CORE, 256)
    maps = []
    for c in range(NCORES):
        maps.append(dict(x=x[c], p=p[c], wp=wp, cpar=cp, cf=cf, cosT=cosT, ssinT=ssinT, cb=cb))
    return maps


def kernel(**inputs):
    inp = {k: np.asarray(v) for k, v in inputs.items()}
    if "nc" not in _CACHE:
        _CACHE["nc"] = build_program(NTILES)
    nc = _CACHE["nc"]
    maps = _host_inputs(inp)
    res = run_bass_kernel_spmd(nc, maps, core_ids=list(range(NCORES)))
    out = np.stack([np.asarray(r["out"], np.float32) for r in res.results], axis=0)
    return out.reshape(BATCH, SEQ, D)
```

```python
import contextlib
import numpy as np
import ml_dtypes
import concourse.bass as bass
import concourse.mybir as mybir
from concourse.bass_utils import run_bass_kernel_spmd

F32 = mybir.dt.float32
BF16 = mybir.dt.bfloat16
I32 = mybir.dt.int32
AF = mybir.ActivationFunctionType
ALU = mybir.AluOpType

D = 1024
DFF = 2816
NFC = 22
SEQ = 4096
BATCH = 16
NCORES = 8
TOK_CORE = BATCH * SEQ // NCORES
T = 512
NTILES = TOK_CORE // T
TILES_PER_SEQ = SEQ // T
EPS = 1e-6
NSLOT = 4
PIECE = 4096
NPIECE = 45
KCONV = 31

P_GU1 = 0
P_DN1 = 11
P_WIN = 17
P_WOUT = 23
P_GU2 = 25
P_DN2 = 36
P_PLEW = 42
P_GATE = 43
WIN_ORDER = [4, 5, 0, 1, 2, 3]

C_GPRE = 0
C_GPOST = 32
C_CONVW = C_GPOST + 4096
C_CONVB = C_CONVW + 124
C_CLNG = C_CONVB + 4
C_CLNB = C_CLNG + 4
C_RGG = C_CLNB + 4
C_RGB = C_RGG + 4
NCPAR = C_RGB + 4

F_QDEC = 0
F_MASK = 512
F_BDM = 1536
F_KDEC = 1664
F_CDEC = 1672
F_MHALF = 1676
NCF = 1680


class Buf:
    __slots__ = ("name", "w", "r", "over", "excl")

    def __init__(self, name, excl=False):
        self.name = name
        self.w = {}
        self.r = {}
        self.over = []
        self.excl = excl


def overlap(a_list, b_list):
    for a in a_list:
        for b in b_list:
            a.over.append(b)
            b.over.append(a)


class Prog:
    ENG = ("pe", "act", "dve", "pool", "sp")

    def __init__(self, nc):
        self.nc = nc
        self.engs = {"pe": nc.tensor, "act": nc.scalar, "dve": nc.vector, "pool": nc.gpsimd, "sp": nc.sync}
        self.ops = []
        self.nseq = {e: 0 for e in self.ENG}
        self.seen = {e: {} for e in self.ENG}
        self.nchan = 0
        self.chan_count = []

    def chan(self):
        self.chan_count.append(0)
        self.nchan += 1
        return self.nchan - 1

    def _deps(self, eng, reads, writes):
        deps = {}
        seen = self.seen[eng]

        def need(ev, hazard):
            kind, key, val = ev
            if kind == "c" and key == eng:
                if eng == "pe" or hazard == "WAR":
                    return
            k = (kind, key)
            if seen.get(k, 0) >= val:
                return
            if deps.get(k, 0) < val:
                deps[k] = val

        for b in reads:
            for ev in b.w.values():
                need(ev, "RAW")
            if b.excl:
                for (kk, key), ev in b.r.items():
                    if not (kk == "c" and key == eng):
                        need(ev, "RAR")
        for b in writes:
            for bb in [b] + b.over:
                for ev in bb.w.values():
                    need(ev, "WAW")
                for ev in bb.r.values():
                    need(ev, "WAR")
        for k, v in deps.items():
            seen[k] = v
        return deps

    def op(self, eng, name, kw, reads=(), writes=()):
        deps = self._deps(eng, reads, writes)
        self.nseq[eng] += 1
        seq = self.nseq[eng]
        ev = ("c", eng, seq)
        for b in reads:
            b.r[("c", eng)] = ev
        for b in writes:
            b.w = {("c", eng): ev}
            b.r = {}
        self.ops.append((eng, name, kw, deps, seq, None))

    def dma(self, q, out, in_, reads, writes, chan):
        deps = self._deps(q, reads, writes)
        self.nseq[q] += 1
        seq = self.nseq[q]
        self.chan_count[chan] += 16
        ev = ("d", chan, self.chan_count[chan])
        for b in reads:
            b.r[("d", chan)] = ev
        for b in writes:
            b.w = {("d", chan): ev}
            b.r = {}
        self.ops.append((q, "dma_start", dict(out=out, in_=in_), deps, seq, chan))

    def wait_all(self, eng, bufs):
        deps = self._deps(eng, bufs, bufs)
        self.nseq[eng] += 1
        self.ops.append((eng, None, None, deps, self.nseq[eng], None))

    def emit(self, stack):
        nc = self.nc
        esem = {e: stack.enter_context(nc.semaphore("s_" + e)) for e in self.ENG}
        csem = [stack.enter_context(nc.semaphore("c_%d" % i)) for i in range(self.nchan)]
        needed = {e: set() for e in self.ENG}
        for (_, _, _, deps, _, _) in self.ops:
            for (kind, key), val in deps.items():
                if kind == "c":
                    needed[key].add(val)
        cnt = {}
        for e in self.ENG:
            cnt[e] = {s: i + 1 for i, s in enumerate(sorted(needed[e]))}
        nwait = 0
        for (eng, name, kw, deps, seq, chan) in self.ops:
            E = self.engs[eng]
            for (kind, key), val in deps.items():
                if kind == "c":
                    E.wait_ge(esem[key], cnt[key][val])
                else:
                    E.wait_ge(csem[key], val)
                nwait += 1
            if name is None:
                continue
            ins = getattr(E, name)(**kw)
            if chan is not None:
                ins.then_inc(csem[chan], 16)
            elif seq in cnt[eng]:
                ins.then_inc(esem[eng], 1)
        return nwait


def _const_tables():
    lg = np.log1p(-np.exp2(-5.0 - np.arange(8, dtype=np.float32))).astype(np.float32)
    pos = np.arange(128, dtype=np.float32)
    diff = pos[None, :] - pos[:, None]
    mask = np.where(diff[:, None, :] >= 0,
                    np.exp(lg[None, :, None] * np.maximum(diff, 0.0)[:, None, :]), 0.0).astype(np.float32)
    mask = mask[:, [0, 2, 4, 6, 1, 3, 5, 7], :]
    kdec = np.exp(lg[None, :] * (127.0 - pos)[:, None]).astype(np.float32)
    qdec_h = np.exp(lg[:, None] * (pos + 1.0)[None, :]).astype(np.float32)
    cdec_h = np.exp(lg * 128.0).astype(np.float32)
    qdec = np.zeros((128, 4, 128), np.float32)
    cdec = np.zeros((128, 4), np.float32)
    for p in range(4):
        for hh in range(2):
            qdec[hh * 64:(hh + 1) * 64, p, :] = qdec_h[2 * p + hh][None, :]
            cdec[hh * 64:(hh + 1) * 64, p] = cdec_h[2 * p + hh]
    bdm = np.zeros((128, 128), np.float32)
    bdm[:64, :64] = 1.0
    bdm[64:, 64:] = 1.0
    cf = np.zeros((128, NCF), np.float32)
    cf[:, F_QDEC:F_QDEC + 512] = qdec.reshape(128, 512)
    cf[:, F_MASK:F_MASK + 1024] = mask.reshape(128, 1024)
    cf[:, F_BDM:F_BDM + 128] = bdm
    cf[:, F_KDEC:F_KDEC + 8] = kdec
    cf[:, F_CDEC:F_CDEC + 4] = cdec
    cf[:, F_MHALF] = -0.5
    inv_freq = (10000.0 ** (-np.arange(0, 64, 2, dtype=np.float32) / 64.0)).astype(np.float32)
    ang = (np.arange(SEQ, dtype=np.float32)[:, None] * inv_freq[None, :]).astype(np.float32)
    cos = np.cos(ang).astype(np.float32)
    sin = np.sin(ang).astype(np.float32)
    cos64 = np.concatenate([cos, cos], axis=1).T
    ssin64 = np.concatenate([-sin, sin], axis=1).T
    cosT = np.ascontiguousarray(np.concatenate([cos64, cos64], axis=0))
    ssinT = np.ascontiguousarray(np.concatenate([ssin64, ssin64], axis=0))
    ident = np.eye(128, dtype=np.float32)
    pm = np.zeros((128, 128), np.float32)
    for f in range(128):
        pm[f ^ 32, f] = 1.0
    bd64 = bdm / 64.0
    o512 = np.full((128, 128), 1.0 / 512.0, np.float32)
    cb = np.concatenate([ident, pm, bd64, o512], axis=1).astype(ml_dtypes.bfloat16)
    return cf, cosT, ssinT, cb


def _layout_weights(inp):
    wp = np.zeros((NPIECE, 128, PIECE), np.float32)

    def rows(w):
        return w.reshape(-1, 128, w.shape[1]).transpose(1, 0, 2)

    for base_gu, base_dn, wgu, wdn in ((P_GU1, P_DN1, inp["ffn1_w_gu"][0], inp["ffn1_w_down"][0]),
                                        (P_GU2, P_DN2, inp["ffn2_w_gu"][0], inp["ffn2_w_down"][0])):
        r = rows(wgu)
        for jj in range(11):
            pc = wp[base_gu + jj].reshape(128, 8, 512)
            pc[:, :, 0:256] = r[:, :, jj * 256:(jj + 1) * 256]
            pc[:, :, 256:512] = r[:, :, DFF + jj * 256:DFF + (jj + 1) * 256]
        r = rows(wdn)
        for dh in range(2):
            for fb in range(3):
                n = min(8, NFC - fb * 8)
                pc = wp[base_dn + dh * 3 + fb].reshape(128, 8, 512)
                pc[:, 0:n, :] = r[:, fb * 8:fb * 8 + n, dh * 512:(dh + 1) * 512]
    r = rows(inp["w_in"][0])
    for i, cbk in enumerate(WIN_ORDER):
        wp[P_WIN + i].reshape(128, 8, 512)[:] = r[:, :, cbk * 512:(cbk + 1) * 512]
    r = rows(inp["w_out"][0])
    for dh in range(2):
        wp[P_WOUT + dh].reshape(128, 8, 512)[:] = r[:, :, dh * 512:(dh + 1) * 512]
    r = rows(inp["ple_w"][0])
    wp[P_PLEW][:, 0:2048] = r.reshape(128, 2048)
    r = rows(inp["ple_gate_w"][0])
    for dh in range(2):
        wp[P_GATE + dh].reshape(128, 8, 512)[:] = r[:, :, dh * 512:(dh + 1) * 512]
    return wp


def _layout_params(inp):
    cp = np.zeros((128, NCPAR), np.float32)

    def col(v, n):
        return v.reshape(n, 128).T

    for i, k in enumerate(("ffn1_pre_g", "mix_pre_g", "ffn2_pre_g", "ple_gate_norm_g")):
        cp[:, C_GPRE + 8 * i:C_GPRE + 8 * (i + 1)] = col(inp[k][0], 8)
    for i, k in enumerate(("ffn1_post_g", "mix_post_g", "ffn2_post_g", "ple_post_g")):
        cp[:, C_GPOST + 1024 * i:C_GPOST + 1024 * (i + 1)] = np.broadcast_to(inp[k][0][None, :], (128, 1024))
    cw = inp["conv_w"][0]
    cp[:, C_CONVW:C_CONVW + 124] = cw.T.reshape(4, 128, KCONV).transpose(1, 0, 2).reshape(128, 124)
    cp[:, C_CONVB:C_CONVB + 4] = col(inp["conv_b"][0], 4)
    cp[:, C_CLNG:C_CLNG + 4] = col(inp["conv_ln_g"][0], 4)
    cp[:, C_CLNB:C_CLNB + 4] = col(inp["conv_ln_b"][0], 4)
    cp[:, C_RGG:C_RGG + 4] = col(inp["ret_gn_g"][0], 4)
    cp[:, C_RGB:C_RGB + 4] = col(inp["ret_gn_b"][0], 4)
    return cp


def build_program(ntiles=NTILES, debug=None):
    nc = bass.Bass("TRN2", target_bir_lowering=False)
    x_d = nc.dram_tensor("x", [TOK_CORE, D], F32, kind="ExternalInput").ap()
    p_d = nc.dram_tensor("p", [TOK_CORE, 256], F32, kind="ExternalInput").ap()
    wp_d = nc.dram_tensor("wp", [NPIECE, 128, PIECE], F32, kind="ExternalInput").ap()
    cpar_d = nc.dram_tensor("cpar", [128, NCPAR], F32, kind="ExternalInput").ap()
    cf_d = nc.dram_tensor("cf", [128, NCF], F32, kind="ExternalInput").ap()
    cos_d = nc.dram_tensor("cosT", [128, SEQ], F32, kind="ExternalInput").ap()
    ssin_d = nc.dram_tensor("ssinT", [128, SEQ], F32, kind="ExternalInput").ap()
    cb_d = nc.dram_tensor("cb", [128, 512], BF16, kind="ExternalInput").ap()
    out_d = nc.dram_tensor("out", [TOK_CORE, D], F32, kind="ExternalOutput").ap()
    scr_d = nc.dram_tensor("wscr", [NPIECE, 128, PIECE], BF16, kind="Internal").ap()

    stack = contextlib.ExitStack()
    with stack:
        def sb(name, shape, dt):
            return stack.enter_context(nc.sbuf_tensor(name, shape, dt))

        P = Prog(nc)

        h_t = sb("h", [128, 4, D], F32)
        arenaA = sb("arenaA", [128, 4096], F32)
        fbuf = arenaA[:].rearrange("p (m d) -> p m d", m=4)
        xn = arenaA[:, 0:2048].bitcast(BF16).rearrange("p (m d) -> p m d", m=4)
        xT = arenaA[:, 2048:4096].bitcast(BF16).rearrange("p (c t) -> p c t", c=8)
        arenaB = sb("arenaB", [128, NFC * 512], BF16)
        hidT = arenaB[:].rearrange("p (j t) -> p j t", j=NFC)
        cext = arenaB[:, 0:2168].rearrange("p (c t) -> p c t", c=4)
        cacc = arenaB[:, 4336:8432].bitcast(F32).rearrange("p (c t) -> p c t", c=4)
        ctmp = arenaB[:, 8432:9456].bitcast(F32)
        wring = sb("wring", [128, NSLOT, PIECE], BF16)
        ptile = sb("ptile", [128, 4, 256], F32)
        p_bf = sb("p_bf", [128, 4, 256], BF16)
        pT = sb("pT", [128, 2, 512], BF16)
        cpar = sb("cpar_s", [128, NCPAR], F32)
        cf = sb("cf_s", [128, NCF], F32)
        cb = sb("cb_s", [128, 512], BF16)
        cosS = sb("cosS", [128, 512], F32)
        ssinS = sb("ssinS", [128, 512], F32)
        sgt = sb("sgt", [128, 2, 512], F32)
        junk = sb("junk", [128, 1024], BF16)
        stats = sb("stats", [128, 64], F32)
        qraw = sb("qraw", [128, 512], BF16)
        t1 = sb("t1", [128, 512], F32)
        t2 = sb("t2", [128, 512], F32)
        qT = sb("qT", [128, 4, 512], BF16)
        qdT = sb("qdT", [128, 4, 512], BF16)
        kT = sb("kT", [128, 4, 512], BF16)
        kd = sb("kd", [128, 2, 512], BF16)
        vpl = sb("vpl", [128, 4, 512], BF16)
        vpad = sb("vpad", [128, 2, 8, 128], BF16)
        sgr = sb("sgr", [128, 4, 512], BF16)
        sgrb = sb("sgrb", [128, 4, 512], BF16)
        sTm = sb("sTm", [128, 2, 8, 128], BF16)
        R_t = sb("R", [128, 512], F32)
        Rbf = sb("Rbf", [128, 512], BF16)
        kvt = sb("kvt", [128, 512], F32)
        o_f = sb("o_f", [128, 512], F32)
        o_bf = sb("o_bf", [128, 512], BF16)
        cen2 = sb("cen", [128, 2, 512], F32)
        csq2 = sb("csq", [128, 2, 512], BF16)
        vare = sb("vare", [128, 512], F32)
        rstdL = sb("rstdL", [128, 512], F32)
        zt = sb("zt", [128, 512], F32)
        mixT = sb("mixT", [128, 8, 512], BF16)
        cb16 = sb("cb16", [128, 4, 512], BF16)
        halo = sb("halo", [128, 4, 30], BF16)
        dg = sb("dg", [128, 2, 8, 128], BF16)
        fple = arenaB[:, 0:8192].bitcast(F32).rearrange("p (m d) -> p m d", m=4)
        banks = [stack.enter_context(nc.psum_tensor("bank%d" % i, [128, 512], F32)) for i in range(8)]

        ident = cb[:, 0:128]
        pm = cb[:, 128:256]
        bd64 = cb[:, 256:384]
        o512 = cb[:, 384:512]
        mhalf = cf[:, F_MHALF:F_MHALF + 1]

        B_h = [Buf("h%d" % m) for m in range(4)]
        B_xn = [Buf("xn%d" % m) for m in range(4)]
        B_xT = [Buf("xT%d" % c) for c in range(8)]
        B_f = [Buf("f%d" % m) for m in range(4)]
        overlap(B_f, B_xn + B_xT)
        B_hid = [Buf("hid%d" % j) for j in range(NFC)]
        B_cext = [Buf("cext%d" % c) for c in range(4)]
        B_cacc = [Buf("cacc%d" % c) for c in range(4)]
        B_ctmp = Buf("ctmp")
        overlap(B_hid, B_cext + B_cacc + [B_ctmp])
        B_fp = [Buf("fp%d" % m) for m in range(4)]
        overlap(B_fp, B_hid + B_cext + B_cacc + [B_ctmp])
        B_halo = [Buf("halo%d" % c) for c in range(4)]
        B_dg = [Buf("dg0"), Buf("dg1")]
        B_ss_f, B_ss_p = Buf("ss8f"), Buf("ss8p")
        B_slot = [Buf("slot%d" % s) for s in range(NSLOT)]
        B_scr = [Buf("scr%d" % k) for k in range(NPIECE)]
        B_bank = [Buf("bank%d" % i, excl=True) for i in range(8)]
        B_const = Buf("const")
        B_tab = Buf("tab")
        B_p = Buf("ptile")
        B_pbf = Buf("pbf")
        B_pT = Buf("pT")
        B_sg = [Buf("sg0"), Buf("sg1")]
        B_st = [Buf("st%d" % i) for i in range(16)]
        B_qraw, B_t1, B_t2 = Buf("qraw"), Buf("t1"), Buf("t2")
        B_qT = [Buf("qT%d" % c) for c in range(4)]
        B_qdT = [Buf("qdT%d" % c) for c in range(4)]
        B_kT = [Buf("kT%d" % c) for c in range(4)]
        B_kd = [Buf("kd0"), Buf("kd1")]
        B_vpl = [Buf("vpl%d" % m) for m in range(4)]
        B_vpad = [Buf("vpad0"), Buf("vpad1")]
        B_sgr = [Buf("sgr%d" % c) for c in range(4)]
        B_sgrb = [Buf("sgrb%d" % c) for c in range(4)]
        B_sTm = [Buf("sTm0"), Buf("sTm1")]
        B_R, B_Rbf, B_kvt = Buf("R"), Buf("Rbf"), Buf("kvt")
        B_of, B_obf, B_vare, B_rstdL, B_zt = (Buf(n) for n in ("of", "obf", "vare", "rstdL", "zt"))
        B_cen2 = [Buf("cen0"), Buf("cen1")]
        B_csq2 = [Buf("csq0"), Buf("csq1")]
        B_mix = [Buf("mix%d" % c) for c in range(8)]
        B_cb16 = [Buf("cb16_%d" % c) for c in range(4)]
        B_out = [Buf("out%d" % m) for m in range(4)]

        ch_slot = [P.chan() for _ in range(NSLOT)]
        ch_store = [P.chan() for _ in range(NSLOT)]
        ch_x = [P.chan() for _ in range(4)]
        ch_o = [P.chan() for _ in range(4)]
        ch_p = P.chan()
        ch_tab = P.chan()
        ch_const = P.chan()

        state = {"bank": 0, "loaded": 0, "st": 0}

        def next_bank():
            i = state["bank"]
            state["bank"] = (i + 1) % 7
            return banks[i], B_bank[i]

        def next_st():
            i = state["st"]
            state["st"] = (i + 1) % 16
            return stats[:, 4 * i:4 * i + 4], B_st[i]

        total_pieces = ntiles * NPIECE

        def piece_len(k):
            if k == P_PLEW:
                return 2048
            if k in (P_DN1 + 2, P_DN1 + 5, P_DN2 + 2, P_DN2 + 5):
                return 6 * 512
            return PIECE

        def load_piece(g):
            t, k = divmod(g, NPIECE)
            s = g % NSLOT
            n = piece_len(k)
            if t == 0:
                P.dma("pool", wring[:, s, 0:n], wp_d[k, :, 0:n], [], [B_slot[s]], ch_slot[s])
                P.dma("sp", scr_d[k, :, 0:n], wring[:, s, 0:n], [B_slot[s]], [B_scr[k]], ch_store[s])
            else:
                P.dma("sp", wring[:, s, 0:n], scr_d[k, :, 0:n], [B_scr[k]], [B_slot[s]], ch_slot[s])

        def consume(t, k, hold=0):
            g = t * NPIECE + k
            while state["loaded"] < min(total_pieces, g + NSLOT - hold):
                load_piece(state["loaded"])
                state["loaded"] += 1
            s = g % NSLOT
            return wring[:, s, :], B_slot[s]

        P.dma("sp", cpar[:], cpar_d, [], [B_const], ch_const)
        P.dma("sp", cf[:], cf_d, [], [B_const], ch_const)
        P.dma("sp", cb[:], cb_d, [], [B_const], ch_const)
        P.op("pool", "memset", dict(ap=vpad[:, 0], constant=0.0), [], [B_vpad[0]])
        P.op("pool", "memset", dict(ap=vpad[:, 1], constant=0.0), [], [B_vpad[1]])

        def load_tile(t):
            r0 = t * T
            for m in range(4):
                P.dma("sp", h_t[:, m, :], x_d[r0 + m * 128:r0 + (m + 1) * 128, :], [], [B_h[m]], ch_x[m])
            P.dma("sp", ptile[:], p_d[r0:r0 + T, :].rearrange("(m q) c -> q m c", q=128), [], [B_p], ch_p)
            pos0 = (t % TILES_PER_SEQ) * T
            P.dma("sp", cosS[:], cos_d[:, pos0:pos0 + T], [], [B_tab], ch_tab)
            P.dma("sp", ssinS[:], ssin_d[:, pos0:pos0 + T], [], [B_tab], ch_tab)

        def rsqrt_dve(x_ap, B_x, y_ap, B_y, t_ap, B_t, iters=2):
            xi = x_ap.bitcast(I32)
            yi = y_ap.bitcast(I32)
            P.op("dve", "tensor_scalar", dict(out=yi, in0=xi, scalar1=1, scalar2=None, op0=ALU.arith_shift_right),
                 [B_x], [B_y])
            P.op("dve", "tensor_scalar", dict(out=yi, in0=yi, scalar1=-1, scalar2=0x5f3759df, op0=ALU.mult, op1=ALU.add),
                 [B_y], [B_y])
            for _ in range(iters):
                P.op("dve", "scalar_tensor_tensor", dict(out=t_ap, in0=y_ap, scalar=-0.5, in1=y_ap, op0=ALU.mult,
                                                         op1=ALU.mult), [B_y], [B_t])
                P.op("dve", "tensor_tensor", dict(out=t_ap, in0=t_ap, in1=x_ap, op=ALU.mult), [B_t, B_x], [B_t])
                P.op("dve", "scalar_tensor_tensor", dict(out=y_ap, in0=t_ap, scalar=1.5, in1=y_ap, op0=ALU.add,
                                                         op1=ALU.mult), [B_t, B_y], [B_y])

        def rstd_from(ms_ap, B_ms, n, factor=1.0):
            rs, B_rs = next_st()
            tt_, B_tt = next_st()
            rsqrt_dve(ms_ap, B_ms, rs[:, 0:n], B_rs, tt_[:, 0:n], B_tt, iters=2)
            if factor != 1.0:
                rs2, B_rs2 = next_st()
                P.op("dve", "tensor_scalar", dict(out=rs2[:, 0:n], in0=rs[:, 0:n], scalar1=float(factor),
                                                  scalar2=None, op0=ALU.mult), [B_rs], [B_rs2])
                return rs2, B_rs2
            return rs, B_rs

        def norm_to_xT(gidx):
            ms, B_ms = next_st()
            for m in range(4):
                P.op("act", "activation", dict(out=junk[:], in_=h_t[:, m, :], func=AF.Square, scale=1.0 / 32.0,
                                               accum_out=ms[:, m:m + 1]), [B_h[m]], [B_ms])
            me, B_me = next_st()
            P.op("dve", "tensor_scalar", dict(out=me[:, 0:4], in0=ms[:, 0:4], scalar1=EPS, scalar2=None,
                                              op0=ALU.add), [B_ms], [B_me])
            rs, B_rs = rstd_from(me[:, 0:4], B_me, 4)
            for m in range(4):
                P.op("act", "activation", dict(out=xn[:, m, :], in_=h_t[:, m, :], func=AF.Copy, scale=rs[:, m:m + 1]),
                     [B_h[m], B_rs], [B_xn[m]])
            for dc in range(8):
                bk, B_bk = next_bank()
                bv = bk[:].bitcast(BF16)
                for m in range(4):
                    P.op("pe", "transpose", dict(out=bv[:, m * 128:(m + 1) * 128],
                                                 in_=xn[:, m, dc * 128:(dc + 1) * 128], identity=ident),
                         [B_xn[m], B_const], [B_bk])
                gcol = cpar[:, C_GPRE + 8 * gidx + dc:C_GPRE + 8 * gidx + dc + 1]
                if dc % 2 == 0:
                    P.op("act", "activation", dict(out=xT[:, dc, :], in_=bv[:, 0:512], func=AF.Copy, scale=gcol),
                         [B_bk, B_const], [B_xT[dc]])
                else:
                    P.op("dve", "tensor_scalar", dict(out=xT[:, dc, :], in0=bv[:, 0:512], scalar1=gcol,
                                                      scalar2=None, op0=ALU.mult), [B_bk, B_const], [B_xT[dc]])

        def aform(t, k, nchunk, col0, width=128):
            w, B_w = consume(t, k)
            wv = w.rearrange("p (c n) -> p c n", c=8)
            for ch in range(nchunk):
                bk, B_bk = next_bank()
                for dc in range(8):
                    P.op("pe", "matmul", dict(out=bk[:], lhsT=wv[:, dc, col0 + ch * width:col0 + (ch + 1) * width],
                                              rhs=xT[:, dc, :], start=(dc == 0), stop=(dc == 7)),
                         [B_w, B_xT[dc]], [B_bk])
                yield ch, bk, B_bk

        def post_update(gidx, factor, ss, B_ss, fb=None, B_fb=None, pre_scaled=False, out_fb=False):
            fb = fbuf if fb is None else fb
            B_fb = B_f if B_fb is None else B_fb
            me, B_me = next_st()
            ssv = ss.rearrange("p (m two) -> p m two", two=2)
            P.op("dve", "scalar_tensor_tensor", dict(out=me[:, 0:4], in0=ssv[:, :, 0], scalar=EPS, in1=ssv[:, :, 1],
                                                     op0=ALU.add, op1=ALU.add), [B_ss], [B_me])
            ck("post_a")
            rs, B_rs = rstd_from(me[:, 0:4], B_me, 4, factor)
            ck("post_b")
            gtab = cpar[:, C_GPOST + 1024 * gidx:C_GPOST + 1024 * (gidx + 1)]
            for m in range(4):
                if pre_scaled and out_fb:
                    P.op("dve", "scalar_tensor_tensor", dict(out=fb[:, m, :], in0=fb[:, m, :], scalar=rs[:, m:m + 1],
                                                             in1=h_t[:, m, :], op0=ALU.mult, op1=ALU.add),
                         [B_fb[m], B_rs, B_h[m]], [B_fb[m]])
                elif pre_scaled:
                    P.op("dve", "scalar_tensor_tensor", dict(out=h_t[:, m, :], in0=fb[:, m, :], scalar=rs[:, m:m + 1],
                                                             in1=h_t[:, m, :], op0=ALU.mult, op1=ALU.add),
                         [B_fb[m], B_rs, B_h[m]], [B_h[m]])
                else:
                    P.op("dve", "scalar_tensor_tensor", dict(out=fb[:, m, :], in0=fb[:, m, :], scalar=rs[:, m:m + 1],
                                                             in1=gtab, op0=ALU.mult, op1=ALU.mult),
                         [B_fb[m], B_rs, B_const], [B_fb[m]])
                    P.op("dve", "tensor_tensor", dict(out=h_t[:, m, :], in0=h_t[:, m, :], in1=fb[:, m, :], op=ALU.add),
                         [B_h[m], B_fb[m]], [B_h[m]])

        def bform_post(t, chunks, pieces, gidx, factor):
            nchunks = len(chunks)
            ss8 = stats[:, 56:64]
            B_ss = B_ss_f
            for dh in range(2):
                acc = [next_bank() for _ in range(4)]
                fc = 0
                for (k, n) in pieces[dh]:
                    w, B_w = consume(t, k)
                    for fcl in range(n):
                        ap, B_c = chunks[fc]
                        for m in range(4):
                            P.op("pe", "matmul", dict(out=acc[m][0][:], lhsT=ap[:, m * 128:(m + 1) * 128],
                                                      rhs=w[:, fcl * 512:(fcl + 1) * 512],
                                                      start=(fc == 0), stop=(fc == nchunks - 1)),
                                 [B_c, B_w], [acc[m][1]])
                        fc += 1
                gt_h = cpar[:, C_GPOST + 1024 * gidx + dh * 512:C_GPOST + 1024 * gidx + (dh + 1) * 512]
                for m in range(4):
                    bk, B_bk = acc[m]
                    fsl = fbuf[:, m, dh * 512:(dh + 1) * 512]
                    def _sq():
                        P.op("act", "activation", dict(out=junk[:, 0:512], in_=bk[:], func=AF.Square, scale=1.0 / 32.0,
                                                       accum_out=ss8[:, 2 * m + dh:2 * m + dh + 1]), [B_bk], [B_ss])

                    def _mul():
                        P.op("dve", "tensor_tensor", dict(out=fsl, in0=bk[:], in1=gt_h, op=ALU.mult),
                             [B_bk, B_const], [B_f[m]])
                    if m % 2 == 0:
                        _sq()
                        _mul()
                    else:
                        _mul()
                        _sq()
            post_update(gidx, factor, ss8, B_ss, pre_scaled=True)

        class _Stop(Exception):
            pass

        def ck(name):
            if debug == name:
                raise _Stop()

        def ffn(t, gidx, p_gu, p_dn):
            norm_to_xT(gidx)
            ck("norm%d" % gidx)
            for jj in range(11):
                w, B_w = consume(t, p_gu + jj)
                wv = w.rearrange("p (c n) -> p c n", c=8)
                for jl in range(2):
                    j = 2 * jj + jl
                    gb, B_gb = next_bank()
                    ub, B_ub = next_bank()
                    for (bk, B_bk, c0) in ((gb, B_gb, jl * 128), (ub, B_ub, 256 + jl * 128)):
                        for dc in range(8):
                            P.op("pe", "matmul", dict(out=bk[:], lhsT=wv[:, dc, c0:c0 + 128], rhs=xT[:, dc, :],
                                                      start=(dc == 0), stop=(dc == 7)), [B_w, B_xT[dc]], [B_bk])
                    P.op("act", "activation", dict(out=sgt[:, j % 2, :], in_=gb[:], func=AF.Silu),
                         [B_gb], [B_sg[j % 2]])
                    P.op("dve", "tensor_tensor", dict(out=hidT[:, j, :], in0=sgt[:, j % 2, :], in1=ub[:], op=ALU.mult),
                         [B_sg[j % 2], B_ub], [B_hid[j]])
            ck("gu%d" % gidx)
            chunks = [(hidT[:, j, :], B_hid[j]) for j in range(NFC)]
            pieces = [[(p_dn + dh * 3 + fb, min(8, NFC - 8 * fb)) for fb in range(3)] for dh in range(2)]
            bform_post(t, chunks, pieces, gidx, 0.5)

        def ln_feature_major(src_list, ones_ap, n_src, gb_cols, out_fn):
            raise NotImplementedError

        def mixer(t):
            first = (t % TILES_PER_SEQ == 0)
            norm_to_xT(1)
            if first:
                for c in range(4):
                    P.op("pool", "memset", dict(ap=cext[:, c, 0:30], constant=0.0), [], [B_cext[c]])
            else:
                for c in range(4):
                    P.op("pool", "tensor_copy", dict(out=cext[:, c, 0:30], in_=halo[:, c, :]),
                         [B_halo[c]], [B_cext[c]])
            for c, bk, B_bk in aform(t, P_WIN + 0, 4, 0):
                P.op("act", "activation", dict(out=cacc[:, c, :], in_=bk[:], func=AF.Copy, scale=0.5),
                     [B_bk], [B_cacc[c]])
            for c, bk, B_bk in aform(t, P_WIN + 1, 4, 0):
                P.op("act", "activation", dict(out=ctmp, in_=bk[:], func=AF.Tanh, scale=0.5), [B_bk], [B_ctmp])
                P.op("dve", "scalar_tensor_tensor", dict(out=cext[:, c, 30:542], in0=ctmp, scalar=1.0,
                                                         in1=cacc[:, c, :], op0=ALU.add, op1=ALU.mult),
                     [B_ctmp, B_cacc[c], B_cext[c]], [B_cext[c]])
            for c in range(4):
                P.op("pool", "tensor_copy", dict(out=halo[:, c, :], in_=cext[:, c, 512:542]), [B_cext[c]], [B_halo[c]])
            bgq = []

            def pump(n):
                for _ in range(min(n, len(bgq))):
                    bgq.pop(0)()

            def gen_dg(c, k0, b2):
                nk = min(8, KCONV - k0)
                wk = cpar[:, C_CONVW + c * KCONV + k0:C_CONVW + c * KCONV + k0 + nk]
                P.op("pool", "tensor_tensor", dict(out=dg[:, b2, 0:nk, :],
                                                   in0=ident.unsqueeze(1).to_broadcast([128, nk, 128]),
                                                   in1=wk.unsqueeze(2).to_broadcast([128, nk, 128]), op=ALU.mult),
                     [B_const], [B_dg[b2]])

            gen_dg(0, 0, 0)
            gen_dg(0, 8, 1)

            def conv_pe():
                di = 0
                for c in range(4):
                    bk, B_bk = next_bank()
                    for k0 in range(0, KCONV, 8):
                        nk = min(8, KCONV - k0)
                        b2 = di % 2
                        di += 1
                        if di > 2:
                            gen_dg(c, k0, b2)
                        for kk in range(nk):
                            k = k0 + kk
                            P.op("pe", "matmul", dict(out=bk[:], lhsT=dg[:, b2, kk, :], rhs=cext[:, c, k:k + 512],
                                                      start=(k == 0), stop=(k == KCONV - 1)),
                                 [B_dg[b2], B_cext[c]], [B_bk])
                    P.op("act", "activation", dict(out=cacc[:, c, :], in_=bk[:], func=AF.Identity,
                                                   bias=cpar[:, C_CONVB + c:C_CONVB + c + 1]),
                         [B_bk, B_const], [B_cacc[c]])

            ck("conv")
            for (pk, dstT, B_dst, scale, is_q) in ((P_WIN + 2, qT, B_qT, 0.125, True), (P_WIN + 3, kT, B_kT, 1.0, False)):
                if not is_q:
                    conv_pe()
                for c, bk, B_bk in aform(t, pk, 4, 0):
                    P.op("act", "activation", dict(out=qraw[:], in_=bk[:], func=AF.Copy, scale=scale), [B_bk], [B_qraw])
                    P.op("act", "activation", dict(out=t1[:], in_=bk[:], func=AF.Copy, scale=scale), [B_bk], [B_t1])
                    P.op("pool", "tensor_tensor", dict(out=t1[:], in0=t1[:], in1=cosS[:], op=ALU.mult),
                         [B_t1, B_tab], [B_t1])
                    sw, B_sw = next_bank()
                    P.op("pe", "matmul", dict(out=sw[:], lhsT=pm, rhs=qraw[:], start=True, stop=True),
                         [B_qraw, B_const], [B_sw])
                    P.op("dve", "tensor_tensor", dict(out=t2[:], in0=sw[:], in1=ssinS[:], op=ALU.mult),
                         [B_sw, B_tab], [B_t2])
                    P.op("pool", "tensor_tensor", dict(out=dstT[:, c, :], in0=t1[:], in1=t2[:], op=ALU.add),
                         [B_t1, B_t2], [B_dst[c]])
                    if is_q:
                        qdec_c = cf[:, F_QDEC + c * 128:F_QDEC + (c + 1) * 128]
                        P.op("pool", "tensor_tensor", dict(
                            out=qdT[:, c, :].rearrange("p (m i) -> p m i", m=4),
                            in0=qT[:, c, :].rearrange("p (m i) -> p m i", m=4),
                            in1=qdec_c.unsqueeze(1).to_broadcast([128, 4, 128]), op=ALU.mult),
                             [B_qT[c], B_const], [B_qdT[c]])
            ck("qk")
            w, B_w = consume(t, P_WIN + 4)
            wv = w.rearrange("p (c n) -> p c n", c=8)
            for m in range(4):
                bk, B_bk = next_bank()
                for dc in range(8):
                    P.op("pe", "matmul", dict(out=bk[:], lhsT=xT[:, dc, m * 128:(m + 1) * 128], rhs=wv[:, dc, :],
                                              start=(dc == 0), stop=(dc == 7)), [B_w, B_xT[dc]], [B_bk])
                P.op("act", "activation", dict(out=vpl[:, m, :], in_=bk[:], func=AF.Copy), [B_bk], [B_vpl[m]])
            for c, bk, B_bk in aform(t, P_WIN + 5, 4, 0):
                P.op("act", "activation", dict(out=sgr[:, c, :], in_=bk[:], func=AF.Silu), [B_bk], [B_sgr[c]])
                P.op("dve", "tensor_scalar", dict(out=sgrb[:, c, :], in0=sgr[:, c, :], scalar1=cpar[:, C_RGB + c:C_RGB + c + 1],
                                                  scalar2=None, op0=ALU.mult), [B_sgr[c], B_const], [B_sgrb[c]])
                P.op("dve", "tensor_scalar", dict(out=sgr[:, c, :], in0=sgr[:, c, :], scalar1=cpar[:, C_RGG + c:C_RGG + c + 1],
                                                  scalar2=None, op0=ALU.mult), [B_sgr[c], B_const], [B_sgr[c]])
            ck("vg")
            cl = {}

            def conv_ln1():
                for c in range(4):
                    P.op("act", "activation", dict(out=cb16[:, c, :], in_=cacc[:, c, :], func=AF.Copy),
                         [B_cacc[c]], [B_cb16[c]])
                mb, B_mb = banks[7], B_bank[7]
                cl["mb"] = (mb, B_mb)
                for c in range(4):
                    P.op("pe", "matmul", dict(out=mb[:], lhsT=o512, rhs=cb16[:, c, :], start=(c == 0), stop=(c == 3)),
                         [B_cb16[c], B_const], [B_mb])

            def conv_ln2():
                mb, B_mb = cl["mb"]
                for c in range(4):
                    P.op("dve", "tensor_tensor", dict(out=cacc[:, c, :], in0=cacc[:, c, :], in1=mb[:], op=ALU.subtract),
                         [B_cacc[c], B_mb], [B_cacc[c]])
                    P.op("act", "activation", dict(out=cb16[:, c, :], in_=cacc[:, c, :], func=AF.Square),
                         [B_cacc[c]], [B_cb16[c]])
                vb, B_vb = banks[7], B_bank[7]
                cl["vb"] = (vb, B_vb)
                for c in range(4):
                    P.op("pe", "matmul", dict(out=vb[:], lhsT=o512, rhs=cb16[:, c, :], start=(c == 0), stop=(c == 3)),
                         [B_cb16[c], B_const], [B_vb])

            def conv_ln3():
                vb, B_vb = cl["vb"]
                P.op("act", "activation", dict(out=ctmp, in_=vb[:], func=AF.Sqrt, bias=EPS), [B_vb], [B_ctmp])
                P.op("dve", "reciprocal", dict(out=ctmp, in_=ctmp), [B_ctmp], [B_ctmp])
                for c in range(4):
                    P.op("pool" if c % 2 else "dve", "tensor_tensor",
                         dict(out=cacc[:, c, :], in0=cacc[:, c, :], in1=ctmp, op=ALU.mult),
                         [B_cacc[c], B_ctmp], [B_cacc[c]])
                    P.op("act", "activation", dict(out=mixT[:, 4 + c, :], in_=cacc[:, c, :], func=AF.Silu,
                                                   scale=cpar[:, C_CLNG + c:C_CLNG + c + 1],
                                                   bias=cpar[:, C_CLNB + c:C_CLNB + c + 1]),
                         [B_cacc[c], B_const], [B_mix[4 + c]])

            ck("convln")
            if first:
                P.op("pool", "memset", dict(ap=R_t[:], constant=0.0), [], [B_R])
                P.op("pool", "memset", dict(ap=Rbf[:], constant=0.0), [], [B_Rbf])
            def A1(m):
                cs = slice(m * 128, (m + 1) * 128)
                i2 = m % 2
                kb, B_kb = next_bank()
                kbv = kb[:].bitcast(BF16)
                for pp in range(4):
                    P.op("pe", "transpose", dict(out=kbv[:, pp * 128:(pp + 1) * 128], in_=kT[:, pp, cs], identity=ident),
                         [B_kT[pp], B_const], [B_kb])
                P.op("dve", "tensor_tensor", dict(
                    out=kd[:, i2, :].rearrange("p (h d) -> p h d", h=8),
                    in0=kbv[:, 0:512].rearrange("p (h d) -> p h d", h=8),
                    in1=cf[:, F_KDEC:F_KDEC + 8].unsqueeze(2).to_broadcast([128, 8, 64]), op=ALU.mult),
                     [B_kb, B_const], [B_kd[i2]])
                vsrc = vpl[:, m, :].rearrange("p (a b e) -> p a b e", a=4, b=2)
                vdst = vpad[:, i2].rearrange("p (a b) e -> p a b e", a=4)
                P.op("pool", "tensor_copy", dict(out=vdst[:, :, 0, 0:64], in_=vsrc[:, :, 0, :]), [B_vpl[m]], [B_vpad[i2]])
                P.op("pool", "tensor_copy", dict(out=vdst[:, :, 1, 64:128], in_=vsrc[:, :, 1, :]), [B_vpl[m]], [B_vpad[i2]])
                sa, B_sa = next_bank()
                sbk, B_sbk = next_bank()
                for pp in range(4):
                    for hh, (bk, B_bk) in enumerate(((sa, B_sa), (sbk, B_sbk))):
                        rs_ = slice(hh * 64, (hh + 1) * 64)
                        P.op("pe", "matmul", dict(out=bk[:, pp * 128:(pp + 1) * 128], lhsT=kT[rs_, pp, cs],
                                                  rhs=qT[rs_, pp, cs], start=True, stop=True),
                             [B_kT[pp], B_qT[pp]], [B_bk])
                for hh, (bk, B_bk) in enumerate(((sa, B_sa), (sbk, B_sbk))):
                    P.op("dve", "tensor_tensor", dict(
                        out=sTm[:, i2, hh * 4:(hh + 1) * 4, :].rearrange("p a i -> p (a i)"),
                        in0=bk[:], in1=cf[:, F_MASK + hh * 512:F_MASK + (hh + 1) * 512], op=ALU.mult),
                         [B_bk, B_const], [B_sTm[i2]])

            obanks = {}

            def A2(m):
                cs = slice(m * 128, (m + 1) * 128)
                i2 = m % 2
                ob, B_ob = next_bank()
                obanks[m] = (ob, B_ob)
                for pp in range(4):
                    osl = ob[:, pp * 128:(pp + 1) * 128]
                    P.op("pe", "matmul", dict(out=osl, lhsT=vpad[:, i2, 2 * pp, :], rhs=sTm[:, i2, pp, :],
                                              start=True, stop=False), [B_vpad[i2], B_sTm[i2]], [B_ob])
                    P.op("pe", "matmul", dict(out=osl, lhsT=vpad[:, i2, 2 * pp + 1, :], rhs=sTm[:, i2, 4 + pp, :],
                                              start=False, stop=False), [B_vpad[i2], B_sTm[i2]], [B_ob])
                    P.op("pe", "matmul", dict(out=osl, lhsT=Rbf[:, pp * 128:(pp + 1) * 128], rhs=qdT[:, pp, cs],
                                              start=False, stop=True), [B_Rbf, B_qdT[pp]], [B_ob])
                kvb, B_kvb = next_bank()
                for pp in range(4):
                    P.op("pe", "matmul", dict(out=kvb[:, pp * 128:(pp + 1) * 128], lhsT=kd[:, i2, pp * 128:(pp + 1) * 128],
                                              rhs=vpl[:, m, pp * 128:(pp + 1) * 128], start=True, stop=True),
                         [B_kd[i2], B_vpl[m]], [B_kvb])
                P.op("dve", "tensor_tensor", dict(
                    out=kvt[:].rearrange("p (a e) -> p a e", a=4), in0=kvb[:].rearrange("p (a e) -> p a e", a=4),
                    in1=cf[:, F_BDM:F_BDM + 128].unsqueeze(1).to_broadcast([128, 4, 128]), op=ALU.mult),
                     [B_kvb, B_const], [B_kvt])
                P.op("pool", "tensor_tensor", dict(
                    out=R_t[:].rearrange("p (a e) -> p a e", a=4), in0=R_t[:].rearrange("p (a e) -> p a e", a=4),
                    in1=cf[:, F_CDEC:F_CDEC + 4].unsqueeze(2).to_broadcast([128, 4, 128]), op=ALU.mult),
                     [B_R, B_const], [B_R])
                P.op("pool", "tensor_tensor", dict(out=R_t[:], in0=R_t[:], in1=kvt[:], op=ALU.add), [B_R, B_kvt], [B_R])
                P.op("act", "activation", dict(out=Rbf[:], in_=R_t[:], func=AF.Copy), [B_R], [B_Rbf])

            def B1(m):
                cen, B_cen, csq, B_csq = cen2[:, m % 2, :], B_cen2[m % 2], csq2[:, m % 2, :], B_csq2[m % 2]
                ob, B_ob = obanks[m]
                P.op("act", "activation", dict(out=o_f[:], in_=ob[:], func=AF.Copy), [B_ob], [B_of])
                P.op("act", "activation", dict(out=o_bf[:], in_=ob[:], func=AF.Copy), [B_ob], [B_obf])
                mb, B_mb = next_bank()
                P.op("pe", "matmul", dict(out=mb[:], lhsT=bd64, rhs=o_bf[:], start=True, stop=True),
                     [B_obf, B_const], [B_mb])
                P.op("dve", "tensor_tensor", dict(out=cen[:], in0=o_f[:], in1=mb[:], op=ALU.subtract),
                     [B_of, B_mb], [B_cen])
                P.op("act", "activation", dict(out=csq[:], in_=cen[:], func=AF.Square), [B_cen], [B_csq])

            def B2(m):
                cen, B_cen, csq, B_csq = cen2[:, m % 2, :], B_cen2[m % 2], csq2[:, m % 2, :], B_csq2[m % 2]
                cs = slice(m * 128, (m + 1) * 128)
                vb, B_vb = next_bank()
                P.op("pe", "matmul", dict(out=vb[:], lhsT=bd64, rhs=csq[:], start=True, stop=True),
                     [B_csq, B_const], [B_vb])
                P.op("act", "activation", dict(out=vare[:], in_=vb[:], func=AF.Sqrt, bias=EPS), [B_vb], [B_vare])
                P.op("dve", "reciprocal", dict(out=rstdL[:], in_=vare[:]), [B_vare], [B_rstdL])
                P.op("dve", "tensor_tensor", dict(out=cen[:], in0=cen[:], in1=rstdL[:], op=ALU.mult),
                     [B_cen, B_rstdL], [B_cen])
                P.op("pool", "tensor_tensor", dict(out=zt[:].rearrange("p (a i) -> p a i", a=4),
                                                   in0=cen[:].rearrange("p (a i) -> p a i", a=4),
                                                   in1=sgr[:, :, cs], op=ALU.mult), [B_cen] + B_sgr, [B_zt])
                P.op("pool", "tensor_tensor", dict(out=mixT[:, 0:4, cs], in0=zt[:].rearrange("p (a i) -> p a i", a=4),
                                                   in1=sgrb[:, :, cs], op=ALU.add), [B_zt] + B_sgrb, B_mix[0:4])

            conv_ln1()
            A1(0)
            A2(0)
            B1(0)
            for m in range(1, 4):
                A1(m)
                A2(m)
                B1(m)
                B2(m - 1)
                if m == 1:
                    conv_ln2()
            B2(3)
            conv_ln3()
            pump(len(bgq))
            ck("ret")
            chunks = [(mixT[:, c, :], B_mix[c]) for c in range(8)]
            bform_post(t, chunks, [[(P_WOUT + 0, 8)], [(P_WOUT + 1, 8)]], 1, 1.0)

        def ple(t):
            norm_to_xT(3)
            P.op("pool", "tensor_copy", dict(out=p_bf[:], in_=ptile[:]), [B_p], [B_pbf])
            bk, B_bk = next_bank()
            bv = bk[:].bitcast(BF16)
            for dc in range(2):
                for m in range(4):
                    P.op("pe", "transpose", dict(out=bv[:, dc * 512 + m * 128:dc * 512 + (m + 1) * 128],
                                                 in_=p_bf[:, m, dc * 128:(dc + 1) * 128], identity=ident),
                         [B_pbf, B_const], [B_bk])
            P.op("act", "activation", dict(out=pT[:].rearrange("p c t -> p (c t)"), in_=bv[:, 0:1024], func=AF.Copy),
                 [B_bk], [B_pT])
            we, B_we = consume(t, P_PLEW)
            wev = we[:, 0:2048].rearrange("p (c n) -> p c n", c=2)
            ss8 = stats[:, 48:56]
            B_ss = B_ss_p
            for dh in range(2):
                wg, B_wg = consume(t, P_GATE + dh, hold=1 + dh)
                wgv = wg.rearrange("p (c n) -> p c n", c=8)
                for m in range(4):
                    ms_ = slice(m * 128, (m + 1) * 128)
                    gbk, B_gbk = next_bank()
                    ebk, B_ebk = next_bank()
                    for dc in range(8):
                        P.op("pe", "matmul", dict(out=gbk[:], lhsT=xT[:, dc, ms_], rhs=wgv[:, dc, :],
                                                  start=(dc == 0), stop=(dc == 7)), [B_xT[dc], B_wg], [B_gbk])
                    for dc in range(2):
                        P.op("pe", "matmul", dict(out=ebk[:], lhsT=pT[:, dc, ms_], rhs=wev[:, dc, dh * 512:(dh + 1) * 512],
                                                  start=(dc == 0), stop=(dc == 1)), [B_pT, B_we], [B_ebk])
                    P.op("act", "activation", dict(out=sgt[:, m % 2, :], in_=gbk[:], func=AF.Tanh, scale=0.5),
                         [B_gbk], [B_sg[m % 2]])
                    fsl = fple[:, m, dh * 512:(dh + 1) * 512]
                    P.op("dve", "scalar_tensor_tensor", dict(out=fsl, in0=sgt[:, m % 2, :], scalar=1.0, in1=ebk[:],
                                                             op0=ALU.add, op1=ALU.mult), [B_sg[m % 2], B_ebk], [B_fp[m]])
                    P.op("act", "activation", dict(out=junk[:, 0:512], in_=fsl, func=AF.Square, scale=1.0 / 64.0,
                                                   accum_out=ss8[:, 2 * m + dh:2 * m + dh + 1]), [B_fp[m]], [B_ss])
                    P.op("dve", "tensor_tensor", dict(
                        out=fsl, in0=fsl, in1=cpar[:, C_GPOST + 3 * 1024 + dh * 512:C_GPOST + 3 * 1024 + (dh + 1) * 512],
                        op=ALU.mult), [B_fp[m], B_const], [B_fp[m]])
            post_update(3, 0.5, ss8, B_ss, fple, B_fp, pre_scaled=True, out_fb=True)
            state["final_in_fple"] = True

        def store_tile(t):
            r0 = t * T
            for m in range(4):
                if state.get("final_in_fple"):
                    P.dma("sp", out_d[r0 + m * 128:r0 + (m + 1) * 128, :], fple[:, m, :], [B_fp[m]], [B_out[m]], ch_o[m])
                else:
                    P.dma("sp", out_d[r0 + m * 128:r0 + (m + 1) * 128, :], h_t[:, m, :], [B_h[m]], [B_out[m]], ch_o[m])
            state["final_in_fple"] = False

        for t in range(ntiles):
            load_tile(t)
            try:
                ck("load")
                ffn(t, 0, P_GU1, P_DN1)
                ck("ffn1")
                mixer(t)
                ck("mixer")
                ffn(t, 2, P_GU2, P_DN2)
                ck("ffn2")
                ple(t)
            except _Stop:
                pass
            store_tile(t)
        P.wait_all("sp", B_out + B_scr)
        nwait = P.emit(stack)
        build_program.info = dict(nops=len(P.ops), nwait=nwait, per_eng=dict(P.nseq))
    return nc


_CACHE = {}


def _host_inputs(inp):
    cf, cosT, ssinT, cb = _const_tables()
    wp = _layout_weights(inp)
    cp = _layout_params(inp)
    x = np.ascontiguousarray(np.asarray(inp["x"], np.float32)).reshape(NCORES, TOK_CORE, D)
    p = np.ascontiguousarray(np.asarray(inp["p"], np.float32)[0]).reshape(NCORES, TOK_CORE, 256)
    maps = []
    for c in range(NCORES):
        maps.append(dict(x=x[c], p=p[c], wp=wp, cpar=cp, cf=cf, cosT=cosT, ssinT=ssinT, cb=cb))
    return maps


def kernel(**inputs):
    inp = {k: np.asarray(v) for k, v in inputs.items()}
    if "nc" not in _CACHE:
        _CACHE["nc"] = build_program(NTILES)
    nc = _CACHE["nc"]
    maps = _host_inputs(inp)
    res = run_bass_kernel_spmd(nc, maps, core_ids=list(range(NCORES)))
    out = np.stack([np.asarray(r["out"], np.float32) for r in res.results], axis=0)
    return out.reshape(BATCH, SEQ, D)
```

```python
import contextlib
import numpy as np
import ml_dtypes
import concourse.bass as bass
import concourse.mybir as mybir
from concourse.bass_utils import run_bass_kernel_spmd

F32 = mybir.dt.float32
BF16 = mybir.dt.bfloat16
I32 = mybir.dt.int32
AF = mybir.ActivationFunctionType
ALU = mybir.AluOpType

D = 1024
DFF = 2816
NFC = 22
SEQ = 4096
BATCH = 16
NCORES = 8
TOK_CORE = BATCH * SEQ // NCORES
T = 512
NTILES = TOK_CORE // T
TILES_PER_SEQ = SEQ // T
EPS = 1e-6
NSLOT = 4
PIECE = 4096
NPIECE = 45
KCONV = 31

P_GU1 = 0
P_DN1 = 11
P_WIN = 17
P_WOUT = 23
P_GU2 = 25
P_DN2 = 36
P_PLEW = 42
P_GATE = 43
WIN_ORDER = [4, 5, 0, 1, 2, 3]

C_GPRE = 0
C_GPOST = 32
C_CONVW = C_GPOST + 4096
C_CONVB = C_CONVW + 124
C_CLNG = C_CONVB + 4
C_CLNB = C_CLNG + 4
C_RGG = C_CLNB + 4
C_RGB = C_RGG + 4
NCPAR = C_RGB + 4

F_QDEC = 0
F_MASK = 512
F_BDM = 1536
F_KDEC = 1664
F_CDEC = 1672
F_MHALF = 1676
NCF = 1680


class Buf:
    __slots__ = ("name", "w", "r", "over", "excl")

    def __init__(self, name, excl=False):
        self.name = name
        self.w = {}
        self.r = {}
        self.over = []
        self.excl = excl


def overlap(a_list, b_list):
    for a in a_list:
        for b in b_list:
            a.over.append(b)
            b.over.append(a)


class Prog:
    ENG = ("pe", "act", "dve", "pool", "sp")

    def __init__(self, nc):
        self.nc = nc
        self.engs = {"pe": nc.tensor, "act": nc.scalar, "dve": nc.vector, "pool": nc.gpsimd, "sp": nc.sync}
        self.ops = []
        self.nseq = {e: 0 for e in self.ENG}
        self.seen = {e: {} for e in self.ENG}
        self.nchan = 0
        self.chan_count = []

    def chan(self):
        self.chan_count.append(0)
        self.nchan += 1
        return self.nchan - 1

    def _deps(self, eng, reads, writes):
        deps = {}
        seen = self.seen[eng]

        def need(ev, hazard):
            kind, key, val = ev
            if kind == "c" and key == eng and eng == "pe":
                return
            k = (kind, key)
            if seen.get(k, 0) >= val:
                return
            if deps.get(k, 0) < val:
                deps[k] = val

        for b in reads:
            for ev in b.w.values():
                need(ev, "RAW")
            if b.excl:
                for (kk, key), ev in b.r.items():
                    if not (kk == "c" and key == eng):
                        need(ev, "RAR")
        for b in writes:
            for bb in [b] + b.over:
                for ev in bb.w.values():
                    need(ev, "WAW")
                for ev in bb.r.values():
                    need(ev, "WAR")
        for k, v in deps.items():
            seen[k] = v
        return deps

    def op(self, eng, name, kw, reads=(), writes=()):
        deps = self._deps(eng, reads, writes)
        self.nseq[eng] += 1
        seq = self.nseq[eng]
        ev = ("c", eng, seq)
        for b in reads:
            b.r[("c", eng)] = ev
        for b in writes:
            b.w = {("c", eng): ev}
            b.r = {}
        self.ops.append((eng, name, kw, deps, seq, None))

    def dma(self, q, out, in_, reads, writes, chan):
        deps = self._deps(q, reads, writes)
        self.nseq[q] += 1
        seq = self.nseq[q]
        self.chan_count[chan] += 16
        ev = ("d", chan, self.chan_count[chan])
        for b in reads:
            b.r[("d", chan)] = ev
        for b in writes:
            b.w = {("d", chan): ev}
            b.r = {}
        self.ops.append((q, "dma_start", dict(out=out, in_=in_), deps, seq, chan))

    def wait_all(self, eng, bufs):
        deps = self._deps(eng, bufs, bufs)
        self.nseq[eng] += 1
        self.ops.append((eng, None, None, deps, self.nseq[eng], None))

    def emit(self, stack):
        nc = self.nc
        esem = {e: stack.enter_context(nc.semaphore("s_" + e)) for e in self.ENG}
        csem = [stack.enter_context(nc.semaphore("c_%d" % i)) for i in range(self.nchan)]
        needed = {e: set() for e in self.ENG}
        for (_, _, _, deps, _, _) in self.ops:
            for (kind, key), val in deps.items():
                if kind == "c":
                    needed[key].add(val)
        cnt = {}
        for e in self.ENG:
            cnt[e] = {s: i + 1 for i, s in enumerate(sorted(needed[e]))}
        nwait = 0
        for (eng, name, kw, deps, seq, chan) in self.ops:
            E = self.engs[eng]
            for (kind, key), val in deps.items():
                if kind == "c":
                    E.wait_ge(esem[key], cnt[key][val])
                else:
                    E.wait_ge(csem[key], val)
                nwait += 1
            if name is None:
                continue
            ins = getattr(E, name)(**kw)
            if chan is not None:
                ins.then_inc(csem[chan], 16)
            elif seq in cnt[eng]:
                ins.then_inc(esem[eng], 1)
        return nwait


def _const_tables():
    lg = np.log1p(-np.exp2(-5.0 - np.arange(8, dtype=np.float32))).astype(np.float32)
    pos = np.arange(128, dtype=np.float32)
    diff = pos[None, :] - pos[:, None]
    mask = np.where(diff[:, None, :] >= 0,
                    np.exp(lg[None, :, None] * np.maximum(diff, 0.0)[:, None, :]), 0.0).astype(np.float32)
    mask = mask[:, [0, 2, 4, 6, 1, 3, 5, 7], :]
    kdec = np.exp(lg[None, :] * (127.0 - pos)[:, None]).astype(np.float32)
    qdec_h = np.exp(lg[:, None] * (pos + 1.0)[None, :]).astype(np.float32)
    cdec_h = np.exp(lg * 128.0).astype(np.float32)
    qdec = np.zeros((128, 4, 128), np.float32)
    cdec = np.zeros((128, 4), np.float32)
    for p in range(4):
        for hh in range(2):
            qdec[hh * 64:(hh + 1) * 64, p, :] = qdec_h[2 * p + hh][None, :]
            cdec[hh * 64:(hh + 1) * 64, p] = cdec_h[2 * p + hh]
    bdm = np.zeros((128, 128), np.float32)
    bdm[:64, :64] = 1.0
    bdm[64:, 64:] = 1.0
    cf = np.zeros((128, NCF), np.float32)
    cf[:, F_QDEC:F_QDEC + 512] = qdec.reshape(128, 512)
    cf[:, F_MASK:F_MASK + 1024] = mask.reshape(128, 1024)
    cf[:, F_BDM:F_BDM + 128] = bdm
    cf[:, F_KDEC:F_KDEC + 8] = kdec
    cf[:, F_CDEC:F_CDEC + 4] = cdec
    cf[:, F_MHALF] = -0.5
    inv_freq = (10000.0 ** (-np.arange(0, 64, 2, dtype=np.float32) / 64.0)).astype(np.float32)
    ang = (np.arange(SEQ, dtype=np.float32)[:, None] * inv_freq[None, :]).astype(np.float32)
    cos = np.cos(ang).astype(np.float32)
    sin = np.sin(ang).astype(np.float32)
    cos64 = np.concatenate([cos, cos], axis=1).T
    ssin64 = np.concatenate([-sin, sin], axis=1).T
    cosT = np.ascontiguousarray(np.concatenate([cos64, cos64], axis=0))
    ssinT = np.ascontiguousarray(np.concatenate([ssin64, ssin64], axis=0))
    ident = np.eye(128, dtype=np.float32)
    pm = np.zeros((128, 128), np.float32)
    for f in range(128):
        pm[f ^ 32, f] = 1.0
    bd64 = bdm / 64.0
    o512 = np.full((128, 128), 1.0 / 512.0, np.float32)
    cb = np.concatenate([ident, pm, bd64, o512], axis=1).astype(ml_dtypes.bfloat16)
    return cf, cosT, ssinT, cb


def _layout_weights(inp):
    wp = np.zeros((NPIECE, 128, PIECE), np.float32)

    def rows(w):
        return w.reshape(-1, 128, w.shape[1]).transpose(1, 0, 2)

    for base_gu, base_dn, wgu, wdn in ((P_GU1, P_DN1, inp["ffn1_w_gu"][0], inp["ffn1_w_down"][0]),
                                        (P_GU2, P_DN2, inp["ffn2_w_gu"][0], inp["ffn2_w_down"][0])):
        r = rows(wgu)
        for jj in range(11):
            pc = wp[base_gu + jj].reshape(128, 8, 512)
            pc[:, :, 0:256] = r[:, :, jj * 256:(jj + 1) * 256]
            pc[:, :, 256:512] = r[:, :, DFF + jj * 256:DFF + (jj + 1) * 256]
        r = rows(wdn)
        for dh in range(2):
            for fb in range(3):
                n = min(8, NFC - fb * 8)
                pc = wp[base_dn + dh * 3 + fb].reshape(128, 8, 512)
                pc[:, 0:n, :] = r[:, fb * 8:fb * 8 + n, dh * 512:(dh + 1) * 512]
    r = rows(inp["w_in"][0])
    for i, cbk in enumerate(WIN_ORDER):
        wp[P_WIN + i].reshape(128, 8, 512)[:] = r[:, :, cbk * 512:(cbk + 1) * 512]
    r = rows(inp["w_out"][0])
    for dh in range(2):
        wp[P_WOUT + dh].reshape(128, 8, 512)[:] = r[:, :, dh * 512:(dh + 1) * 512]
    r = rows(inp["ple_w"][0])
    wp[P_PLEW][:, 0:2048] = r.reshape(128, 2048)
    r = rows(inp["ple_gate_w"][0])
    for dh in range(2):
        wp[P_GATE + dh].reshape(128, 8, 512)[:] = r[:, :, dh * 512:(dh + 1) * 512]
    return wp


def _layout_params(inp):
    cp = np.zeros((128, NCPAR), np.float32)

    def col(v, n):
        return v.reshape(n, 128).T

    for i, k in enumerate(("ffn1_pre_g", "mix_pre_g", "ffn2_pre_g", "ple_gate_norm_g")):
        cp[:, C_GPRE + 8 * i:C_GPRE + 8 * (i + 1)] = col(inp[k][0], 8)
    for i, k in enumerate(("ffn1_post_g", "mix_post_g", "ffn2_post_g", "ple_post_g")):
        cp[:, C_GPOST + 1024 * i:C_GPOST + 1024 * (i + 1)] = np.broadcast_to(inp[k][0][None, :], (128, 1024))
    cw = inp["conv_w"][0]
    cp[:, C_CONVW:C_CONVW + 124] = cw.T.reshape(4, 128, KCONV).transpose(1, 0, 2).reshape(128, 124)
    cp[:, C_CONVB:C_CONVB + 4] = col(inp["conv_b"][0], 4)
    cp[:, C_CLNG:C_CLNG + 4] = col(inp["conv_ln_g"][0], 4)
    cp[:, C_CLNB:C_CLNB + 4] = col(inp["conv_ln_b"][0], 4)
    cp[:, C_RGG:C_RGG + 4] = col(inp["ret_gn_g"][0], 4)
    cp[:, C_RGB:C_RGB + 4] = col(inp["ret_gn_b"][0], 4)
    return cp


def build_program(ntiles=NTILES, debug=None):
    nc = bass.Bass("TRN2", target_bir_lowering=False)
    x_d = nc.dram_tensor("x", [TOK_CORE, D], F32, kind="ExternalInput").ap()
    p_d = nc.dram_tensor("p", [TOK_CORE, 256], F32, kind="ExternalInput").ap()
    wp_d = nc.dram_tensor("wp", [NPIECE, 128, PIECE], F32, kind="ExternalInput").ap()
    cpar_d = nc.dram_tensor("cpar", [128, NCPAR], F32, kind="ExternalInput").ap()
    cf_d = nc.dram_tensor("cf", [128, NCF], F32, kind="ExternalInput").ap()
    cos_d = nc.dram_tensor("cosT", [128, SEQ], F32, kind="ExternalInput").ap()
    ssin_d = nc.dram_tensor("ssinT", [128, SEQ], F32, kind="ExternalInput").ap()
    cb_d = nc.dram_tensor("cb", [128, 512], BF16, kind="ExternalInput").ap()
    out_d = nc.dram_tensor("out", [TOK_CORE, D], F32, kind="ExternalOutput").ap()
    scr_d = nc.dram_tensor("wscr", [NPIECE, 128, PIECE], BF16, kind="Internal").ap()

    stack = contextlib.ExitStack()
    with stack:
        def sb(name, shape, dt):
            return stack.enter_context(nc.sbuf_tensor(name, shape, dt))

        P = Prog(nc)

        h_t = sb("h", [128, 4, D], F32)
        arenaA = sb("arenaA", [128, 4096], F32)
        fbuf = arenaA[:].rearrange("p (m d) -> p m d", m=4)
        xn = arenaA[:, 0:2048].bitcast(BF16).rearrange("p (m d) -> p m d", m=4)
        xT = arenaA[:, 2048:4096].bitcast(BF16).rearrange("p (c t) -> p c t", c=8)
        arenaB = sb("arenaB", [128, NFC * 512], BF16)
        hidT = arenaB[:].rearrange("p (j t) -> p j t", j=NFC)
        cext = arenaB[:, 0:2168].rearrange("p (c t) -> p c t", c=4)
        cacc = arenaB[:, 4336:8432].bitcast(F32).rearrange("p (c t) -> p c t", c=4)
        ctmp = arenaB[:, 8432:9456].bitcast(F32)
        wring = sb("wring", [128, NSLOT, PIECE], BF16)
        ptile = sb("ptile", [128, 4, 256], F32)
        p_bf = sb("p_bf", [128, 4, 256], BF16)
        pT = sb("pT", [128, 2, 512], BF16)
        cpar = sb("cpar_s", [128, NCPAR], F32)
        cf = sb("cf_s", [128, NCF], F32)
        cb = sb("cb_s", [128, 512], BF16)
        cosS = sb("cosS", [128, 512], F32)
        ssinS = sb("ssinS", [128, 512], F32)
        sgt = sb("sgt", [128, 2, 512], F32)
        junk = sb("junk", [128, 1024], BF16)
        stats = sb("stats", [128, 64], F32)
        qraw = sb("qraw", [128, 512], BF16)
        t1 = sb("t1", [128, 512], F32)
        t2 = sb("t2", [128, 512], F32)
        qT = sb("qT", [128, 4, 512], BF16)
        qdT = sb("qdT", [128, 4, 512], BF16)
        kT = sb("kT", [128, 4, 512], BF16)
        kd = sb("kd", [128, 2, 512], BF16)
        vpl = sb("vpl", [128, 4, 512], BF16)
        vpad = sb("vpad", [128, 2, 8, 128], BF16)
        sgr = sb("sgr", [128, 4, 512], BF16)
        sgrb = sb("sgrb", [128, 4, 512], BF16)
        sTm = sb("sTm", [128, 2, 8, 128], BF16)
        R_t = sb("R", [128, 512], F32)
        Rbf = sb("Rbf", [128, 512], BF16)
        kvt = sb("kvt", [128, 512], F32)
        o_f = sb("o_f", [128, 512], F32)
        o_bf = sb("o_bf", [128, 512], BF16)
        cen2 = sb("cen", [128, 2, 512], F32)
        csq2 = sb("csq", [128, 2, 512], BF16)
        vare = sb("vare", [128, 512], F32)
        rstdL = sb("rstdL", [128, 512], F32)
        zt = sb("zt", [128, 512], F32)
        mixT = sb("mixT", [128, 8, 512], BF16)
        cb16 = sb("cb16", [128, 4, 512], BF16)
        halo = sb("halo", [128, 4, 30], BF16)
        dg = sb("dg", [128, 2, 8, 128], BF16)
        fple = arenaB[:, 0:8192].bitcast(F32).rearrange("p (m d) -> p m d", m=4)
        banks = [stack.enter_context(nc.psum_tensor("bank%d" % i, [128, 512], F32)) for i in range(8)]

        ident = cb[:, 0:128]
        pm = cb[:, 128:256]
        bd64 = cb[:, 256:384]
        o512 = cb[:, 384:512]
        mhalf = cf[:, F_MHALF:F_MHALF + 1]

        B_h = [Buf("h%d" % m) for m in range(4)]
        B_xn = [Buf("xn%d" % m) for m in range(4)]
        B_xT = [Buf("xT%d" % c) for c in range(8)]
        B_f = [Buf("f%d" % m) for m in range(4)]
        overlap(B_f, B_xn + B_xT)
        B_hid = [Buf("hid%d" % j) for j in range(NFC)]
        B_cext = [Buf("cext%d" % c) for c in range(4)]
        B_cacc = [Buf("cacc%d" % c) for c in range(4)]
        B_ctmp = Buf("ctmp")
        overlap(B_hid, B_cext + B_cacc + [B_ctmp])
        B_fp = [Buf("fp%d" % m) for m in range(4)]
        overlap(B_fp, B_hid + B_cext + B_cacc + [B_ctmp])
        B_halo = [Buf("halo%d" % c) for c in range(4)]
        B_dg = [Buf("dg0"), Buf("dg1")]
        B_ss_f, B_ss_p = Buf("ss8f"), Buf("ss8p")
        B_slot = [Buf("slot%d" % s) for s in range(NSLOT)]
        B_scr = [Buf("scr%d" % k) for k in range(NPIECE)]
        B_bank = [Buf("bank%d" % i, excl=True) for i in range(8)]
        B_const = Buf("const")
        B_tab = Buf("tab")
        B_p = Buf("ptile")
        B_pbf = Buf("pbf")
        B_pT = Buf("pT")
        B_sg = [Buf("sg0"), Buf("sg1")]
        B_st = [Buf("st%d" % i) for i in range(16)]
        B_qraw, B_t1, B_t2 = Buf("qraw"), Buf("t1"), Buf("t2")
        B_qT = [Buf("qT%d" % c) for c in range(4)]
        B_qdT = [Buf("qdT%d" % c) for c in range(4)]
        B_kT = [Buf("kT%d" % c) for c in range(4)]
        B_kd = [Buf("kd0"), Buf("kd1")]
        B_vpl = [Buf("vpl%d" % m) for m in range(4)]
        B_vpad = [Buf("vpad0"), Buf("vpad1")]
        B_sgr = [Buf("sgr%d" % c) for c in range(4)]
        B_sgrb = [Buf("sgrb%d" % c) for c in range(4)]
        B_sTm = [Buf("sTm0"), Buf("sTm1")]
        B_R, B_Rbf, B_kvt = Buf("R"), Buf("Rbf"), Buf("kvt")
        B_of, B_obf, B_vare, B_rstdL, B_zt = (Buf(n) for n in ("of", "obf", "vare", "rstdL", "zt"))
        B_cen2 = [Buf("cen0"), Buf("cen1")]
        B_csq2 = [Buf("csq0"), Buf("csq1")]
        B_mix = [Buf("mix%d" % c) for c in range(8)]
        B_cb16 = [Buf("cb16_%d" % c) for c in range(4)]
        B_out = [Buf("out%d" % m) for m in range(4)]

        ch_slot = [P.chan() for _ in range(NSLOT)]
        ch_store = [P.chan() for _ in range(NSLOT)]
        ch_x = [P.chan() for _ in range(4)]
        ch_o = [P.chan() for _ in range(4)]
        ch_p = P.chan()
        ch_tab = P.chan()
        ch_const = P.chan()

        state = {"bank": 0, "loaded": 0, "st": 0}

        def next_bank():
            i = state["bank"]
            state["bank"] = (i + 1) % 7
            return banks[i], B_bank[i]

        def next_st():
            i = state["st"]
            state["st"] = (i + 1) % 16
            return stats[:, 4 * i:4 * i + 4], B_st[i]

        total_pieces = ntiles * NPIECE

        def piece_len(k):
            if k == P_PLEW:
                return 2048
            if k in (P_DN1 + 2, P_DN1 + 5, P_DN2 + 2, P_DN2 + 5):
                return 6 * 512
            return PIECE

        def load_piece(g):
            t, k = divmod(g, NPIECE)
            s = g % NSLOT
            n = piece_len(k)
            if t == 0:
                P.dma("pool", wring[:, s, 0:n], wp_d[k, :, 0:n], [], [B_slot[s]], ch_slot[s])
                P.dma("sp", scr_d[k, :, 0:n], wring[:, s, 0:n], [B_slot[s]], [B_scr[k]], ch_store[s])
            else:
                P.dma("sp", wring[:, s, 0:n], scr_d[k, :, 0:n], [B_scr[k]], [B_slot[s]], ch_slot[s])

        def consume(t, k, hold=0):
            g = t * NPIECE + k
            while state["loaded"] < min(total_pieces, g + NSLOT - hold):
                load_piece(state["loaded"])
                state["loaded"] += 1
            s = g % NSLOT
            return wring[:, s, :], B_slot[s]

        P.dma("sp", cpar[:], cpar_d, [], [B_const], ch_const)
        P.dma("sp", cf[:], cf_d, [], [B_const], ch_const)
        P.dma("sp", cb[:], cb_d, [], [B_const], ch_const)
        P.op("pool", "memset", dict(ap=vpad[:, 0], constant=0.0), [], [B_vpad[0]])
        P.op("pool", "memset", dict(ap=vpad[:, 1], constant=0.0), [], [B_vpad[1]])

        def load_tile(t):
            r0 = t * T
            for m in range(4):
                P.dma("sp", h_t[:, m, :], x_d[r0 + m * 128:r0 + (m + 1) * 128, :], [], [B_h[m]], ch_x[m])
            P.dma("sp", ptile[:], p_d[r0:r0 + T, :].rearrange("(m q) c -> q m c", q=128), [], [B_p], ch_p)
            pos0 = (t % TILES_PER_SEQ) * T
            P.dma("sp", cosS[:], cos_d[:, pos0:pos0 + T], [], [B_tab], ch_tab)
            P.dma("sp", ssinS[:], ssin_d[:, pos0:pos0 + T], [], [B_tab], ch_tab)

        def rsqrt_dve(x_ap, B_x, y_ap, B_y, t_ap, B_t, iters=2):
            xi = x_ap.bitcast(I32)
            yi = y_ap.bitcast(I32)
            P.op("dve", "tensor_scalar", dict(out=yi, in0=xi, scalar1=1, scalar2=None, op0=ALU.arith_shift_right),
                 [B_x], [B_y])
            P.op("dve", "tensor_scalar", dict(out=yi, in0=yi, scalar1=-1, scalar2=0x5f3759df, op0=ALU.mult, op1=ALU.add),
                 [B_y], [B_y])
            for _ in range(iters):
                P.op("dve", "scalar_tensor_tensor", dict(out=t_ap, in0=y_ap, scalar=-0.5, in1=y_ap, op0=ALU.mult,
                                                         op1=ALU.mult), [B_y], [B_t])
                P.op("dve", "tensor_tensor", dict(out=t_ap, in0=t_ap, in1=x_ap, op=ALU.mult), [B_t, B_x], [B_t])
                P.op("dve", "scalar_tensor_tensor", dict(out=y_ap, in0=t_ap, scalar=1.5, in1=y_ap, op0=ALU.add,
                                                         op1=ALU.mult), [B_t, B_y], [B_y])

        def rstd_from(ms_ap, B_ms, n, factor=1.0):
            rs, B_rs = next_st()
            tt_, B_tt = next_st()
            rsqrt_dve(ms_ap, B_ms, rs[:, 0:n], B_rs, tt_[:, 0:n], B_tt, iters=2)
            if factor != 1.0:
                rs2, B_rs2 = next_st()
                P.op("dve", "tensor_scalar", dict(out=rs2[:, 0:n], in0=rs[:, 0:n], scalar1=float(factor),
                                                  scalar2=None, op0=ALU.mult), [B_rs], [B_rs2])
                return rs2, B_rs2
            return rs, B_rs

        def norm_to_xT(gidx):
            ms, B_ms = next_st()
            for m in range(4):
                P.op("act", "activation", dict(out=junk[:], in_=h_t[:, m, :], func=AF.Square, scale=1.0 / 32.0,
                                               accum_out=ms[:, m:m + 1]), [B_h[m]], [B_ms])
            me, B_me = next_st()
            P.op("dve", "tensor_scalar", dict(out=me[:, 0:4], in0=ms[:, 0:4], scalar1=EPS, scalar2=None,
                                              op0=ALU.add), [B_ms], [B_me])
            rs, B_rs = rstd_from(me[:, 0:4], B_me, 4)
            for m in range(4):
                P.op("act", "activation", dict(out=xn[:, m, :], in_=h_t[:, m, :], func=AF.Copy, scale=rs[:, m:m + 1]),
                     [B_h[m], B_rs], [B_xn[m]])
            for dc in range(8):
                bk, B_bk = next_bank()
                bv = bk[:].bitcast(BF16)
                for m in range(4):
                    P.op("pe", "transpose", dict(out=bv[:, m * 128:(m + 1) * 128],
                                                 in_=xn[:, m, dc * 128:(dc + 1) * 128], identity=ident),
                         [B_xn[m], B_const], [B_bk])
                gcol = cpar[:, C_GPRE + 8 * gidx + dc:C_GPRE + 8 * gidx + dc + 1]
                if dc % 2 == 0:
                    P.op("act", "activation", dict(out=xT[:, dc, :], in_=bv[:, 0:512], func=AF.Copy, scale=gcol),
                         [B_bk, B_const], [B_xT[dc]])
                else:
                    P.op("dve", "tensor_scalar", dict(out=xT[:, dc, :], in0=bv[:, 0:512], scalar1=gcol,
                                                      scalar2=None, op0=ALU.mult), [B_bk, B_const], [B_xT[dc]])

        def aform(t, k, nchunk, col0, width=128):
            w, B_w = consume(t, k)
            wv = w.rearrange("p (c n) -> p c n", c=8)
            for ch in range(nchunk):
                bk, B_bk = next_bank()
                for dc in range(8):
                    P.op("pe", "matmul", dict(out=bk[:], lhsT=wv[:, dc, col0 + ch * width:col0 + (ch + 1) * width],
                                              rhs=xT[:, dc, :], start=(dc == 0), stop=(dc == 7)),
                         [B_w, B_xT[dc]], [B_bk])
                yield ch, bk, B_bk

        def post_update(gidx, factor, ss, B_ss, fb=None, B_fb=None, pre_scaled=False, out_fb=False):
            fb = fbuf if fb is None else fb
            B_fb = B_f if B_fb is None else B_fb
            me, B_me = next_st()
            ssv = ss.rearrange("p (m two) -> p m two", two=2)
            P.op("dve", "scalar_tensor_tensor", dict(out=me[:, 0:4], in0=ssv[:, :, 0], scalar=EPS, in1=ssv[:, :, 1],
                                                     op0=ALU.add, op1=ALU.add), [B_ss], [B_me])
            ck("post_a")
            rs, B_rs = rstd_from(me[:, 0:4], B_me, 4, factor)
            ck("post_b")
            gtab = cpar[:, C_GPOST + 1024 * gidx:C_GPOST + 1024 * (gidx + 1)]
            for m in range(4):
                if pre_scaled and out_fb:
                    P.op("dve", "scalar_tensor_tensor", dict(out=fb[:, m, :], in0=fb[:, m, :], scalar=rs[:, m:m + 1],
                                                             in1=h_t[:, m, :], op0=ALU.mult, op1=ALU.add),
                         [B_fb[m], B_rs, B_h[m]], [B_fb[m]])
                elif pre_scaled:
                    P.op("dve", "scalar_tensor_tensor", dict(out=h_t[:, m, :], in0=fb[:, m, :], scalar=rs[:, m:m + 1],
                                                             in1=h_t[:, m, :], op0=ALU.mult, op1=ALU.add),
                         [B_fb[m], B_rs, B_h[m]], [B_h[m]])
                else:
                    P.op("dve", "scalar_tensor_tensor", dict(out=fb[:, m, :], in0=fb[:, m, :], scalar=rs[:, m:m + 1],
                                                             in1=gtab, op0=ALU.mult, op1=ALU.mult),
                         [B_fb[m], B_rs, B_const], [B_fb[m]])
                    P.op("dve", "tensor_tensor", dict(out=h_t[:, m, :], in0=h_t[:, m, :], in1=fb[:, m, :], op=ALU.add),
                         [B_h[m], B_fb[m]], [B_h[m]])

        def bform_post(t, chunks, pieces, gidx, factor):
            nchunks = len(chunks)
            ss8 = stats[:, 56:64]
            B_ss = B_ss_f
            for dh in range(2):
                acc = [next_bank() for _ in range(4)]
                fc = 0
                for (k, n) in pieces[dh]:
                    w, B_w = consume(t, k)
                    for fcl in range(n):
                        ap, B_c = chunks[fc]
                        for m in range(4):
                            P.op("pe", "matmul", dict(out=acc[m][0][:], lhsT=ap[:, m * 128:(m + 1) * 128],
                                                      rhs=w[:, fcl * 512:(fcl + 1) * 512],
                                                      start=(fc == 0), stop=(fc == nchunks - 1)),
                                 [B_c, B_w], [acc[m][1]])
                        fc += 1
                gt_h = cpar[:, C_GPOST + 1024 * gidx + dh * 512:C_GPOST + 1024 * gidx + (dh + 1) * 512]
                for m in range(4):
                    bk, B_bk = acc[m]
                    fsl = fbuf[:, m, dh * 512:(dh + 1) * 512]
                    def _sq():
                        P.op("act", "activation", dict(out=junk[:, 0:512], in_=bk[:], func=AF.Square, scale=1.0 / 32.0,
                                                       accum_out=ss8[:, 2 * m + dh:2 * m + dh + 1]), [B_bk], [B_ss])

                    def _mul():
                        P.op("dve", "tensor_tensor", dict(out=fsl, in0=bk[:], in1=gt_h, op=ALU.mult),
                             [B_bk, B_const], [B_f[m]])
                    if m % 2 == 0:
                        _sq()
                        _mul()
                    else:
                        _mul()
                        _sq()
            post_update(gidx, factor, ss8, B_ss, pre_scaled=True)

        class _Stop(Exception):
            pass

        def ck(name):
            if debug == name:
                raise _Stop()

        def ffn(t, gidx, p_gu, p_dn):
            norm_to_xT(gidx)
            ck("norm%d" % gidx)
            for jj in range(11):
                w, B_w = consume(t, p_gu + jj)
                wv = w.rearrange("p (c n) -> p c n", c=8)
                for jl in range(2):
                    j = 2 * jj + jl
                    gb, B_gb = next_bank()
                    ub, B_ub = next_bank()
                    for (bk, B_bk, c0) in ((gb, B_gb, jl * 128), (ub, B_ub, 256 + jl * 128)):
                        for dc in range(8):
                            P.op("pe", "matmul", dict(out=bk[:], lhsT=wv[:, dc, c0:c0 + 128], rhs=xT[:, dc, :],
                                                      start=(dc == 0), stop=(dc == 7)), [B_w, B_xT[dc]], [B_bk])
                    P.op("act", "activation", dict(out=sgt[:, j % 2, :], in_=gb[:], func=AF.Silu),
                         [B_gb], [B_sg[j % 2]])
                    P.op("dve", "tensor_tensor", dict(out=hidT[:, j, :], in0=sgt[:, j % 2, :], in1=ub[:], op=ALU.mult),
                         [B_sg[j % 2], B_ub], [B_hid[j]])
            ck("gu%d" % gidx)
            chunks = [(hidT[:, j, :], B_hid[j]) for j in range(NFC)]
            pieces = [[(p_dn + dh * 3 + fb, min(8, NFC - 8 * fb)) for fb in range(3)] for dh in range(2)]
            bform_post(t, chunks, pieces, gidx, 0.5)

        def ln_feature_major(src_list, ones_ap, n_src, gb_cols, out_fn):
            raise NotImplementedError

        def mixer(t):
            first = (t % TILES_PER_SEQ == 0)
            norm_to_xT(1)
            if first:
                for c in range(4):
                    P.op("pool", "memset", dict(ap=cext[:, c, 0:30], constant=0.0), [], [B_cext[c]])
            else:
                for c in range(4):
                    P.op("pool", "tensor_copy", dict(out=cext[:, c, 0:30], in_=halo[:, c, :]),
                         [B_halo[c]], [B_cext[c]])
            for c, bk, B_bk in aform(t, P_WIN + 0, 4, 0):
                P.op("act", "activation", dict(out=cacc[:, c, :], in_=bk[:], func=AF.Copy, scale=0.5),
                     [B_bk], [B_cacc[c]])
            for c, bk, B_bk in aform(t, P_WIN + 1, 4, 0):
                P.op("act", "activation", dict(out=ctmp, in_=bk[:], func=AF.Tanh, scale=0.5), [B_bk], [B_ctmp])
                P.op("dve", "scalar_tensor_tensor", dict(out=cext[:, c, 30:542], in0=ctmp, scalar=1.0,
                                                         in1=cacc[:, c, :], op0=ALU.add, op1=ALU.mult),
                     [B_ctmp, B_cacc[c], B_cext[c]], [B_cext[c]])
            for c in range(4):
                P.op("pool", "tensor_copy", dict(out=halo[:, c, :], in_=cext[:, c, 512:542]), [B_cext[c]], [B_halo[c]])
            bgq = []

            def pump(n):
                for _ in range(min(n, len(bgq))):
                    bgq.pop(0)()

            def gen_dg(c, k0, b2):
                nk = min(8, KCONV - k0)
                wk = cpar[:, C_CONVW + c * KCONV + k0:C_CONVW + c * KCONV + k0 + nk]
                P.op("pool", "tensor_tensor", dict(out=dg[:, b2, 0:nk, :],
                                                   in0=ident.unsqueeze(1).to_broadcast([128, nk, 128]),
                                                   in1=wk.unsqueeze(2).to_broadcast([128, nk, 128]), op=ALU.mult),
                     [B_const], [B_dg[b2]])

            gen_dg(0, 0, 0)
            gen_dg(0, 8, 1)

            def conv_pe():
                di = 0
                for c in range(4):
                    bk, B_bk = next_bank()
                    for k0 in range(0, KCONV, 8):
                        nk = min(8, KCONV - k0)
                        b2 = di % 2
                        di += 1
                        if di > 2:
                            gen_dg(c, k0, b2)
                        for kk in range(nk):
                            k = k0 + kk
                            P.op("pe", "matmul", dict(out=bk[:], lhsT=dg[:, b2, kk, :], rhs=cext[:, c, k:k + 512],
                                                      start=(k == 0), stop=(k == KCONV - 1)),
                                 [B_dg[b2], B_cext[c]], [B_bk])
                    P.op("act", "activation", dict(out=cacc[:, c, :], in_=bk[:], func=AF.Identity,
                                                   bias=cpar[:, C_CONVB + c:C_CONVB + c + 1]),
                         [B_bk, B_const], [B_cacc[c]])

            ck("conv")
            for (pk, dstT, B_dst, scale, is_q) in ((P_WIN + 2, qT, B_qT, 0.125, True), (P_WIN + 3, kT, B_kT, 1.0, False)):
                if not is_q:
                    conv_pe()
                for c, bk, B_bk in aform(t, pk, 4, 0):
                    P.op("act", "activation", dict(out=qraw[:], in_=bk[:], func=AF.Copy, scale=scale), [B_bk], [B_qraw])
                    P.op("act", "activation", dict(out=t1[:], in_=bk[:], func=AF.Copy, scale=scale), [B_bk], [B_t1])
                    P.op("pool", "tensor_tensor", dict(out=t1[:], in0=t1[:], in1=cosS[:], op=ALU.mult),
                         [B_t1, B_tab], [B_t1])
                    sw, B_sw = next_bank()
                    P.op("pe", "matmul", dict(out=sw[:], lhsT=pm, rhs=qraw[:], start=True, stop=True),
                         [B_qraw, B_const], [B_sw])
                    P.op("dve", "tensor_tensor", dict(out=t2[:], in0=sw[:], in1=ssinS[:], op=ALU.mult),
                         [B_sw, B_tab], [B_t2])
                    P.op("pool", "tensor_tensor", dict(out=dstT[:, c, :], in0=t1[:], in1=t2[:], op=ALU.add),
                         [B_t1, B_t2], [B_dst[c]])
                    if is_q:
                        qdec_c = cf[:, F_QDEC + c * 128:F_QDEC + (c + 1) * 128]
                        P.op("pool", "tensor_tensor", dict(
                            out=qdT[:, c, :].rearrange("p (m i) -> p m i", m=4),
                            in0=qT[:, c, :].rearrange("p (m i) -> p m i", m=4),
                            in1=qdec_c.unsqueeze(1).to_broadcast([128, 4, 128]), op=ALU.mult),
                             [B_qT[c], B_const], [B_qdT[c]])
            ck("qk")
            w, B_w = consume(t, P_WIN + 4)
            wv = w.rearrange("p (c n) -> p c n", c=8)
            for m in range(4):
                bk, B_bk = next_bank()
                for dc in range(8):
                    P.op("pe", "matmul", dict(out=bk[:], lhsT=xT[:, dc, m * 128:(m + 1) * 128], rhs=wv[:, dc, :],
                                              start=(dc == 0), stop=(dc == 7)), [B_w, B_xT[dc]], [B_bk])
                P.op("act", "activation", dict(out=vpl[:, m, :], in_=bk[:], func=AF.Copy), [B_bk], [B_vpl[m]])
            for c, bk, B_bk in aform(t, P_WIN + 5, 4, 0):
                P.op("act", "activation", dict(out=sgr[:, c, :], in_=bk[:], func=AF.Silu), [B_bk], [B_sgr[c]])
                P.op("dve", "tensor_scalar", dict(out=sgrb[:, c, :], in0=sgr[:, c, :], scalar1=cpar[:, C_RGB + c:C_RGB + c + 1],
                                                  scalar2=None, op0=ALU.mult), [B_sgr[c], B_const], [B_sgrb[c]])
                P.op("dve", "tensor_scalar", dict(out=sgr[:, c, :], in0=sgr[:, c, :], scalar1=cpar[:, C_RGG + c:C_RGG + c + 1],
                                                  scalar2=None, op0=ALU.mult), [B_sgr[c], B_const], [B_sgr[c]])
            ck("vg")
            cl = {}

            def conv_ln1():
                for c in range(4):
                    P.op("act", "activation", dict(out=cb16[:, c, :], in_=cacc[:, c, :], func=AF.Copy),
                         [B_cacc[c]], [B_cb16[c]])
                mb, B_mb = banks[7], B_bank[7]
                cl["mb"] = (mb, B_mb)
                for c in range(4):
                    P.op("pe", "matmul", dict(out=mb[:], lhsT=o512, rhs=cb16[:, c, :], start=(c == 0), stop=(c == 3)),
                         [B_cb16[c], B_const], [B_mb])

            def conv_ln2():
                mb, B_mb = cl["mb"]
                for c in range(4):
                    P.op("dve", "tensor_tensor", dict(out=cacc[:, c, :], in0=cacc[:, c, :], in1=mb[:], op=ALU.subtract),
                         [B_cacc[c], B_mb], [B_cacc[c]])
                    P.op("act", "activation", dict(out=cb16[:, c, :], in_=cacc[:, c, :], func=AF.Square),
                         [B_cacc[c]], [B_cb16[c]])
                vb, B_vb = banks[7], B_bank[7]
                cl["vb"] = (vb, B_vb)
                for c in range(4):
                    P.op("pe", "matmul", dict(out=vb[:], lhsT=o512, rhs=cb16[:, c, :], start=(c == 0), stop=(c == 3)),
                         [B_cb16[c], B_const], [B_vb])

            def conv_ln3():
                vb, B_vb = cl["vb"]
                P.op("act", "activation", dict(out=ctmp, in_=vb[:], func=AF.Sqrt, bias=EPS), [B_vb], [B_ctmp])
                P.op("dve", "reciprocal", dict(out=ctmp, in_=ctmp), [B_ctmp], [B_ctmp])
                for c in range(4):
                    P.op("pool" if c % 2 else "dve", "tensor_tensor",
                         dict(out=cacc[:, c, :], in0=cacc[:, c, :], in1=ctmp, op=ALU.mult),
                         [B_cacc[c], B_ctmp], [B_cacc[c]])
                    P.op("act", "activation", dict(out=mixT[:, 4 + c, :], in_=cacc[:, c, :], func=AF.Silu,
                                                   scale=cpar[:, C_CLNG + c:C_CLNG + c + 1],
                                                   bias=cpar[:, C_CLNB + c:C_CLNB + c + 1]),
                         [B_cacc[c], B_const], [B_mix[4 + c]])

            ck("convln")
            if first:
                P.op("pool", "memset", dict(ap=R_t[:], constant=0.0), [], [B_R])
                P.op("pool", "memset", dict(ap=Rbf[:], constant=0.0), [], [B_Rbf])
            def A1(m):
                cs = slice(m * 128, (m + 1) * 128)
                i2 = m % 2
                kb, B_kb = next_bank()
                kbv = kb[:].bitcast(BF16)
                for pp in range(4):
                    P.op("pe", "transpose", dict(out=kbv[:, pp * 128:(pp + 1) * 128], in_=kT[:, pp, cs], identity=ident),
                         [B_kT[pp], B_const], [B_kb])
                P.op("dve", "tensor_tensor", dict(
                    out=kd[:, i2, :].rearrange("p (h d) -> p h d", h=8),
                    in0=kbv[:, 0:512].rearrange("p (h d) -> p h d", h=8),
                    in1=cf[:, F_KDEC:F_KDEC + 8].unsqueeze(2).to_broadcast([128, 8, 64]), op=ALU.mult),
                     [B_kb, B_const], [B_kd[i2]])
                vsrc = vpl[:, m, :].rearrange("p (a b e) -> p a b e", a=4, b=2)
                vdst = vpad[:, i2].rearrange("p (a b) e -> p a b e", a=4)
                P.op("pool", "tensor_copy", dict(out=vdst[:, :, 0, 0:64], in_=vsrc[:, :, 0, :]), [B_vpl[m]], [B_vpad[i2]])
                P.op("pool", "tensor_copy", dict(out=vdst[:, :, 1, 64:128], in_=vsrc[:, :, 1, :]), [B_vpl[m]], [B_vpad[i2]])
                sa, B_sa = next_bank()
                sbk, B_sbk = next_bank()
                for pp in range(4):
                    for hh, (bk, B_bk) in enumerate(((sa, B_sa), (sbk, B_sbk))):
                        rs_ = slice(hh * 64, (hh + 1) * 64)
                        P.op("pe", "matmul", dict(out=bk[:, pp * 128:(pp + 1) * 128], lhsT=kT[rs_, pp, cs],
                                                  rhs=qT[rs_, pp, cs], start=True, stop=True),
                             [B_kT[pp], B_qT[pp]], [B_bk])
                for hh, (bk, B_bk) in enumerate(((sa, B_sa), (sbk, B_sbk))):
                    P.op("dve", "tensor_tensor", dict(
                        out=sTm[:, i2, hh * 4:(hh + 1) * 4, :].rearrange("p a i -> p (a i)"),
                        in0=bk[:], in1=cf[:, F_MASK + hh * 512:F_MASK + (hh + 1) * 512], op=ALU.mult),
                         [B_bk, B_const], [B_sTm[i2]])

            obanks = {}

            def A2(m):
                cs = slice(m * 128, (m + 1) * 128)
                i2 = m % 2
                ob, B_ob = next_bank()
                obanks[m] = (ob, B_ob)
                for pp in range(4):
                    osl = ob[:, pp * 128:(pp + 1) * 128]
                    P.op("pe", "matmul", dict(out=osl, lhsT=vpad[:, i2, 2 * pp, :], rhs=sTm[:, i2, pp, :],
                                              start=True, stop=False), [B_vpad[i2], B_sTm[i2]], [B_ob])
                    P.op("pe", "matmul", dict(out=osl, lhsT=vpad[:, i2, 2 * pp + 1, :], rhs=sTm[:, i2, 4 + pp, :],
                                              start=False, stop=False), [B_vpad[i2], B_sTm[i2]], [B_ob])
                    P.op("pe", "matmul", dict(out=osl, lhsT=Rbf[:, pp * 128:(pp + 1) * 128], rhs=qdT[:, pp, cs],
                                              start=False, stop=True), [B_Rbf, B_qdT[pp]], [B_ob])
                kvb, B_kvb = next_bank()
                for pp in range(4):
                    P.op("pe", "matmul", dict(out=kvb[:, pp * 128:(pp + 1) * 128], lhsT=kd[:, i2, pp * 128:(pp + 1) * 128],
                                              rhs=vpl[:, m, pp * 128:(pp + 1) * 128], start=True, stop=True),
                         [B_kd[i2], B_vpl[m]], [B_kvb])
                P.op("dve", "tensor_tensor", dict(
                    out=kvt[:].rearrange("p (a e) -> p a e", a=4), in0=kvb[:].rearrange("p (a e) -> p a e", a=4),
                    in1=cf[:, F_BDM:F_BDM + 128].unsqueeze(1).to_broadcast([128, 4, 128]), op=ALU.mult),
                     [B_kvb, B_const], [B_kvt])
                P.op("pool", "tensor_tensor", dict(
                    out=R_t[:].rearrange("p (a e) -> p a e", a=4), in0=R_t[:].rearrange("p (a e) -> p a e", a=4),
                    in1=cf[:, F_CDEC:F_CDEC + 4].unsqueeze(2).to_broadcast([128, 4, 128]), op=ALU.mult),
                     [B_R, B_const], [B_R])
                P.op("pool", "tensor_tensor", dict(out=R_t[:], in0=R_t[:], in1=kvt[:], op=ALU.add), [B_R, B_kvt], [B_R])
                P.op("act", "activation", dict(out=Rbf[:], in_=R_t[:], func=AF.Copy), [B_R], [B_Rbf])

            def B1(m):
                cen, B_cen, csq, B_csq = cen2[:, m % 2, :], B_cen2[m % 2], csq2[:, m % 2, :], B_csq2[m % 2]
                ob, B_ob = obanks[m]
                P.op("act", "activation", dict(out=o_f[:], in_=ob[:], func=AF.Copy), [B_ob], [B_of])
                P.op("act", "activation", dict(out=o_bf[:], in_=ob[:], func=AF.Copy), [B_ob], [B_obf])
                mb, B_mb = next_bank()
                P.op("pe", "matmul", dict(out=mb[:], lhsT=bd64, rhs=o_bf[:], start=True, stop=True),
                     [B_obf, B_const], [B_mb])
                P.op("dve", "tensor_tensor", dict(out=cen[:], in0=o_f[:], in1=mb[:], op=ALU.subtract),
                     [B_of, B_mb], [B_cen])
                P.op("act", "activation", dict(out=csq[:], in_=cen[:], func=AF.Square), [B_cen], [B_csq])

            def B2(m):
                cen, B_cen, csq, B_csq = cen2[:, m % 2, :], B_cen2[m % 2], csq2[:, m % 2, :], B_csq2[m % 2]
                cs = slice(m * 128, (m + 1) * 128)
                vb, B_vb = next_bank()
                P.op("pe", "matmul", dict(out=vb[:], lhsT=bd64, rhs=csq[:], start=True, stop=True),
                     [B_csq, B_const], [B_vb])
                P.op("act", "activation", dict(out=vare[:], in_=vb[:], func=AF.Sqrt, bias=EPS), [B_vb], [B_vare])
                P.op("dve", "reciprocal", dict(out=rstdL[:], in_=vare[:]), [B_vare], [B_rstdL])
                P.op("dve", "tensor_tensor", dict(out=cen[:], in0=cen[:], in1=rstdL[:], op=ALU.mult),
                     [B_cen, B_rstdL], [B_cen])
                P.op("pool", "tensor_tensor", dict(out=zt[:].rearrange("p (a i) -> p a i", a=4),
                                                   in0=cen[:].rearrange("p (a i) -> p a i", a=4),
                                                   in1=sgr[:, :, cs], op=ALU.mult), [B_cen] + B_sgr, [B_zt])
                P.op("pool", "tensor_tensor", dict(out=mixT[:, 0:4, cs], in0=zt[:].rearrange("p (a i) -> p a i", a=4),
                                                   in1=sgrb[:, :, cs], op=ALU.add), [B_zt] + B_sgrb, B_mix[0:4])

            conv_ln1()
            A1(0)
            A2(0)
            B1(0)
            for m in range(1, 4):
                A1(m)
                A2(m)
                B1(m)
                B2(m - 1)
                if m == 1:
                    conv_ln2()
            B2(3)
            conv_ln3()
            pump(len(bgq))
            ck("ret")
            chunks = [(mixT[:, c, :], B_mix[c]) for c in range(8)]
            bform_post(t, chunks, [[(P_WOUT + 0, 8)], [(P_WOUT + 1, 8)]], 1, 1.0)

        def ple(t):
            norm_to_xT(3)
            P.op("pool", "tensor_copy", dict(out=p_bf[:], in_=ptile[:]), [B_p], [B_pbf])
            bk, B_bk = next_bank()
            bv = bk[:].bitcast(BF16)
            for dc in range(2):
                for m in range(4):
                    P.op("pe", "transpose", dict(out=bv[:, dc * 512 + m * 128:dc * 512 + (m + 1) * 128],
                                                 in_=p_bf[:, m, dc * 128:(dc + 1) * 128], identity=ident),
                         [B_pbf, B_const], [B_bk])
            P.op("act", "activation", dict(out=pT[:].rearrange("p c t -> p (c t)"), in_=bv[:, 0:1024], func=AF.Copy),
                 [B_bk], [B_pT])
            we, B_we = consume(t, P_PLEW)
            wev = we[:, 0:2048].rearrange("p (c n) -> p c n", c=2)
            ss8 = stats[:, 48:56]
            B_ss = B_ss_p
            for dh in range(2):
                wg, B_wg = consume(t, P_GATE + dh, hold=1 + dh)
                wgv = wg.rearrange("p (c n) -> p c n", c=8)
                for m in range(4):
                    ms_ = slice(m * 128, (m + 1) * 128)
                    gbk, B_gbk = next_bank()
                    ebk, B_ebk = next_bank()
                    for dc in range(8):
                        P.op("pe", "matmul", dict(out=gbk[:], lhsT=xT[:, dc, ms_], rhs=wgv[:, dc, :],
                                                  start=(dc == 0), stop=(dc == 7)), [B_xT[dc], B_wg], [B_gbk])
                    for dc in range(2):
                        P.op("pe", "matmul", dict(out=ebk[:], lhsT=pT[:, dc, ms_], rhs=wev[:, dc, dh * 512:(dh + 1) * 512],
                                                  start=(dc == 0), stop=(dc == 1)), [B_pT, B_we], [B_ebk])
                    P.op("act", "activation", dict(out=sgt[:, m % 2, :], in_=gbk[:], func=AF.Tanh, scale=0.5),
                         [B_gbk], [B_sg[m % 2]])
                    fsl = fple[:, m, dh * 512:(dh + 1) * 512]
                    P.op("dve", "scalar_tensor_tensor", dict(out=fsl, in0=sgt[:, m % 2, :], scalar=1.0, in1=ebk[:],
                                                             op0=ALU.add, op1=ALU.mult), [B_sg[m % 2], B_ebk], [B_fp[m]])
                    P.op("act", "activation", dict(out=junk[:, 0:512], in_=fsl, func=AF.Square, scale=1.0 / 64.0,
                                                   accum_out=ss8[:, 2 * m + dh:2 * m + dh + 1]), [B_fp[m]], [B_ss])
                    P.op("dve", "tensor_tensor", dict(
                        out=fsl, in0=fsl, in1=cpar[:, C_GPOST + 3 * 1024 + dh * 512:C_GPOST + 3 * 1024 + (dh + 1) * 512],
                        op=ALU.mult), [B_fp[m], B_const], [B_fp[m]])
            post_update(3, 0.5, ss8, B_ss, fple, B_fp, pre_scaled=True, out_fb=True)
            state["final_in_fple"] = True

        def store_tile(t):
            r0 = t * T
            for m in range(4):
                if state.get("final_in_fple"):
                    P.dma("sp", out_d[r0 + m * 128:r0 + (m + 1) * 128, :], fple[:, m, :], [B_fp[m]], [B_out[m]], ch_o[m])
                else:
                    P.dma("sp", out_d[r0 + m * 128:r0 + (m + 1) * 128, :], h_t[:, m, :], [B_h[m]], [B_out[m]], ch_o[m])
            state["final_in_fple"] = False

        for t in range(ntiles):
            load_tile(t)
            try:
                ck("load")
                ffn(t, 0, P_GU1, P_DN1)
                ck("ffn1")
                mixer(t)
                ck("mixer")
                ffn(t, 2, P_GU2, P_DN2)
                ck("ffn2")
                ple(t)
            except _Stop:
                pass
            store_tile(t)
        P.wait_all("sp", B_out + B_scr)
        nwait = P.emit(stack)
        build_program.info = dict(nops=len(P.ops), nwait=nwait, per_eng=dict(P.nseq))
    return nc


_CACHE = {}


def _host_inputs(inp):
    cf, cosT, ssinT, cb = _const_tables()
    wp = _layout_weights(inp)
    cp = _layout_params(inp)
    x = np.ascontiguousarray(np.asarray(inp["x"], np.float32)).reshape(NCORES, TOK_CORE, D)
    p = np.ascontiguousarray(np.asarray(inp["p"], np.float32)[0]).reshape(NCORES, TOK_CORE, 256)
    maps = []
    for c in range(NCORES):
        maps.append(dict(x=x[c], p=p[c], wp=wp, cpar=cp, cf=cf, cosT=cosT, ssinT=ssinT, cb=cb))
    return maps


def kernel(**inputs):
    inp = {k: np.asarray(v) for k, v in inputs.items()}
    if "nc" not in _CACHE:
        _CACHE["nc"] = build_program(NTILES)
    nc = _CACHE["nc"]
    maps = _host_inputs(inp)
    res = run_bass_kernel_spmd(nc, maps, core_ids=list(range(NCORES)))
    out = np.stack([np.asarray(r["out"], np.float32) for r in res.results], axis=0)
    return out.reshape(BATCH, SEQ, D)
```

```python
import contextlib
import numpy as np
import ml_dtypes
import concourse.bass as bass
import concourse.mybir as mybir
from concourse.bass_utils import run_bass_kernel_spmd

F32 = mybir.dt.float32
BF16 = mybir.dt.bfloat16
I32 = mybir.dt.int32
AF = mybir.ActivationFunctionType
ALU = mybir.AluOpType

D = 1024
DFF = 2816
NFC = 22
SEQ = 4096
BATCH = 16
NCORES = 8
TOK_CORE = BATCH * SEQ // NCORES
T = 512
NTILES = TOK_CORE // T
TILES_PER_SEQ = SEQ // T
EPS = 1e-6
NSLOT = 4
PIECE = 4096
NPIECE = 45
KCONV = 31

P_GU1 = 0
P_DN1 = 11
P_WIN = 17
P_WOUT = 23
P_GU2 = 25
P_DN2 = 36
P_PLEW = 42
P_GATE = 43
WIN_ORDER = [4, 5, 0, 1, 2, 3]

C_GPRE = 0
C_GPOST = 32
C_CONVW = C_GPOST + 4096
C_CONVB = C_CONVW + 124
C_CLNG = C_CONVB + 4
C_CLNB = C_CLNG + 4
C_RGG = C_CLNB + 4
C_RGB = C_RGG + 4
NCPAR = C_RGB + 4

F_QDEC = 0
F_MASK = 512
F_BDM = 1536
F_KDEC = 1664
F_CDEC = 1672
F_MHALF = 1676
NCF = 1680


class Buf:
    __slots__ = ("name", "w", "r", "over", "excl")

    def __init__(self, name, excl=False):
        self.name = name
        self.w = {}
        self.r = {}
        self.over = []
        self.excl = excl


def overlap(a_list, b_list):
    for a in a_list:
        for b in b_list:
            a.over.append(b)
            b.over.append(a)


class Prog:
    ENG = ("pe", "act", "dve", "pool", "sp")

    def __init__(self, nc):
        self.nc = nc
        self.engs = {"pe": nc.tensor, "act": nc.scalar, "dve": nc.vector, "pool": nc.gpsimd, "sp": nc.sync}
        self.ops = []
        self.nseq = {e: 0 for e in self.ENG}
        self.seen = {e: {} for e in self.ENG}
        self.nchan = 0
        self.chan_count = []

    def chan(self):
        self.chan_count.append(0)
        self.nchan += 1
        return self.nchan - 1

    def _deps(self, eng, reads, writes):
        deps = {}
        seen = self.seen[eng]

        def need(ev, hazard):
            kind, key, val = ev
            if kind == "c" and key == eng and eng == "pe":
                return
            k = (kind, key)
            if seen.get(k, 0) >= val:
                return
            if deps.get(k, 0) < val:
                deps[k] = val

        for b in reads:
            for ev in b.w.values():
                need(ev, "RAW")
            if b.excl:
                for (kk, key), ev in b.r.items():
                    if not (kk == "c" and key == eng):
                        need(ev, "RAR")
        for b in writes:
            for bb in [b] + b.over:
                for ev in bb.w.values():
                    need(ev, "WAW")
                for ev in bb.r.values():
                    need(ev, "WAR")
        for k, v in deps.items():
            seen[k] = v
        return deps

    def op(self, eng, name, kw, reads=(), writes=()):
        deps = self._deps(eng, reads, writes)
        self.nseq[eng] += 1
        seq = self.nseq[eng]
        ev = ("c", eng, seq)
        for b in reads:
            b.r[("c", eng)] = ev
        for b in writes:
            b.w = {("c", eng): ev}
            b.r = {}
        self.ops.append((eng, name, kw, deps, seq, None))

    def dma(self, q, out, in_, reads, writes, chan):
        deps = self._deps(q, reads, writes)
        self.nseq[q] += 1
        seq = self.nseq[q]
        self.chan_count[chan] += 16
        ev = ("d", chan, self.chan_count[chan])
        for b in reads:
            b.r[("d", chan)] = ev
        for b in writes:
            b.w = {("d", chan): ev}
            b.r = {}
        self.ops.append((q, "dma_start", dict(out=out, in_=in_), deps, seq, chan))

    def wait_all(self, eng, bufs):
        deps = self._deps(eng, bufs, bufs)
        self.nseq[eng] += 1
        self.ops.append((eng, None, None, deps, self.nseq[eng], None))

    def emit(self, stack):
        nc = self.nc
        esem = {e: stack.enter_context(nc.semaphore("s_" + e)) for e in self.ENG}
        csem = [stack.enter_context(nc.semaphore("c_%d" % i)) for i in range(self.nchan)]
        needed = {e: set() for e in self.ENG}
        for (_, _, _, deps, _, _) in self.ops:
            for (kind, key), val in deps.items():
                if kind == "c":
                    needed[key].add(val)
        cnt = {}
        for e in self.ENG:
            cnt[e] = {s: i + 1 for i, s in enumerate(sorted(needed[e]))}
        nwait = 0
        for (eng, name, kw, deps, seq, chan) in self.ops:
            E = self.engs[eng]
            for (kind, key), val in deps.items():
                if kind == "c":
                    E.wait_ge(esem[key], cnt[key][val])
                else:
                    E.wait_ge(csem[key], val)
                nwait += 1
            if name is None:
                continue
            ins = getattr(E, name)(**kw)
            if chan is not None:
                ins.then_inc(csem[chan], 16)
            elif seq in cnt[eng]:
                ins.then_inc(esem[eng], 1)
        return nwait


def _const_tables():
    lg = np.log1p(-np.exp2(-5.0 - np.arange(8, dtype=np.float32))).astype(np.float32)
    pos = np.arange(128, dtype=np.float32)
    diff = pos[None, :] - pos[:, None]
    mask = np.where(diff[:, None, :] >= 0,
                    np.exp(lg[None, :, None] * np.maximum(diff, 0.0)[:, None, :]), 0.0).astype(np.float32)
    mask = mask[:, [0, 2, 4, 6, 1, 3, 5, 7], :]
    kdec = np.exp(lg[None, :] * (127.0 - pos)[:, None]).astype(np.float32)
    qdec_h = np.exp(lg[:, None] * (pos + 1.0)[None, :]).astype(np.float32)
    cdec_h = np.exp(lg * 128.0).astype(np.float32)
    qdec = np.zeros((128, 4, 128), np.float32)
    cdec = np.zeros((128, 4), np.float32)
    for p in range(4):
        for hh in range(2):
            qdec[hh * 64:(hh + 1) * 64, p, :] = qdec_h[2 * p + hh][None, :]
            cdec[hh * 64:(hh + 1) * 64, p] = cdec_h[2 * p + hh]
    bdm = np.zeros((128, 128), np.float32)
    bdm[:64, :64] = 1.0
    bdm[64:, 64:] = 1.0
    cf = np.zeros((128, NCF), np.float32)
    cf[:, F_QDEC:F_QDEC + 512] = qdec.reshape(128, 512)
    cf[:, F_MASK:F_MASK + 1024] = mask.reshape(128, 1024)
    cf[:, F_BDM:F_BDM + 128] = bdm
    cf[:, F_KDEC:F_KDEC + 8] = kdec
    cf[:, F_CDEC:F_CDEC + 4] = cdec
    cf[:, F_MHALF] = -0.5
    inv_freq = (10000.0 ** (-np.arange(0, 64, 2, dtype=np.float32) / 64.0)).astype(np.float32)
    ang = (np.arange(SEQ, dtype=np.float32)[:, None] * inv_freq[None, :]).astype(np.float32)
    cos = np.cos(ang).astype(np.float32)
    sin = np.sin(ang).astype(np.float32)
    cos64 = np.concatenate([cos, cos], axis=1).T
    ssin64 = np.concatenate([-sin, sin], axis=1).T
    cosT = np.ascontiguousarray(np.concatenate([cos64, cos64], axis=0))
    ssinT = np.ascontiguousarray(np.concatenate([ssin64, ssin64], axis=0))
    ident = np.eye(128, dtype=np.float32)
    pm = np.zeros((128, 128), np.float32)
    for f in range(128):
        pm[f ^ 32, f] = 1.0
    bd64 = bdm / 64.0
    o512 = np.full((128, 128), 1.0 / 512.0, np.float32)
    cb = np.concatenate([ident, pm, bd64, o512], axis=1).astype(ml_dtypes.bfloat16)
    return cf, cosT, ssinT, cb


def _layout_weights(inp):
    wp = np.zeros((NPIECE, 128, PIECE), np.float32)

    def rows(w):
        return w.reshape(-1, 128, w.shape[1]).transpose(1, 0, 2)

    for base_gu, base_dn, wgu, wdn in ((P_GU1, P_DN1, inp["ffn1_w_gu"][0], inp["ffn1_w_down"][0]),
                                        (P_GU2, P_DN2, inp["ffn2_w_gu"][0], inp["ffn2_w_down"][0])):
        r = rows(wgu)
        for jj in range(11):
            pc = wp[base_gu + jj].reshape(128, 8, 512)
            pc[:, :, 0:256] = r[:, :, jj * 256:(jj + 1) * 256]
            pc[:, :, 256:512] = r[:, :, DFF + jj * 256:DFF + (jj + 1) * 256]
        r = rows(wdn)
        for dh in range(2):
            for fb in range(3):
                n = min(8, NFC - fb * 8)
                pc = wp[base_dn + dh * 3 + fb].reshape(128, 8, 512)
                pc[:, 0:n, :] = r[:, fb * 8:fb * 8 + n, dh * 512:(dh + 1) * 512]
    r = rows(inp["w_in"][0])
    for i, cbk in enumerate(WIN_ORDER):
        wp[P_WIN + i].reshape(128, 8, 512)[:] = r[:, :, cbk * 512:(cbk + 1) * 512]
    r = rows(inp["w_out"][0])
    for dh in range(2):
        wp[P_WOUT + dh].reshape(128, 8, 512)[:] = r[:, :, dh * 512:(dh + 1) * 512]
    r = rows(inp["ple_w"][0])
    wp[P_PLEW][:, 0:2048] = r.reshape(128, 2048)
    r = rows(inp["ple_gate_w"][0])
    for dh in range(2):
        wp[P_GATE + dh].reshape(128, 8, 512)[:] = r[:, :, dh * 512:(dh + 1) * 512]
    return wp


def _layout_params(inp):
    cp = np.zeros((128, NCPAR), np.float32)

    def col(v, n):
        return v.reshape(n, 128).T

    for i, k in enumerate(("ffn1_pre_g", "mix_pre_g", "ffn2_pre_g", "ple_gate_norm_g")):
        cp[:, C_GPRE + 8 * i:C_GPRE + 8 * (i + 1)] = col(inp[k][0], 8)
    for i, k in enumerate(("ffn1_post_g", "mix_post_g", "ffn2_post_g", "ple_post_g")):
        cp[:, C_GPOST + 1024 * i:C_GPOST + 1024 * (i + 1)] = np.broadcast_to(inp[k][0][None, :], (128, 1024))
    cw = inp["conv_w"][0]
    cp[:, C_CONVW:C_CONVW + 124] = cw.T.reshape(4, 128, KCONV).transpose(1, 0, 2).reshape(128, 124)
    cp[:, C_CONVB:C_CONVB + 4] = col(inp["conv_b"][0], 4)
    cp[:, C_CLNG:C_CLNG + 4] = col(inp["conv_ln_g"][0], 4)
    cp[:, C_CLNB:C_CLNB + 4] = col(inp["conv_ln_b"][0], 4)
    cp[:, C_RGG:C_RGG + 4] = col(inp["ret_gn_g"][0], 4)
    cp[:, C_RGB:C_RGB + 4] = col(inp["ret_gn_b"][0], 4)
    return cp


def build_program(ntiles=NTILES, debug=None):
    nc = bass.Bass("TRN2", target_bir_lowering=False)
    x_d = nc.dram_tensor("x", [TOK_CORE, D], F32, kind="ExternalInput").ap()
    p_d = nc.dram_tensor("p", [TOK_CORE, 256], F32, kind="ExternalInput").ap()
    wp_d = nc.dram_tensor("wp", [NPIECE, 128, PIECE], F32, kind="ExternalInput").ap()
    cpar_d = nc.dram_tensor("cpar", [128, NCPAR], F32, kind="ExternalInput").ap()
    cf_d = nc.dram_tensor("cf", [128, NCF], F32, kind="ExternalInput").ap()
    cos_d = nc.dram_tensor("cosT", [128, SEQ], F32, kind="ExternalInput").ap()
    ssin_d = nc.dram_tensor("ssinT", [128, SEQ], F32, kind="ExternalInput").ap()
    cb_d = nc.dram_tensor("cb", [128, 512], BF16, kind="ExternalInput").ap()
    out_d = nc.dram_tensor("out", [TOK_CORE, D], F32, kind="ExternalOutput").ap()
    scr_d = nc.dram_tensor("wscr", [NPIECE, 128, PIECE], BF16, kind="Internal").ap()

    stack = contextlib.ExitStack()
    with stack:
        def sb(name, shape, dt):
            return stack.enter_context(nc.sbuf_tensor(name, shape, dt))

        P = Prog(nc)

        h_t = sb("h", [128, 4, D], F32)
        arenaA = sb("arenaA", [128, 4096], F32)
        fbuf = arenaA[:].rearrange("p (m d) -> p m d", m=4)
        xn = arenaA[:, 0:2048].bitcast(BF16).rearrange("p (m d) -> p m d", m=4)
        xT = arenaA[:, 2048:4096].bitcast(BF16).rearrange("p (c t) -> p c t", c=8)
        arenaB = sb("arenaB", [128, NFC * 512], BF16)
        hidT = arenaB[:].rearrange("p (j t) -> p j t", j=NFC)
        cext = arenaB[:, 0:2168].rearrange("p (c t) -> p c t", c=4)
        cacc = arenaB[:, 4336:8432].bitcast(F32).rearrange("p (c t) -> p c t", c=4)
        ctmp = arenaB[:, 8432:9456].bitcast(F32)
        wring = sb("wring", [128, NSLOT, PIECE], BF16)
        ptile = sb("ptile", [128, 4, 256], F32)
        p_bf = sb("p_bf", [128, 4, 256], BF16)
        pT = sb("pT", [128, 2, 512], BF16)
        cpar = sb("cpar_s", [128, NCPAR], F32)
        cf = sb("cf_s", [128, NCF], F32)
        cb = sb("cb_s", [128, 512], BF16)
        cosS = sb("cosS", [128, 512], F32)
        ssinS = sb("ssinS", [128, 512], F32)
        sgt = sb("sgt", [128, 2, 512], F32)
        junk = sb("junk", [128, 1024], BF16)
        stats = sb("stats", [128, 64], F32)
        qraw = sb("qraw", [128, 512], BF16)
        t1 = sb("t1", [128, 512], F32)
        t2 = sb("t2", [128, 512], F32)
        qT = sb("qT", [128, 4, 512], BF16)
        qdT = sb("qdT", [128, 4, 512], BF16)
        kT = sb("kT", [128, 4, 512], BF16)
        kd = sb("kd", [128, 2, 512], BF16)
        vpl = sb("vpl", [128, 4, 512], BF16)
        vpad = sb("vpad", [128, 2, 8, 128], BF16)
        sgr = sb("sgr", [128, 4, 512], BF16)
        sgrb = sb("sgrb", [128, 4, 512], BF16)
        sTm = sb("sTm", [128, 2, 8, 128], BF16)
        R_t = sb("R", [128, 512], F32)
        Rbf = sb("Rbf", [128, 512], BF16)
        kvt = sb("kvt", [128, 512], F32)
        o_f = sb("o_f", [128, 512], F32)
        o_bf = sb("o_bf", [128, 512], BF16)
        cen2 = sb("cen", [128, 2, 512], F32)
        csq2 = sb("csq", [128, 2, 512], BF16)
        vare = sb("vare", [128, 512], F32)
        rstdL = sb("rstdL", [128, 512], F32)
        zt = sb("zt", [128, 512], F32)
        mixT = sb("mixT", [128, 8, 512], BF16)
        cb16 = sb("cb16", [128, 4, 512], BF16)
        halo = sb("halo", [128, 4, 30], BF16)
        dg = sb("dg", [128, 2, 8, 128], BF16)
        fple = arenaB[:, 0:8192].bitcast(F32).rearrange("p (m d) -> p m d", m=4)
        banks = [stack.enter_context(nc.psum_tensor("bank%d" % i, [128, 512], F32)) for i in range(8)]

        ident = cb[:, 0:128]
        pm = cb[:, 128:256]
        bd64 = cb[:, 256:384]
        o512 = cb[:, 384:512]
        mhalf = cf[:, F_MHALF:F_MHALF + 1]

        B_h = [Buf("h%d" % m) for m in range(4)]
        B_xn = [Buf("xn%d" % m) for m in range(4)]
        B_xT = [Buf("xT%d" % c) for c in range(8)]
        B_f = [Buf("f%d" % m) for m in range(4)]
        overlap(B_f, B_xn + B_xT)
        B_hid = [Buf("hid%d" % j) for j in range(NFC)]
        B_cext = [Buf("cext%d" % c) for c in range(4)]
        B_cacc = [Buf("cacc%d" % c) for c in range(4)]
        B_ctmp = Buf("ctmp")
        overlap(B_hid, B_cext + B_cacc + [B_ctmp])
        B_fp = [Buf("fp%d" % m) for m in range(4)]
        overlap(B_fp, B_hid + B_cext + B_cacc + [B_ctmp])
        B_halo = [Buf("halo%d" % c) for c in range(4)]
        B_dg = [Buf("dg0"), Buf("dg1")]
        B_ss_f, B_ss_p = Buf("ss8f"), Buf("ss8p")
        B_slot = [Buf("slot%d" % s) for s in range(NSLOT)]
        B_scr = [Buf("scr%d" % k) for k in range(NPIECE)]
        B_bank = [Buf("bank%d" % i, excl=True) for i in range(8)]
        B_const = Buf("const")
        B_tab = Buf("tab")
        B_p = Buf("ptile")
        B_pbf = Buf("pbf")
        B_pT = Buf("pT")
        B_sg = [Buf("sg0"), Buf("sg1")]
        B_st = [Buf("st%d" % i) for i in range(16)]
        B_qraw, B_t1, B_t2 = Buf("qraw"), Buf("t1"), Buf("t2")
        B_qT = [Buf("qT%d" % c) for c in range(4)]
        B_qdT = [Buf("qdT%d" % c) for c in range(4)]
        B_kT = [Buf("kT%d" % c) for c in range(4)]
        B_kd = [Buf("kd0"), Buf("kd1")]
        B_vpl = [Buf("vpl%d" % m) for m in range(4)]
        B_vpad = [Buf("vpad0"), Buf("vpad1")]
        B_sgr = [Buf("sgr%d" % c) for c in range(4)]
        B_sgrb = [Buf("sgrb%d" % c) for c in range(4)]
        B_sTm = [Buf("sTm0"), Buf("sTm1")]
        B_R, B_Rbf, B_kvt = Buf("R"), Buf("Rbf"), Buf("kvt")
        B_of, B_obf, B_vare, B_rstdL, B_zt = (Buf(n) for n in ("of", "obf", "vare", "rstdL", "zt"))
        B_cen2 = [Buf("cen0"), Buf("cen1")]
        B_csq2 = [Buf("csq0"), Buf("csq1")]
        B_mix = [Buf("mix%d" % c) for c in range(8)]
        B_cb16 = [Buf("cb16_%d" % c) for c in range(4)]
        B_out = [Buf("out%d" % m) for m in range(4)]

        ch_slot = [P.chan() for _ in range(NSLOT)]
        ch_store = [P.chan() for _ in range(NSLOT)]
        ch_cast = [P.chan() for _ in range(NSLOT)]
        ch_x = [P.chan() for _ in range(4)]
        ch_o = [P.chan() for _ in range(4)]
        ch_p = P.chan()
        ch_tab = P.chan()
        ch_const = P.chan()

        state = {"bank": 0, "loaded": 0, "st": 0}

        def next_bank():
            i = state["bank"]
            state["bank"] = (i + 1) % 7
            return banks[i], B_bank[i]

        def next_st():
            i = state["st"]
            state["st"] = (i + 1) % 16
            return stats[:, 4 * i:4 * i + 4], B_st[i]

        total_pieces = ntiles * NPIECE

        def piece_len(k):
            if k == P_PLEW:
                return 2048
            if k in (P_DN1 + 2, P_DN1 + 5, P_DN2 + 2, P_DN2 + 5):
                return 6 * 512
            return PIECE

        def load_piece(g):
            t, k = divmod(g, NPIECE)
            s = g % NSLOT
            n = piece_len(k)
            if t == 0:
                P.dma("pool", wring[:, s, 0:n], wp_d[k, :, 0:n], [], [B_slot[s]], ch_cast[s])
                P.dma("sp", scr_d[k, :, 0:n], wring[:, s, 0:n], [B_slot[s]], [B_scr[k]], ch_store[s])
            else:
                P.dma("sp", wring[:, s, 0:n], scr_d[k, :, 0:n], [B_scr[k]], [B_slot[s]], ch_slot[s])

        def consume(t, k, hold=0):
            g = t * NPIECE + k
            while state["loaded"] < min(total_pieces, g + NSLOT - hold):
                load_piece(state["loaded"])
                state["loaded"] += 1
            s = g % NSLOT
            return wring[:, s, :], B_slot[s]

        P.dma("sp", cpar[:], cpar_d, [], [B_const], ch_const)
        P.dma("sp", cf[:], cf_d, [], [B_const], ch_const)
        P.dma("sp", cb[:], cb_d, [], [B_const], ch_const)
        P.op("pool", "memset", dict(ap=vpad[:, 0], constant=0.0), [], [B_vpad[0]])
        P.op("pool", "memset", dict(ap=vpad[:, 1], constant=0.0), [], [B_vpad[1]])

        def load_tile(t):
            r0 = t * T
            for m in range(4):
                P.dma("sp", h_t[:, m, :], x_d[r0 + m * 128:r0 + (m + 1) * 128, :], [], [B_h[m]], ch_x[m])
            P.dma("sp", ptile[:], p_d[r0:r0 + T, :].rearrange("(m q) c -> q m c", q=128), [], [B_p], ch_p)
            pos0 = (t % TILES_PER_SEQ) * T
            P.dma("sp", cosS[:], cos_d[:, pos0:pos0 + T], [], [B_tab], ch_tab)
            P.dma("sp", ssinS[:], ssin_d[:, pos0:pos0 + T], [], [B_tab], ch_tab)

        def rsqrt_dve(x_ap, B_x, y_ap, B_y, t_ap, B_t, iters=2):
            xi = x_ap.bitcast(I32)
            yi = y_ap.bitcast(I32)
            P.op("dve", "tensor_scalar", dict(out=yi, in0=xi, scalar1=1, scalar2=None, op0=ALU.arith_shift_right),
                 [B_x], [B_y])
            P.op("dve", "tensor_scalar", dict(out=yi, in0=yi, scalar1=-1, scalar2=0x5f3759df, op0=ALU.mult, op1=ALU.add),
                 [B_y], [B_y])
            for _ in range(iters):
                P.op("dve", "scalar_tensor_tensor", dict(out=t_ap, in0=y_ap, scalar=-0.5, in1=y_ap, op0=ALU.mult,
                                                         op1=ALU.mult), [B_y], [B_t])
                P.op("dve", "tensor_tensor", dict(out=t_ap, in0=t_ap, in1=x_ap, op=ALU.mult), [B_t, B_x], [B_t])
                P.op("dve", "scalar_tensor_tensor", dict(out=y_ap, in0=t_ap, scalar=1.5, in1=y_ap, op0=ALU.add,
                                                         op1=ALU.mult), [B_t, B_y], [B_y])

        def rstd_from(ms_ap, B_ms, n, factor=1.0):
            rs, B_rs = next_st()
            tt_, B_tt = next_st()
            rsqrt_dve(ms_ap, B_ms, rs[:, 0:n], B_rs, tt_[:, 0:n], B_tt, iters=2)
            if factor != 1.0:
                rs2, B_rs2 = next_st()
                P.op("dve", "tensor_scalar", dict(out=rs2[:, 0:n], in0=rs[:, 0:n], scalar1=float(factor),
                                                  scalar2=None, op0=ALU.mult), [B_rs], [B_rs2])
                return rs2, B_rs2
            return rs, B_rs

        def norm_to_xT(gidx):
            ms, B_ms = next_st()
            for m in range(4):
                P.op("act", "activation", dict(out=junk[:], in_=h_t[:, m, :], func=AF.Square, scale=1.0 / 32.0,
                                               accum_out=ms[:, m:m + 1]), [B_h[m]], [B_ms])
            me, B_me = next_st()
            P.op("dve", "tensor_scalar", dict(out=me[:, 0:4], in0=ms[:, 0:4], scalar1=EPS, scalar2=None,
                                              op0=ALU.add), [B_ms], [B_me])
            rs, B_rs = rstd_from(me[:, 0:4], B_me, 4)
            for m in range(4):
                P.op("act", "activation", dict(out=xn[:, m, :], in_=h_t[:, m, :], func=AF.Copy, scale=rs[:, m:m + 1]),
                     [B_h[m], B_rs], [B_xn[m]])
            for dc in range(8):
                bk, B_bk = next_bank()
                bv = bk[:].bitcast(BF16)
                for m in range(4):
                    P.op("pe", "transpose", dict(out=bv[:, m * 128:(m + 1) * 128],
                                                 in_=xn[:, m, dc * 128:(dc + 1) * 128], identity=ident),
                         [B_xn[m], B_const], [B_bk])
                gcol = cpar[:, C_GPRE + 8 * gidx + dc:C_GPRE + 8 * gidx + dc + 1]
                if dc % 2 == 0:
                    P.op("act", "activation", dict(out=xT[:, dc, :], in_=bv[:, 0:512], func=AF.Copy, scale=gcol),
                         [B_bk, B_const], [B_xT[dc]])
                else:
                    P.op("dve", "tensor_scalar", dict(out=xT[:, dc, :], in0=bv[:, 0:512], scalar1=gcol,
                                                      scalar2=None, op0=ALU.mult), [B_bk, B_const], [B_xT[dc]])

        def aform(t, k, nchunk, col0, width=128):
            w, B_w = consume(t, k)
            wv = w.rearrange("p (c n) -> p c n", c=8)
            for ch in range(nchunk):
                bk, B_bk = next_bank()
                for dc in range(8):
                    P.op("pe", "matmul", dict(out=bk[:], lhsT=wv[:, dc, col0 + ch * width:col0 + (ch + 1) * width],
                                              rhs=xT[:, dc, :], start=(dc == 0), stop=(dc == 7)),
                         [B_w, B_xT[dc]], [B_bk])
                yield ch, bk, B_bk

        def post_update(gidx, factor, ss, B_ss, fb=None, B_fb=None, pre_scaled=False, out_fb=False):
            fb = fbuf if fb is None else fb
            B_fb = B_f if B_fb is None else B_fb
            me, B_me = next_st()
            ssv = ss.rearrange("p (m two) -> p m two", two=2)
            P.op("dve", "scalar_tensor_tensor", dict(out=me[:, 0:4], in0=ssv[:, :, 0], scalar=EPS, in1=ssv[:, :, 1],
                                                     op0=ALU.add, op1=ALU.add), [B_ss], [B_me])
            ck("post_a")
            rs, B_rs = rstd_from(me[:, 0:4], B_me, 4, factor)
            ck("post_b")
            gtab = cpar[:, C_GPOST + 1024 * gidx:C_GPOST + 1024 * (gidx + 1)]
            for m in range(4):
                if pre_scaled and out_fb:
                    P.op("dve", "scalar_tensor_tensor", dict(out=fb[:, m, :], in0=fb[:, m, :], scalar=rs[:, m:m + 1],
                                                             in1=h_t[:, m, :], op0=ALU.mult, op1=ALU.add),
                         [B_fb[m], B_rs, B_h[m]], [B_fb[m]])
                elif pre_scaled:
                    P.op("dve", "scalar_tensor_tensor", dict(out=h_t[:, m, :], in0=fb[:, m, :], scalar=rs[:, m:m + 1],
                                                             in1=h_t[:, m, :], op0=ALU.mult, op1=ALU.add),
                         [B_fb[m], B_rs, B_h[m]], [B_h[m]])
                else:
                    P.op("dve", "scalar_tensor_tensor", dict(out=fb[:, m, :], in0=fb[:, m, :], scalar=rs[:, m:m + 1],
                                                             in1=gtab, op0=ALU.mult, op1=ALU.mult),
                         [B_fb[m], B_rs, B_const], [B_fb[m]])
                    P.op("dve", "tensor_tensor", dict(out=h_t[:, m, :], in0=h_t[:, m, :], in1=fb[:, m, :], op=ALU.add),
                         [B_h[m], B_fb[m]], [B_h[m]])

        def bform_post(t, chunks, pieces, gidx, factor):
            nchunks = len(chunks)
            ss8 = stats[:, 56:64]
            B_ss = B_ss_f
            for dh in range(2):
                acc = [next_bank() for _ in range(4)]
                fc = 0
                for (k, n) in pieces[dh]:
                    w, B_w = consume(t, k)
                    for fcl in range(n):
                        ap, B_c = chunks[fc]
                        for m in range(4):
                            P.op("pe", "matmul", dict(out=acc[m][0][:], lhsT=ap[:, m * 128:(m + 1) * 128],
                                                      rhs=w[:, fcl * 512:(fcl + 1) * 512],
                                                      start=(fc == 0), stop=(fc == nchunks - 1)),
                                 [B_c, B_w], [acc[m][1]])
                        fc += 1
                gt_h = cpar[:, C_GPOST + 1024 * gidx + dh * 512:C_GPOST + 1024 * gidx + (dh + 1) * 512]
                for m in range(4):
                    bk, B_bk = acc[m]
                    fsl = fbuf[:, m, dh * 512:(dh + 1) * 512]
                    def _sq():
                        P.op("act", "activation", dict(out=junk[:, 0:512], in_=bk[:], func=AF.Square, scale=1.0 / 32.0,
                                                       accum_out=ss8[:, 2 * m + dh:2 * m + dh + 1]), [B_bk], [B_ss])

                    def _mul():
                        P.op("dve", "tensor_tensor", dict(out=fsl, in0=bk[:], in1=gt_h, op=ALU.mult),
                             [B_bk, B_const], [B_f[m]])
                    if m % 2 == 0:
                        _sq()
                        _mul()
                    else:
                        _mul()
                        _sq()
            post_update(gidx, factor, ss8, B_ss, pre_scaled=True)

        class _Stop(Exception):
            pass

        def ck(name):
            if debug == name:
                raise _Stop()

        def ffn(t, gidx, p_gu, p_dn):
            norm_to_xT(gidx)
            ck("norm%d" % gidx)
            for jj in range(11):
                w, B_w = consume(t, p_gu + jj)
                wv = w.rearrange("p (c n) -> p c n", c=8)
                for jl in range(2):
                    j = 2 * jj + jl
                    gb, B_gb = next_bank()
                    ub, B_ub = next_bank()
                    for (bk, B_bk, c0) in ((gb, B_gb, jl * 128), (ub, B_ub, 256 + jl * 128)):
                        for dc in range(8):
                            P.op("pe", "matmul", dict(out=bk[:], lhsT=wv[:, dc, c0:c0 + 128], rhs=xT[:, dc, :],
                                                      start=(dc == 0), stop=(dc == 7)), [B_w, B_xT[dc]], [B_bk])
                    P.op("act", "activation", dict(out=sgt[:, j % 2, :], in_=gb[:], func=AF.Silu),
                         [B_gb], [B_sg[j % 2]])
                    P.op("dve", "tensor_tensor", dict(out=hidT[:, j, :], in0=sgt[:, j % 2, :], in1=ub[:], op=ALU.mult),
                         [B_sg[j % 2], B_ub], [B_hid[j]])
            ck("gu%d" % gidx)
            chunks = [(hidT[:, j, :], B_hid[j]) for j in range(NFC)]
            pieces = [[(p_dn + dh * 3 + fb, min(8, NFC - 8 * fb)) for fb in range(3)] for dh in range(2)]
            bform_post(t, chunks, pieces, gidx, 0.5)

        def ln_feature_major(src_list, ones_ap, n_src, gb_cols, out_fn):
            raise NotImplementedError

        def mixer(t):
            first = (t % TILES_PER_SEQ == 0)
            norm_to_xT(1)
            if first:
                for c in range(4):
                    P.op("pool", "memset", dict(ap=cext[:, c, 0:30], constant=0.0), [], [B_cext[c]])
            else:
                for c in range(4):
                    P.op("pool", "tensor_copy", dict(out=cext[:, c, 0:30], in_=halo[:, c, :]),
                         [B_halo[c]], [B_cext[c]])
            for c, bk, B_bk in aform(t, P_WIN + 0, 4, 0):
                P.op("act", "activation", dict(out=cacc[:, c, :], in_=bk[:], func=AF.Copy, scale=0.5),
                     [B_bk], [B_cacc[c]])
            for c, bk, B_bk in aform(t, P_WIN + 1, 4, 0):
                P.op("act", "activation", dict(out=ctmp, in_=bk[:], func=AF.Tanh, scale=0.5), [B_bk], [B_ctmp])
                P.op("dve", "scalar_tensor_tensor", dict(out=cext[:, c, 30:542], in0=ctmp, scalar=1.0,
                                                         in1=cacc[:, c, :], op0=ALU.add, op1=ALU.mult),
                     [B_ctmp, B_cacc[c], B_cext[c]], [B_cext[c]])
            for c in range(4):
                P.op("pool", "tensor_copy", dict(out=halo[:, c, :], in_=cext[:, c, 512:542]), [B_cext[c]], [B_halo[c]])
            bgq = []

            def pump(n):
                for _ in range(min(n, len(bgq))):
                    bgq.pop(0)()

            def gen_dg(c, k0, b2):
                nk = min(8, KCONV - k0)
                wk = cpar[:, C_CONVW + c * KCONV + k0:C_CONVW + c * KCONV + k0 + nk]
                P.op("pool", "tensor_tensor", dict(out=dg[:, b2, 0:nk, :],
                                                   in0=ident.unsqueeze(1).to_broadcast([128, nk, 128]),
                                                   in1=wk.unsqueeze(2).to_broadcast([128, nk, 128]), op=ALU.mult),
                     [B_const], [B_dg[b2]])

            gen_dg(0, 0, 0)
            gen_dg(0, 8, 1)

            def conv_pe():
                di = 0
                for c in range(4):
                    bk, B_bk = next_bank()
                    for k0 in range(0, KCONV, 8):
                        nk = min(8, KCONV - k0)
                        b2 = di % 2
                        di += 1
                        if di > 2:
                            gen_dg(c, k0, b2)
                        for kk in range(nk):
                            k = k0 + kk
                            P.op("pe", "matmul", dict(out=bk[:], lhsT=dg[:, b2, kk, :], rhs=cext[:, c, k:k + 512],
                                                      start=(k == 0), stop=(k == KCONV - 1)),
                                 [B_dg[b2], B_cext[c]], [B_bk])
                    P.op("act", "activation", dict(out=cacc[:, c, :], in_=bk[:], func=AF.Identity,
                                                   bias=cpar[:, C_CONVB + c:C_CONVB + c + 1]),
                         [B_bk, B_const], [B_cacc[c]])

            ck("conv")
            for (pk, dstT, B_dst, scale, is_q) in ((P_WIN + 2, qT, B_qT, 0.125, True), (P_WIN + 3, kT, B_kT, 1.0, False)):
                if not is_q:
                    conv_pe()
                for c, bk, B_bk in aform(t, pk, 4, 0):
                    P.op("act", "activation", dict(out=qraw[:], in_=bk[:], func=AF.Copy, scale=scale), [B_bk], [B_qraw])
                    P.op("act", "activation", dict(out=t1[:], in_=bk[:], func=AF.Copy, scale=scale), [B_bk], [B_t1])
                    P.op("pool", "tensor_tensor", dict(out=t1[:], in0=t1[:], in1=cosS[:], op=ALU.mult),
                         [B_t1, B_tab], [B_t1])
                    sw, B_sw = next_bank()
                    P.op("pe", "matmul", dict(out=sw[:], lhsT=pm, rhs=qraw[:], start=True, stop=True),
                         [B_qraw, B_const], [B_sw])
                    P.op("dve", "tensor_tensor", dict(out=t2[:], in0=sw[:], in1=ssinS[:], op=ALU.mult),
                         [B_sw, B_tab], [B_t2])
                    P.op("pool", "tensor_tensor", dict(out=dstT[:, c, :], in0=t1[:], in1=t2[:], op=ALU.add),
                         [B_t1, B_t2], [B_dst[c]])
                    if is_q:
                        qdec_c = cf[:, F_QDEC + c * 128:F_QDEC + (c + 1) * 128]
                        P.op("pool", "tensor_tensor", dict(
                            out=qdT[:, c, :].rearrange("p (m i) -> p m i", m=4),
                            in0=qT[:, c, :].rearrange("p (m i) -> p m i", m=4),
                            in1=qdec_c.unsqueeze(1).to_broadcast([128, 4, 128]), op=ALU.mult),
                             [B_qT[c], B_const], [B_qdT[c]])
            ck("qk")
            w, B_w = consume(t, P_WIN + 4)
            wv = w.rearrange("p (c n) -> p c n", c=8)
            for m in range(4):
                bk, B_bk = next_bank()
                for dc in range(8):
                    P.op("pe", "matmul", dict(out=bk[:], lhsT=xT[:, dc, m * 128:(m + 1) * 128], rhs=wv[:, dc, :],
                                              start=(dc == 0), stop=(dc == 7)), [B_w, B_xT[dc]], [B_bk])
                P.op("act", "activation", dict(out=vpl[:, m, :], in_=bk[:], func=AF.Copy), [B_bk], [B_vpl[m]])
            for c, bk, B_bk in aform(t, P_WIN + 5, 4, 0):
                P.op("act", "activation", dict(out=sgr[:, c, :], in_=bk[:], func=AF.Silu), [B_bk], [B_sgr[c]])
                P.op("dve", "tensor_scalar", dict(out=sgrb[:, c, :], in0=sgr[:, c, :], scalar1=cpar[:, C_RGB + c:C_RGB + c + 1],
                                                  scalar2=None, op0=ALU.mult), [B_sgr[c], B_const], [B_sgrb[c]])
                P.op("dve", "tensor_scalar", dict(out=sgr[:, c, :], in0=sgr[:, c, :], scalar1=cpar[:, C_RGG + c:C_RGG + c + 1],
                                                  scalar2=None, op0=ALU.mult), [B_sgr[c], B_const], [B_sgr[c]])
            ck("vg")
            cl = {}

            def conv_ln1():
                for c in range(4):
                    P.op("act", "activation", dict(out=cb16[:, c, :], in_=cacc[:, c, :], func=AF.Copy),
                         [B_cacc[c]], [B_cb16[c]])
                mb, B_mb = banks[7], B_bank[7]
                cl["mb"] = (mb, B_mb)
                for c in range(4):
                    P.op("pe", "matmul", dict(out=mb[:], lhsT=o512, rhs=cb16[:, c, :], start=(c == 0), stop=(c == 3)),
                         [B_cb16[c], B_const], [B_mb])

            def conv_ln2():
                mb, B_mb = cl["mb"]
                for c in range(4):
                    P.op("dve", "tensor_tensor", dict(out=cacc[:, c, :], in0=cacc[:, c, :], in1=mb[:], op=ALU.subtract),
                         [B_cacc[c], B_mb], [B_cacc[c]])
                    P.op("act", "activation", dict(out=cb16[:, c, :], in_=cacc[:, c, :], func=AF.Square),
                         [B_cacc[c]], [B_cb16[c]])
                vb, B_vb = banks[7], B_bank[7]
                cl["vb"] = (vb, B_vb)
                for c in range(4):
                    P.op("pe", "matmul", dict(out=vb[:], lhsT=o512, rhs=cb16[:, c, :], start=(c == 0), stop=(c == 3)),
                         [B_cb16[c], B_const], [B_vb])

            def conv_ln3():
                vb, B_vb = cl["vb"]
                P.op("act", "activation", dict(out=ctmp, in_=vb[:], func=AF.Sqrt, bias=EPS), [B_vb], [B_ctmp])
                P.op("dve", "reciprocal", dict(out=ctmp, in_=ctmp), [B_ctmp], [B_ctmp])
                for c in range(4):
                    P.op("pool" if c % 2 else "dve", "tensor_tensor",
                         dict(out=cacc[:, c, :], in0=cacc[:, c, :], in1=ctmp, op=ALU.mult),
                         [B_cacc[c], B_ctmp], [B_cacc[c]])
                    P.op("act", "activation", dict(out=mixT[:, 4 + c, :], in_=cacc[:, c, :], func=AF.Silu,
                                                   scale=cpar[:, C_CLNG + c:C_CLNG + c + 1],
                                                   bias=cpar[:, C_CLNB + c:C_CLNB + c + 1]),
                         [B_cacc[c], B_const], [B_mix[4 + c]])

            ck("convln")
            if first:
                P.op("pool", "memset", dict(ap=R_t[:], constant=0.0), [], [B_R])
                P.op("pool", "memset", dict(ap=Rbf[:], constant=0.0), [], [B_Rbf])
            def A1(m):
                cs = slice(m * 128, (m + 1) * 128)
                i2 = m % 2
                kb, B_kb = next_bank()
                kbv = kb[:].bitcast(BF16)
                for pp in range(4):
                    P.op("pe", "transpose", dict(out=kbv[:, pp * 128:(pp + 1) * 128], in_=kT[:, pp, cs], identity=ident),
                         [B_kT[pp], B_const], [B_kb])
                P.op("dve", "tensor_tensor", dict(
                    out=kd[:, i2, :].rearrange("p (h d) -> p h d", h=8),
                    in0=kbv[:, 0:512].rearrange("p (h d) -> p h d", h=8),
                    in1=cf[:, F_KDEC:F_KDEC + 8].unsqueeze(2).to_broadcast([128, 8, 64]), op=ALU.mult),
                     [B_kb, B_const], [B_kd[i2]])
                vsrc = vpl[:, m, :].rearrange("p (a b e) -> p a b e", a=4, b=2)
                vdst = vpad[:, i2].rearrange("p (a b) e -> p a b e", a=4)
                P.op("pool", "tensor_copy", dict(out=vdst[:, :, 0, 0:64], in_=vsrc[:, :, 0, :]), [B_vpl[m]], [B_vpad[i2]])
                P.op("pool", "tensor_copy", dict(out=vdst[:, :, 1, 64:128], in_=vsrc[:, :, 1, :]), [B_vpl[m]], [B_vpad[i2]])
                sa, B_sa = next_bank()
                sbk, B_sbk = next_bank()
                for pp in range(4):
                    for hh, (bk, B_bk) in enumerate(((sa, B_sa), (sbk, B_sbk))):
                        rs_ = slice(hh * 64, (hh + 1) * 64)
                        P.op("pe", "matmul", dict(out=bk[:, pp * 128:(pp + 1) * 128], lhsT=kT[rs_, pp, cs],
                                                  rhs=qT[rs_, pp, cs], start=True, stop=True),
                             [B_kT[pp], B_qT[pp]], [B_bk])
                for hh, (bk, B_bk) in enumerate(((sa, B_sa), (sbk, B_sbk))):
                    P.op("dve", "tensor_tensor", dict(
                        out=sTm[:, i2, hh * 4:(hh + 1) * 4, :].rearrange("p a i -> p (a i)"),
                        in0=bk[:], in1=cf[:, F_MASK + hh * 512:F_MASK + (hh + 1) * 512], op=ALU.mult),
                         [B_bk, B_const], [B_sTm[i2]])

            obanks = {}

            def A2(m):
                cs = slice(m * 128, (m + 1) * 128)
                i2 = m % 2
                ob, B_ob = next_bank()
                obanks[m] = (ob, B_ob)
                for pp in range(4):
                    osl = ob[:, pp * 128:(pp + 1) * 128]
                    P.op("pe", "matmul", dict(out=osl, lhsT=vpad[:, i2, 2 * pp, :], rhs=sTm[:, i2, pp, :],
                                              start=True, stop=False), [B_vpad[i2], B_sTm[i2]], [B_ob])
                    P.op("pe", "matmul", dict(out=osl, lhsT=vpad[:, i2, 2 * pp + 1, :], rhs=sTm[:, i2, 4 + pp, :],
                                              start=False, stop=False), [B_vpad[i2], B_sTm[i2]], [B_ob])
                    P.op("pe", "matmul", dict(out=osl, lhsT=Rbf[:, pp * 128:(pp + 1) * 128], rhs=qdT[:, pp, cs],
                                              start=False, stop=True), [B_Rbf, B_qdT[pp]], [B_ob])
                kvb, B_kvb = next_bank()
                for pp in range(4):
                    P.op("pe", "matmul", dict(out=kvb[:, pp * 128:(pp + 1) * 128], lhsT=kd[:, i2, pp * 128:(pp + 1) * 128],
                                              rhs=vpl[:, m, pp * 128:(pp + 1) * 128], start=True, stop=True),
                         [B_kd[i2], B_vpl[m]], [B_kvb])
                P.op("dve", "tensor_tensor", dict(
                    out=kvt[:].rearrange("p (a e) -> p a e", a=4), in0=kvb[:].rearrange("p (a e) -> p a e", a=4),
                    in1=cf[:, F_BDM:F_BDM + 128].unsqueeze(1).to_broadcast([128, 4, 128]), op=ALU.mult),
                     [B_kvb, B_const], [B_kvt])
                P.op("pool", "tensor_tensor", dict(
                    out=R_t[:].rearrange("p (a e) -> p a e", a=4), in0=R_t[:].rearrange("p (a e) -> p a e", a=4),
                    in1=cf[:, F_CDEC:F_CDEC + 4].unsqueeze(2).to_broadcast([128, 4, 128]), op=ALU.mult),
                     [B_R, B_const], [B_R])
                P.op("pool", "tensor_tensor", dict(out=R_t[:], in0=R_t[:], in1=kvt[:], op=ALU.add), [B_R, B_kvt], [B_R])
                P.op("act", "activation", dict(out=Rbf[:], in_=R_t[:], func=AF.Copy), [B_R], [B_Rbf])

            def B1(m):
                cen, B_cen, csq, B_csq = cen2[:, m % 2, :], B_cen2[m % 2], csq2[:, m % 2, :], B_csq2[m % 2]
                ob, B_ob = obanks[m]
                P.op("act", "activation", dict(out=o_f[:], in_=ob[:], func=AF.Copy), [B_ob], [B_of])
                P.op("act", "activation", dict(out=o_bf[:], in_=ob[:], func=AF.Copy), [B_ob], [B_obf])
                mb, B_mb = next_bank()
                P.op("pe", "matmul", dict(out=mb[:], lhsT=bd64, rhs=o_bf[:], start=True, stop=True),
                     [B_obf, B_const], [B_mb])
                P.op("dve", "tensor_tensor", dict(out=cen[:], in0=o_f[:], in1=mb[:], op=ALU.subtract),
                     [B_of, B_mb], [B_cen])
                P.op("act", "activation", dict(out=csq[:], in_=cen[:], func=AF.Square), [B_cen], [B_csq])

            def B2(m):
                cen, B_cen, csq, B_csq = cen2[:, m % 2, :], B_cen2[m % 2], csq2[:, m % 2, :], B_csq2[m % 2]
                cs = slice(m * 128, (m + 1) * 128)
                vb, B_vb = next_bank()
                P.op("pe", "matmul", dict(out=vb[:], lhsT=bd64, rhs=csq[:], start=True, stop=True),
                     [B_csq, B_const], [B_vb])
                P.op("act", "activation", dict(out=vare[:], in_=vb[:], func=AF.Sqrt, bias=EPS), [B_vb], [B_vare])
                P.op("dve", "reciprocal", dict(out=rstdL[:], in_=vare[:]), [B_vare], [B_rstdL])
                P.op("dve", "tensor_tensor", dict(out=cen[:], in0=cen[:], in1=rstdL[:], op=ALU.mult),
                     [B_cen, B_rstdL], [B_cen])
                P.op("pool", "tensor_tensor", dict(out=zt[:].rearrange("p (a i) -> p a i", a=4),
                                                   in0=cen[:].rearrange("p (a i) -> p a i", a=4),
                                                   in1=sgr[:, :, cs], op=ALU.mult), [B_cen] + B_sgr, [B_zt])
                P.op("pool", "tensor_tensor", dict(out=mixT[:, 0:4, cs], in0=zt[:].rearrange("p (a i) -> p a i", a=4),
                                                   in1=sgrb[:, :, cs], op=ALU.add), [B_zt] + B_sgrb, B_mix[0:4])

            conv_ln1()
            A1(0)
            A2(0)
            B1(0)
            for m in range(1, 4):
                A1(m)
                A2(m)
                B1(m)
                B2(m - 1)
                if m == 1:
                    conv_ln2()
            B2(3)
            conv_ln3()
            pump(len(bgq))
            ck("ret")
            chunks = [(mixT[:, c, :], B_mix[c]) for c in range(8)]
            bform_post(t, chunks, [[(P_WOUT + 0, 8)], [(P_WOUT + 1, 8)]], 1, 1.0)

        def ple(t):
            norm_to_xT(3)
            P.op("pool", "tensor_copy", dict(out=p_bf[:], in_=ptile[:]), [B_p], [B_pbf])
            bk, B_bk = next_bank()
            bv = bk[:].bitcast(BF16)
            for dc in range(2):
                for m in range(4):
                    P.op("pe", "transpose", dict(out=bv[:, dc * 512 + m * 128:dc * 512 + (m + 1) * 128],
                                                 in_=p_bf[:, m, dc * 128:(dc + 1) * 128], identity=ident),
                         [B_pbf, B_const], [B_bk])
            P.op("act", "activation", dict(out=pT[:].rearrange("p c t -> p (c t)"), in_=bv[:, 0:1024], func=AF.Copy),
                 [B_bk], [B_pT])
            we, B_we = consume(t, P_PLEW)
            wev = we[:, 0:2048].rearrange("p (c n) -> p c n", c=2)
            ss8 = stats[:, 48:56]
            B_ss = B_ss_p
            for dh in range(2):
                wg, B_wg = consume(t, P_GATE + dh, hold=1 + dh)
                wgv = wg.rearrange("p (c n) -> p c n", c=8)
                for m in range(4):
                    ms_ = slice(m * 128, (m + 1) * 128)
                    gbk, B_gbk = next_bank()
                    ebk, B_ebk = next_bank()
                    for dc in range(8):
                        P.op("pe", "matmul", dict(out=gbk[:], lhsT=xT[:, dc, ms_], rhs=wgv[:, dc, :],
                                                  start=(dc == 0), stop=(dc == 7)), [B_xT[dc], B_wg], [B_gbk])
                    for dc in range(2):
                        P.op("pe", "matmul", dict(out=ebk[:], lhsT=pT[:, dc, ms_], rhs=wev[:, dc, dh * 512:(dh + 1) * 512],
                                                  start=(dc == 0), stop=(dc == 1)), [B_pT, B_we], [B_ebk])
                    P.op("act", "activation", dict(out=sgt[:, m % 2, :], in_=gbk[:], func=AF.Tanh, scale=0.5),
                         [B_gbk], [B_sg[m % 2]])
                    fsl = fple[:, m, dh * 512:(dh + 1) * 512]
                    P.op("dve", "scalar_tensor_tensor", dict(out=fsl, in0=sgt[:, m % 2, :], scalar=1.0, in1=ebk[:],
                                                             op0=ALU.add, op1=ALU.mult), [B_sg[m % 2], B_ebk], [B_fp[m]])
                    P.op("act", "activation", dict(out=junk[:, 0:512], in_=fsl, func=AF.Square, scale=1.0 / 64.0,
                                                   accum_out=ss8[:, 2 * m + dh:2 * m + dh + 1]), [B_fp[m]], [B_ss])
                    P.op("dve", "tensor_tensor", dict(
                        out=fsl, in0=fsl, in1=cpar[:, C_GPOST + 3 * 1024 + dh * 512:C_GPOST + 3 * 1024 + (dh + 1) * 512],
                        op=ALU.mult), [B_fp[m], B_const], [B_fp[m]])
            post_update(3, 0.5, ss8, B_ss, fple, B_fp, pre_scaled=True, out_fb=True)
            state["final_in_fple"] = True

        def store_tile(t):
            r0 = t * T
            for m in range(4):
                if state.get("final_in_fple"):
                    P.dma("sp", out_d[r0 + m * 128:r0 + (m + 1) * 128, :], fple[:, m, :], [B_fp[m]], [B_out[m]], ch_o[m])
                else:
                    P.dma("sp", out_d[r0 + m * 128:r0 + (m + 1) * 128, :], h_t[:, m, :], [B_h[m]], [B_out[m]], ch_o[m])
            state["final_in_fple"] = False

        for t in range(ntiles):
            load_tile(t)
            try:
                ck("load")
                ffn(t, 0, P_GU1, P_DN1)
                ck("ffn1")
                mixer(t)
                ck("mixer")
                ffn(t, 2, P_GU2, P_DN2)
                ck("ffn2")
                ple(t)
            except _Stop:
                pass
            store_tile(t)
        P.wait_all("sp", B_out + B_scr)
        nwait = P.emit(stack)
        build_program.info = dict(nops=len(P.ops), nwait=nwait, per_eng=dict(P.nseq))
    return nc


_CACHE = {}


def _host_inputs(inp):
    cf, cosT, ssinT, cb = _const_tables()
    wp = _layout_weights(inp)
    cp = _layout_params(inp)
    x = np.ascontiguousarray(np.asarray(inp["x"], np.float32)).reshape(NCORES, TOK_CORE, D)
    p = np.ascontiguousarray(np.asarray(inp["p"], np.float32)[0]).reshape(NCORES, TOK_CORE, 256)
    maps = []
    for c in range(NCORES):
        maps.append(dict(x=x[c], p=p[c], wp=wp, cpar=cp, cf=cf, cosT=cosT, ssinT=ssinT, cb=cb))
    return maps


def kernel(**inputs):
    inp = {k: np.asarray(v) for k, v in inputs.items()}
    if "nc" not in _CACHE:
        _CACHE["nc"] = build_program(NTILES)
    nc = _CACHE["nc"]
    maps = _host_inputs(inp)
    res = run_bass_kernel_spmd(nc, maps, core_ids=list(range(NCORES)))
    out = np.stack([np.asarray(r["out"], np.float32) for r in res.results], axis=0)
    return out.reshape(BATCH, SEQ, D)
```
